# Optimizing a Trainium2 kernel written in Bass

```python
import jax, jax.numpy as jnp
from jax import lax
import numpy as np

D_MODEL = 1024
BATCH = 2
SEQ = 8192
DEPTH = 4

GRID_W = 64
CTX_LEN = 256
A_GROUPS = 4
A_GROUP_DIM = 128
A_WIDTH = A_GROUPS * A_GROUP_DIM
B_WIDTH = 512
CONV_WIDTH = 31
CONV_PAD = (CONV_WIDTH - 1) // 2
AB_IN = A_WIDTH + 2 * B_WIDTH
AB_MIX = A_WIDTH + B_WIDTH
C_HEADS = 4
C_KV_HEADS = 2
C_GROUP = C_HEADS // C_KV_HEADS
C_HEAD_DIM = 128
C_WIDTH = C_HEADS * C_HEAD_DIM
C_KV_WIDTH = C_KV_HEADS * C_HEAD_DIM
D_HEADS = 4
D_NOPE = 128
D_ROPE = 64
D_V = 128
Q_LORA = 384
KV_LORA = 256
D_WIDTH = D_HEADS * D_V
CD_IN = C_WIDTH + 2 * C_KV_WIDTH + Q_LORA + KV_LORA + D_ROPE
CD_SPLITS = [C_WIDTH, C_WIDTH + C_KV_WIDTH, C_WIDTH + 2 * C_KV_WIDTH,
             C_WIDTH + 2 * C_KV_WIDTH + Q_LORA, C_WIDTH + 2 * C_KV_WIDTH + Q_LORA + KV_LORA]
CD_MIX = C_WIDTH + D_WIDTH
Q_BLOCK = 128
ROPE_THETA = 10000.0
N_GROUPS = 4
EXPERTS_PER_GROUP = 8
N_EXPERTS = N_GROUPS * EXPERTS_PER_GROUP
TOP_K = 2
EXPERT_FF = 512
EXPERT_BLOCK = 128
N_MOD = 6
RMS_EPS = 1e-6
MOD_INIT = 0.5
ROUTER_BIAS_INIT = 0.01
N_EVEN = (DEPTH + 1) // 2
N_ODD = DEPTH // 2

kernel_name = 'hybrid_fourier_conv_gqa_mla_hmoe_dit'


def rmsnorm(x, g):
    xf = x.astype(jnp.float32)
    y = xf * lax.rsqrt(jnp.mean(xf * xf, axis=-1, keepdims=True) + RMS_EPS)
    return y.astype(x.dtype) * g


def axial_rope_tables(row, col, dim):
    n_freq = dim // 4
    inv = ROPE_THETA ** (-jnp.arange(n_freq, dtype=jnp.float32) / n_freq)
    ang = jnp.concatenate([row[:, None] * inv[None, :], col[:, None] * inv[None, :]], axis=-1)
    return jnp.cos(ang), jnp.sin(ang)


def apply_rope(x, cos, sin):
    xf = x.astype(jnp.float32).reshape(x.shape[:-1] + (x.shape[-1] // 2, 2))
    xr, xi = xf[..., 0], xf[..., 1]
    c = cos[None, :, None, :]
    s = sin[None, :, None, :]
    out = jnp.stack([xr * c - xi * s, xr * s + xi * c], axis=-1)
    return out.reshape(x.shape).astype(x.dtype)


def block_attention(q, k, v, scale):
    b, s, kh, g, d = q.shape
    nb = s // Q_BLOCK
    qb = jnp.moveaxis(q.reshape(b, nb, Q_BLOCK, kh, g, d), 1, 0)

    def one_block(qblk):
        sc = jnp.einsum('bqhgd,bkhd->bhgqk', qblk, k).astype(jnp.float32) * scale
        p = jax.nn.softmax(sc, axis=-1).astype(v.dtype)
        return jnp.einsum('bhgqk,bkhe->bqhge', p, v)

    o = lax.map(one_block, qb)
    return jnp.moveaxis(o, 0, 1).reshape(b, s, kh, g, v.shape[-1])


def fourier_conv_mixer(h, w_in, conv_w, conv_b, conv_g, w_out):
    b, L, _ = h.shape
    p = h @ w_in
    pa, pu, pg = jnp.split(p, [A_WIDTH, A_WIDTH + B_WIDTH], axis=-1)
    fa = pa.astype(jnp.float32).reshape(b, L, A_GROUPS, A_GROUP_DIM)
    ya = jnp.fft.fft2(fa, axes=(1, 3), norm='ortho').real.reshape(b, L, A_WIDTH).astype(h.dtype)
    u = pu * jax.nn.sigmoid(pg)
    u = lax.conv_general_dilated(u, conv_w[:, None, :], (1,), [(CONV_PAD, CONV_PAD)],
                                 dimension_numbers=('NWC', 'WIO', 'NWC'),
                                 feature_group_count=B_WIDTH) + conv_b
    yb = jax.nn.silu(rmsnorm(u, conv_g))
    return jnp.concatenate([ya, yb], axis=-1) @ w_out


def cd_project(t, w_in, q_g, k_g, cq_g, ckv_g, w_uq, w_ukv):
    bt, L, _ = t.shape
    q, k, v, cq, ckv, kr = jnp.split(t @ w_in, CD_SPLITS, axis=-1)
    q = rmsnorm(q.reshape(bt, L, C_HEADS, C_HEAD_DIM), q_g)
    k = rmsnorm(k.reshape(bt, L, C_KV_HEADS, C_HEAD_DIM), k_g)
    v = v.reshape(bt, L, C_KV_HEADS, C_HEAD_DIM)
    qd = (rmsnorm(cq, cq_g) @ w_uq).reshape(bt, L, D_HEADS, D_NOPE + D_ROPE)
    kvd = (rmsnorm(ckv, ckv_g) @ w_ukv).reshape(bt, L, D_HEADS, D_NOPE + D_V)
    kd_rope = kr[:, :, None, :]
    return (q, k, v, qd[..., :D_NOPE], qd[..., D_NOPE:], kvd[..., :D_NOPE], kd_rope, kvd[..., D_NOPE:])


def cd_attend(q, qdn, qdr, k, v, kdn, kdr, vd):
    b, L = q.shape[:2]
    oc = block_attention(q.reshape(b, L, C_KV_HEADS, C_GROUP, C_HEAD_DIM), k, v, C_HEAD_DIM ** -0.5)
    qd = jnp.concatenate([qdn, qdr], axis=-1)[:, :, :, None, :]
    kd = jnp.concatenate([kdn, jnp.broadcast_to(kdr, kdn.shape[:-1] + (D_ROPE,))], axis=-1)
    od = block_attention(qd, kd, vd, (D_NOPE + D_ROPE) ** -0.5)
    return jnp.concatenate([oc.reshape(b, L, C_WIDTH), od.reshape(b, L, D_WIDTH)], axis=-1)


def attn_mla_mixer(h, hc, w_in, q_g, k_g, cq_g, ckv_g, w_uq, w_ukv, w_out, rope_c, rope_d, with_ctx_out):
    q, k, v, qdn, qdr, kdn, kdr, vd = cd_project(h, w_in, q_g, k_g, cq_g, ckv_g, w_uq, w_ukv)
    cq, ck, cv, cqdn, cqdr, ckdn, ckdr, cvd = cd_project(hc, w_in, q_g, k_g, cq_g, ckv_g, w_uq, w_ukv)
    q, k = apply_rope(q, *rope_c), apply_rope(k, *rope_c)
    qdr, kdr = apply_rope(qdr, *rope_d), apply_rope(kdr, *rope_d)
    cat = lambda a, b: jnp.concatenate([a, b], axis=1)
    y = cd_attend(q, qdn, qdr, cat(ck, k), cat(cv, v), cat(ckdn, kdn), cat(ckdr, kdr), cat(cvd, vd)) @ w_out
    yc = cd_attend(cq, cqdn, cqdr, ck, cv, ckdn, ckdr, cvd) @ w_out if with_ctx_out else None
    return y, yc


def routed_experts(h, e_idx, e_w, w_gate, w_up, w_down):
    n_tok, d = h.shape
    n_exp = w_gate.shape[0]
    n_asg = e_idx.shape[0] * e_idx.shape[1]
    n_blocks = (n_asg + n_exp * (EXPERT_BLOCK - 1) + EXPERT_BLOCK - 1) // EXPERT_BLOCK
    n_rows = n_blocks * EXPERT_BLOCK
    flat_e = e_idx.reshape(-1)
    flat_tok = jnp.arange(n_asg, dtype=jnp.int32) // e_idx.shape[1]
    flat_w = e_w.reshape(-1)
    order = jnp.argsort(flat_e)
    sorted_e = flat_e[order]
    counts = jnp.bincount(flat_e, length=n_exp)
    padded = (counts + EXPERT_BLOCK - 1) // EXPERT_BLOCK * EXPERT_BLOCK
    padded_end = jnp.cumsum(padded)
    start = jnp.cumsum(counts) - counts
    dest = (padded_end - padded)[sorted_e] + jnp.arange(n_asg, dtype=jnp.int32) - start[sorted_e]
    row_tok = jnp.full((n_rows,), n_tok, jnp.int32).at[dest].set(flat_tok[order])
    row_w = jnp.zeros((n_rows,), h.dtype).at[dest].set(flat_w[order])
    block_exp = jnp.minimum(jnp.searchsorted(padded_end, jnp.arange(n_blocks, dtype=jnp.int32) * EXPERT_BLOCK,
                                             side='right'), n_exp - 1)
    h_pad = jnp.concatenate([h, jnp.zeros((1, d), h.dtype)], axis=0)
    xb = h_pad[row_tok].reshape(n_blocks, EXPERT_BLOCK, d)

    def expert_block(args):
        xblk, e = args
        return (jax.nn.silu(xblk @ w_gate[e]) * (xblk @ w_up[e])) @ w_down[e]

    yb = lax.map(expert_block, (xb, block_exp)).reshape(n_rows, d)
    return jnp.zeros((n_tok + 1, d), h.dtype).at[row_tok].add(yb * row_w[:, None])[:n_tok]


def hier_moe(h, grp_w, grp_b, exp_w, exp_b, w_gate, w_up, w_down):
    n = h.shape[0]
    g_logit = (h @ grp_w).astype(jnp.float32) + grp_b.astype(jnp.float32)
    g_idx = jnp.argmax(g_logit, axis=-1).astype(jnp.int32)
    g_prob = jnp.take_along_axis(jax.nn.softmax(g_logit, axis=-1), g_idx[:, None], axis=-1)
    e_logit = ((h @ exp_w).astype(jnp.float32) + exp_b.astype(jnp.float32)).reshape(n, N_GROUPS, EXPERTS_PER_GROUP)
    e_logit = jnp.take_along_axis(e_logit, g_idx[:, None, None], axis=1)[:, 0]
    top_v, top_i = lax.top_k(e_logit, TOP_K)
    w = (g_prob * jax.nn.softmax(top_v, axis=-1)).astype(h.dtype)
    e_idx = g_idx[:, None] * EXPERTS_PER_GROUP + top_i.astype(jnp.int32)
    return routed_experts(h, e_idx, w, w_gate, w_up, w_down)


def setup_inputs(seed: int = 0) -> dict:
    key = jax.random.key(seed)
    ks = jax.random.split(key, 29)
    f32 = jnp.float32
    nrm = lambda k, shape, s: jax.random.normal(k, shape, f32) * s
    gain = lambda k, shape: 1.0 + 0.05 * jax.random.normal(k, shape, f32)
    ne, no = N_EVEN, N_ODD
    return {
        'x': nrm(ks[0], (BATCH, SEQ, D_MODEL), 1.0),
        'c': nrm(ks[1], (BATCH, D_MODEL), 1.0),
        'ctx': nrm(ks[2], (BATCH, CTX_LEN, D_MODEL), 1.0),
        'c_ctx': nrm(ks[3], (D_MODEL,), 1.0),
        'mod_w': nrm(ks[4], (DEPTH, D_MODEL, N_MOD * D_MODEL), MOD_INIT * D_MODEL ** -0.5),
        'mod_b': nrm(ks[5], (DEPTH, N_MOD * D_MODEL), 0.02),
        'norm_mix_g': gain(ks[6], (DEPTH, D_MODEL)),
        'norm_ffn_g': gain(ks[7], (DEPTH, D_MODEL)),
        'w_in_ab': nrm(ks[8], (ne, D_MODEL, AB_IN), D_MODEL ** -0.5),
        'conv_w': nrm(ks[9], (ne, CONV_WIDTH, B_WIDTH), CONV_WIDTH ** -0.5),
        'conv_b': nrm(ks[10], (ne, B_WIDTH), 0.02),
        'conv_norm_g': gain(ks[11], (ne, B_WIDTH)),
        'w_out_ab': nrm(ks[12], (ne, AB_MIX, D_MODEL), AB_MIX ** -0.5),
        'w_in_cd': nrm(ks[13], (no, D_MODEL, CD_IN), D_MODEL ** -0.5),
        'q_norm_g': gain(ks[14], (no, C_HEAD_DIM)),
        'k_norm_g': gain(ks[15], (no, C_HEAD_DIM)),
        'cq_norm_g': gain(ks[16], (no, Q_LORA)),
        'ckv_norm_g': gain(ks[17], (no, KV_LORA)),
        'w_uq': nrm(ks[18], (no, Q_LORA, D_HEADS * (D_NOPE + D_ROPE)), Q_LORA ** -0.5),
        'w_ukv': nrm(ks[19], (no, KV_LORA, D_HEADS * (D_NOPE + D_V)), KV_LORA ** -0.5),
        'w_out_cd': nrm(ks[20], (no, CD_MIX, D_MODEL), CD_MIX ** -0.5),
        'router_grp_w': nrm(ks[21], (DEPTH, D_MODEL, N_GROUPS), D_MODEL ** -0.5),
        'router_grp_b': nrm(ks[22], (DEPTH, N_GROUPS), ROUTER_BIAS_INIT),
        'router_exp_w': nrm(ks[23], (DEPTH, D_MODEL, N_EXPERTS), D_MODEL ** -0.5),
        'router_exp_b': nrm(ks[24], (DEPTH, N_EXPERTS), ROUTER_BIAS_INIT),
        'exp_w_gate': nrm(ks[25], (DEPTH, N_EXPERTS, D_MODEL, EXPERT_FF), D_MODEL ** -0.5),
        'exp_w_up': nrm(ks[26], (DEPTH, N_EXPERTS, D_MODEL, EXPERT_FF), D_MODEL ** -0.5),
        'exp_w_down': nrm(ks[27], (DEPTH, N_EXPERTS, EXPERT_FF, D_MODEL), EXPERT_FF ** -0.5),
        'final_norm_g': gain(ks[28], (D_MODEL,)),
    }


def reference(x, c, ctx, c_ctx, mod_w, mod_b, norm_mix_g, norm_ffn_g, w_in_ab, conv_w, conv_b,
              conv_norm_g, w_out_ab, w_in_cd, q_norm_g, k_norm_g, cq_norm_g, ckv_norm_g, w_uq, w_ukv,
              w_out_cd, router_grp_w, router_grp_b, router_exp_w, router_exp_b, exp_w_gate, exp_w_up,
              exp_w_down, final_norm_g):
    b, s, d = x.shape
    rows = s // GRID_W
    row_pos = jnp.repeat(jnp.arange(rows, dtype=jnp.float32), GRID_W)
    col_pos = jnp.tile(jnp.arange(GRID_W, dtype=jnp.float32), rows)
    rope_c = axial_rope_tables(row_pos, col_pos, C_HEAD_DIM)
    rope_d = axial_rope_tables(row_pos, col_pos, D_ROPE)
    silu_c = jax.nn.silu(c)
    silu_cc = jax.nn.silu(c_ctx)
    xc = ctx
    for layer in range(DEPTH):
        last = layer == DEPTH - 1
        j = layer // 2
        sh1, sc1, g1, sh2, sc2, g2 = jnp.split((silu_c @ mod_w[layer] + mod_b[layer])[:, None, :], N_MOD, axis=-1)
        csh1, csc1, cg1, csh2, csc2, cg2 = jnp.split(silu_cc @ mod_w[layer] + mod_b[layer], N_MOD)
        h = rmsnorm(x, norm_mix_g[layer]) * (1.0 + sc1) + sh1
        if layer % 2 == 0:
            ab = (w_in_ab[j], conv_w[j], conv_b[j], conv_norm_g[j], w_out_ab[j])
            x = x + g1 * fourier_conv_mixer(h, *ab)
            if not last:
                hc = rmsnorm(xc, norm_mix_g[layer]) * (1.0 + csc1) + csh1
                xc = xc + cg1 * fourier_conv_mixer(hc, *ab)
        else:
            hc = rmsnorm(xc, norm_mix_g[layer]) * (1.0 + csc1) + csh1
            y, yc = attn_mla_mixer(h, hc, w_in_cd[j], q_norm_g[j], k_norm_g[j], cq_norm_g[j], ckv_norm_g[j],
                                   w_uq[j], w_ukv[j], w_out_cd[j], rope_c, rope_d, not last)
            x = x + g1 * y
            if not last:
                xc = xc + cg1 * yc
        moe = (router_grp_w[layer], router_grp_b[layer], router_exp_w[layer], router_exp_b[layer],
               exp_w_gate[layer], exp_w_up[layer], exp_w_down[layer])
        h2 = rmsnorm(x, norm_ffn_g[layer]) * (1.0 + sc2) + sh2
        if last:
            x = x + g2 * hier_moe(h2.reshape(b * s, d), *moe).reshape(x.shape)
        else:
            hc2 = rmsnorm(xc, norm_ffn_g[layer]) * (1.0 + csc2) + csh2
            out = hier_moe(jnp.concatenate([h2.reshape(b * s, d), hc2.reshape(-1, d)], axis=0), *moe)
            x = x + g2 * out[: b * s].reshape(x.shape)
            xc = xc + cg2 * out[b * s:].reshape(xc.shape)
    return rmsnorm(x, final_norm_g)
```

```python
import numpy as np
from contextlib import ExitStack
import concourse.bass as bass
import concourse.mybir as mybir
from concourse.bass_utils import run_bass_kernel_spmd

F32 = mybir.dt.float32
BF16 = mybir.dt.bfloat16
AF = mybir.ActivationFunctionType
ALU = mybir.AluOpType
AX = mybir.AxisListType

NT, TL, TCX = 2304, 2048, 256
CH = [(0, 512), (512, 512), (1024, 512), (1536, 512), (2048, 256)]
EPS = 1e-6


class Trk:
    __slots__ = ("lw", "rd")

    def __init__(self):
        self.lw = None
        self.rd = []


class T:
    def __init__(self, h, name=""):
        self.h = h
        self.name = name
        self.trk = Trk()
        self.subs = {}

    def __getitem__(self, idx):
        return V(self.trk, self.h[idx])

    @property
    def ap(self):
        return V(self.trk, self.h[:])

    def sv(self, key, idx):
        t = self.subs.get(key)
        if t is None:
            t = self.subs[key] = Trk()
        return V(t, self.h[idx])


class V:
    __slots__ = ("t", "a")

    def __init__(self, t, a):
        self.t = t
        self.a = a

    def __getitem__(self, idx):
        return V(self.t, self.a[idx])

    def re(self, pat, **kw):
        return V(self.t, self.a.rearrange(pat, **kw))

    def bc(self, shape):
        return V(self.t, self.a.broadcast_to(list(shape)))


class Sched:
    NDMA = 24
    NSW = 16

    def __init__(self, nc, es):
        self.nc = nc
        self.es = es
        self.engs = {"pe": nc.tensor, "act": nc.scalar, "dve": nc.vector, "pool": nc.gpsimd, "sp": nc.sync}
        self.sem = {}
        self.cnt = {}
        self.es0 = es
        for k in list(self.engs) + ["d%d" % i for i in range(self.NDMA)] + ["cc"]:
            self.sem[k] = es.enter_context(nc.semaphore("s_" + k))
            self.cnt[k] = 0
        self.dma_rr = 0
        self.sw_rr = 0
        self.gen = {}
        self.cons = {}
        self.pend = {e: [] for e in self.engs}
        self.seen = {e: {} for e in self.engs}
        self.ninst = 0
        self.uid = 0

    def sb(self, name, shape, dt=F32, es=None):
        self.uid += 1
        h = (es or self.es).enter_context(self.nc.sbuf_tensor("%s_%d" % (name, self.uid), list(shape), dt))
        return T(h, name)

    def ps(self, name, shape, dt=F32):
        h = self.es.enter_context(self.nc.psum_tensor(name, list(shape), dt))
        return T(h, name)

    def dram(self, name, shape, dt, kind="Internal"):
        return T(self.nc.dram_tensor(name, list(shape), dt, kind=kind), name)

    def _wait(self, e, tok):
        if tok is None:
            return
        if len(tok) == 3:
            k, v, g = tok
            if g != self.gen[k]:
                return
        else:
            k, v = tok
        if self.seen[e].get(k, 0) >= v:
            return
        if k == e and e == "pe":
            return
        self.engs[e].wait_ge(self.sem[k], v)
        self.seen[e][k] = v
        self.ninst += 1
        if len(tok) == 3:
            self.pend[e].append(k)

    def _flush_pend(self, e, tok):
        if self.pend[e]:
            for k in self.pend[e]:
                self.cons[k].append(tok)
            self.pend[e] = []

    def _deps(self, e, reads, writes):
        for r in reads:
            self._wait(e, r.t.lw)
        for w in writes:
            self._wait(e, w.t.lw)
            for tok in w.t.rd:
                self._wait(e, tok)

    def _commit(self, tok, reads, writes):
        for r in reads:
            rd = r.t.rd
            rd.append(tok)
            if len(rd) > 48:
                best = {}
                for k, v in rd:
                    if best.get(k, 0) < v:
                        best[k] = v
                r.t.rd = list(best.items())
        for w in writes:
            w.t.lw = tok
            w.t.rd = []

    def op(self, e, fn, reads, writes):
        reads = [r for r in reads if isinstance(r, V)]
        self._deps(e, reads, writes)
        ins = fn(self.engs[e])
        self.cnt[e] += 1
        ins.then_inc(self.sem[e], 1)
        self._commit((e, self.cnt[e]), reads, writes)
        self._flush_pend(e, (e, self.cnt[e]))
        self.ninst += 1
        return ins

    def dma_sw(self, out, in_):
        k = "w%d" % self.sw_rr
        self.sw_rr += 1
        self.sem[k] = self.es0.enter_context(self.nc.semaphore("s_" + k))
        self.cnt[k] = 0
        self._deps("pool", [in_], [out])
        ins = self.nc.gpsimd.dma_start(out=out.a, in_=in_.a)
        self.cnt[k] = 16
        ins.then_inc(self.sem[k], 16)
        tok = (k, 16)
        self._commit(tok, [in_], [out])
        self.ninst += 1

    def dma(self, out, in_, q="sp"):
        if q == "pool":
            return self.dma_sw(out, in_)
        k = "d%d" % self.dma_rr
        self.dma_rr = (self.dma_rr + 1) % self.NDMA
        if self.cnt[k] > 0:
            self._wait(q, (k, self.cnt[k]))
        self._deps(q, [in_], [out])
        ins = self.engs[q].dma_start(out=out.a, in_=in_.a)
        self.cnt[k] += 16
        ins.then_inc(self.sem[k], 16)
        self._commit((k, self.cnt[k]), [in_], [out])
        self._flush_pend(q, (k, self.cnt[k]))
        self.ninst += 1

    def allgather(self, dst, src, groups):
        self._deps("pool", [src.ap], [dst.ap])
        ins = self.nc.gpsimd.collective_compute("AllGather", ALU.bypass, replica_groups=groups,
                                                ins=[src.h.ap()], outs=[dst.h.ap()])
        self.cnt["cc"] += 1
        ins.then_inc(self.sem["cc"], 1)
        self._commit(("cc", self.cnt["cc"]), [src.ap], [dst.ap])
        self.ninst += 1

    def barrier(self):
        for e in self.engs:
            for k, c in self.cnt.items():
                if c > 0:
                    self._wait(e, (k, c, self.gen[k]) if k in self.gen else (k, c))

    def finish(self, outs):
        for o in outs:
            self._wait("sp", o.trk.lw)
        self.barrier()

    def mm(self, out, lhsT, rhs, start=True, stop=True, **kw):
        return self.op("pe", lambda E: E.matmul(out.a, lhsT.a, rhs.a, start=start, stop=stop, **kw),
                       [lhsT, rhs] + ([] if start else [out]), [out])

    def tr(self, out, in_, ident):
        return self.op("pe", lambda E: E.transpose(out.a, in_.a, ident.a), [in_, ident], [out])

    def act(self, out, in_, func, bias=None, scale=None, accum=None):
        kw = {}
        rd = [in_]
        wr = [out]
        if bias is not None:
            kw["bias"] = bias.a if isinstance(bias, V) else bias
            rd.append(bias)
        if scale is not None:
            kw["scale"] = scale.a if isinstance(scale, V) else scale
            rd.append(scale)
        if accum is not None:
            kw["accum_out"] = accum.a
            wr.append(accum)
        return self.op("act", lambda E: E.activation(out.a, in_.a, func, **kw), rd, wr)

    def tt(self, out, a, b, op, e="dve"):
        return self.op(e, lambda E: E.tensor_tensor(out.a, a.a, b.a, op), [a, b], [out])

    def ts(self, out, a, s1, s2, op0, op1=None, e="dve"):
        g = lambda s: s.a if isinstance(s, V) else s
        if op1 is None:
            return self.op(e, lambda E: E.tensor_scalar(out.a, a.a, g(s1), None, op0), [a, s1], [out])
        return self.op(e, lambda E: E.tensor_scalar(out.a, a.a, g(s1), g(s2), op0, op1), [a, s1, s2], [out])

    def stt(self, out, a, s, b, op0, op1, e="dve"):
        g = lambda x: x.a if isinstance(x, V) else x
        return self.op(e, lambda E: E.scalar_tensor_tensor(out.a, a.a, g(s), b.a, op0, op1), [a, s, b], [out])

    def cp(self, out, in_, e="dve"):
        if e == "act":
            return self.op(e, lambda E: E.copy(out.a, in_.a), [in_], [out])
        return self.op(e, lambda E: E.tensor_copy(out.a, in_.a), [in_], [out])

    def red(self, out, in_, op, e="dve"):
        return self.op(e, lambda E: E.tensor_reduce(out.a, in_.a, AX.X, op), [in_], [out])

    def memset(self, out, val, e="dve"):
        return self.op(e, lambda E: E.memset(out.a, val), [], [out])

    def recip(self, out, in_):
        return self.op("dve", lambda E: E.reciprocal(out.a, in_.a), [in_], [out])


def _consts_common():
    c = np.arange(128)
    Fc = np.exp(-2j * np.pi * np.outer(c, c) / 128) / np.sqrt(128)
    FcCS = np.concatenate([Fc.real, Fc.imag], 1)
    F128 = np.exp(-2j * np.pi * np.outer(c, c) / 128)
    R_re = np.concatenate([F128.real, F128.imag], 1)
    R_im = np.concatenate([-F128.imag, F128.real], 1)
    q4 = lambda R: np.stack([np.concatenate([R[:, 32 * t:32 * t + 32], R[:, 128 + 32 * t:160 + 32 * t]], 1)
                             for t in range(4)], 1)
    n2 = np.arange(64)
    Tw = np.exp(-2j * np.pi * np.outer(n2, c) / 8192)
    n = np.arange(256)
    F256 = np.exp(-2j * np.pi * np.outer(n, n) / 256) / 16.0
    t256 = lambda M: M.reshape(2, 128, 256).transpose(1, 0, 2)
    rot128 = np.zeros((128, 128))
    for i in range(64):
        rot128[2 * i, 2 * i + 1] = 1.0
        rot128[2 * i + 1, 2 * i] = -1.0
    sel = np.zeros((32, 32, 128))
    for e in range(32):
        sel[e, e, :] = 1.0
    d = {"c_ident": np.eye(128), "c_fccs": FcCS, "c_rqre": q4(R_re), "c_rqim": q4(R_im),
         "c_twc": Tw.real, "c_tws": Tw.imag, "c_c256": t256(F256.real), "c_s256": t256(-F256.imag),
         "c_rot": rot128, "c_sel": sel}
    return {k: np.ascontiguousarray(v, dtype=np.float32) for k, v in d.items()}


def _consts_core(q):
    n2 = np.arange(64)
    F64 = np.exp(-2j * np.pi * np.outer(n2, np.arange(64)) / 64) / np.sqrt(8192.0)
    sl = slice(16 * q, 16 * q + 16)
    n = 2048 * q + np.arange(2048)
    row = (n // 64).astype(np.float64)
    col = (n % 64).astype(np.float64)

    def tables(dim):
        nf = dim // 4
        inv = 10000.0 ** (-np.arange(nf, dtype=np.float64) / nf)
        ang = np.concatenate([row[:, None] * inv[None, :], col[:, None] * inv[None, :]], -1)
        ang = np.repeat(ang, 2, axis=1).T
        return np.cos(ang), np.sin(ang)
    c128, s128 = tables(128)
    c64, s64 = tables(64)
    hm = np.zeros((128, 8))
    if q > 0:
        hm[:, q - 1] = 1.0
    if q < 3:
        hm[:, 4 + q + 1] = 1.0
    d = {"k_f64c": F64.real[:, sl], "k_f64s": -F64.imag[:, sl], "k_rc128": c128, "k_rs128": s128,
         "k_rc64": c64, "k_rs64": s64, "k_hmask": hm}
    return {k: np.ascontiguousarray(v, dtype=np.float32) for k, v in d.items()}


def build(n_layers=4, final=True, l0=0):
    nc = bass.Bass("TRN2", target_bir_lowering=False)
    groups = [[0, 1, 2, 3], [4, 5, 6, 7]]
    with ExitStack() as es:
        S = Sched(nc, es)
        din = lambda name, shape: S.dram(name, shape, F32, "ExternalInput")
        x0 = din("x0", [128, 8, NT])
        scT = din("scT", [128, 8, 2])
        mod_w = [din("mod_w%d" % l, [1024, 6144]) for l in range(n_layers)]
        mod_bT = din("mod_bT", [4, 128, 48])
        gmixT = din("gmixT", [4, 128, 8])
        gffnT = din("gffnT", [4, 128, 8])
        gfinT = din("gfinT", [128, 8])
        w_in_ab = din("w_in_ab", [2, 1024, 1536])
        conv_wT = din("conv_wT", [2, 128, 4, 31])
        conv_bT = din("conv_bT", [2, 128, 4])
        conv_gT = din("conv_gT", [2, 128, 4])
        w_out_ab = din("w_out_ab", [2, 1024, 1024])
        w_in_cd = din("w_in_cd", [2, 1024, 1728])
        q_gT = din("q_gT", [2, 128, 1])
        k_gT = din("k_gT", [2, 128, 1])
        cq_gT = din("cq_gT", [2, 128, 3])
        ckv_gT = din("ckv_gT", [2, 128, 2])
        w_uq = din("w_uq", [2, 384, 768])
        w_ukv = din("w_ukv", [2, 256, 1024])
        w_out_cd = din("w_out_cd", [2, 1024, 1024])
        rw = din("rw", [4, 1024, 36])
        rb = din("rb", [4, 1, 36])
        ewg = [din("ewg%d" % l, [32, 1024, 512]) for l in range(n_layers)]
        ewu = [din("ewu%d" % l, [32, 1024, 512]) for l in range(n_layers)]
        ewd = [din("ewd%d" % l, [32, 512, 1024]) for l in range(n_layers)]
        cin = {}
        for nm, shp in [("c_ident", [128, 128]), ("c_fccs", [128, 256]), ("c_rqre", [128, 4, 64]),
                        ("c_rqim", [128, 4, 64]), ("c_twc", [64, 128]), ("c_tws", [64, 128]),
                        ("c_c256", [128, 2, 256]), ("c_s256", [128, 2, 256]), ("c_rot", [128, 128]),
                        ("c_sel", [32, 32, 128]), ("k_f64c", [64, 16]), ("k_f64s", [64, 16]),
                        ("k_rc128", [128, 2048]), ("k_rs128", [128, 2048]), ("k_rc64", [64, 2048]),
                        ("k_rs64", [64, 2048]), ("k_hmask", [128, 8])]:
            cin[nm] = din(nm, shp)
        y = S.dram("y", [128, 8, TL], F32, "ExternalOutput")
        pa_src = [S.dram("pa_src%d" % i, [128, 2 * TL], BF16) for i in range(2)]
        pa_dst = [S.dram("pa_dst%d" % i, [512, 2 * TL], BF16) for i in range(2)]
        halo_src = S.dram("halo_src", [128, 120], BF16)
        halo_dst = S.dram("halo_dst", [512, 120], BF16)
        XC = 26624
        KVW = [4096] * 6 + [2048]
        kv_src = [S.dram("kv_src%d" % i, [128, KVW[i]], BF16) for i in range(7)]
        kv_dst = [S.dram("kv_dst%d" % i, [512, KVW[i]], BF16) for i in range(7)]

        def kvs_(c0, c1):
            pi = c0 // 4096
            assert (c1 - 1) // 4096 == pi
            return kv_src[pi][:, c0 - 4096 * pi:c1 - 4096 * pi]

        def kvd_(c0, c1, rows=128):
            pi = c0 // 4096
            assert (c1 - 1) // 4096 == pi
            return kv_dst[pi].ap.re("(r p) c -> p r c", p=128)[0:rows, :, c0 - 4096 * pi:c1 - 4096 * pi]
        wt_scr = S.dram("wt_scr", [32, NT], F32)
        q_scr = S.dram("q_scr", [128, 12, NT], BF16)

        xT = S.sb("xT", [128, 8, NT], F32)
        PS = [S.ps("ps%d" % i, [128, 512], F32) for i in range(8)]
        ident = S.sb("ident", [128, 128], F32)
        ident_bf = S.sb("ident_bf", [128, 128], BF16)
        ones_bf = S.sb("ones_bf", [128, 128], BF16)
        eps_t = S.sb("eps_t", [128, 1], F32)
        sc_t = S.sb("sc_t", [128, 8, 2], F32)
        modv = S.sb("modv", [128, 48, 2], F32)
        prm = S.sb("prm", [128, 6, 8, 2], F32)
        gtmp = S.sb("gtmp", [128, 16], F32)
        S.dma(ident.ap, cin["c_ident"].ap)
        S.dma(ident_bf.ap, cin["c_ident"].ap, q="pool")
        S.memset(ones_bf.ap, 1.0)
        S.memset(eps_t.ap, EPS)
        S.dma(sc_t.ap, scT.ap)
        S.act(sc_t.ap, sc_t.ap, AF.Silu)
        for c in range(8):
            S.dma(xT[:, c, :], x0[:, c, :], q=("sp" if c % 2 == 0 else "act"))
        S.barrier()

        def xv(c, ci):
            t0, w = CH[ci]
            return xT.sv((c, ci), (slice(None), c, slice(t0, t0 + w)))

        def mod_phase(l):
            with ExitStack() as ph:
                wbuf = [S.sb("modw%d" % i, [128, 3072], F32, es=ph) for i in range(2)]
                mb = S.sb("modb", [128, 48], F32, es=ph)
                gm = S.sb("gm", [128, 16], F32, es=ph)
                S.dma(mb.ap, mod_bT[l])
                S.dma(gm[:, 0:8], gmixT[l])
                S.dma(gm[:, 8:16], gffnT[l])
                mps = PS[7]
                first = True
                i = 0
                for kc in range(8):
                    for hf in range(2):
                        wb = wbuf[i % 2]
                        i += 1
                        S.dma(wb.ap, mod_w[l][kc * 128:(kc + 1) * 128, hf * 3072:(hf + 1) * 3072],
                              q=("sp" if i % 2 else "act"))
                        for j in range(24):
                            jj = hf * 24 + j
                            S.mm(mps[:, 2 * jj:2 * jj + 2], wb[:, j * 128:(j + 1) * 128], sc_t[:, kc, :],
                                 start=first, stop=(kc == 7 and jj == 47), skip_group_check=True)
                            first = False
                S.tt(modv.ap, mps[:, 0:96].re("p (j s) -> p j s", s=2),
                     mb.ap.re("p (j o) -> p j o", o=1).bc([128, 48, 2]), ALU.add)
                for half, (ish, isc, ig) in enumerate([(0, 1, 2), (3, 4, 5)]):
                    gv = gm[:, 8 * half:8 * half + 8].re("p (c o) -> p c o", o=1).bc([128, 8, 2])
                    A = prm[:, 3 * half + 0]
                    S.ts(A, modv[:, 8 * isc:8 * isc + 8, :], 1.0, None, ALU.add)
                    S.tt(A, A, gv, ALU.mult)
                    S.cp(prm[:, 3 * half + 1], modv[:, 8 * ish:8 * ish + 8, :])
                    S.cp(prm[:, 3 * half + 2], modv[:, 8 * ig:8 * ig + 8, :])
                S.barrier()

        def norm_mod(ph, A, B, out_fn, name):
            sq = [S.sb(name + "sq%d" % i, [128, 512], BF16, es=ph) for i in range(2)]
            rs = S.sb(name + "rs", [128, 512], F32, es=ph)
            tmp = [S.sb(name + "tmp%d" % i, [128, 512], F32, es=ph) for i in range(2)]
            for ci, (t0, w) in enumerate(CH):
                st = 0 if t0 < TL else 1
                ss = PS[6]
                for c in range(8):
                    s_ = sq[c % 2]
                    S.act(s_[:, :w], xv(c, ci), AF.Square)
                    S.mm(ss[:, :w], ones_bf.ap, s_[:, :w], start=(c == 0), stop=(c == 7))
                S.act(rs[:, :w], ss[:, :w], AF.Sqrt, bias=eps_t.ap, scale=1.0 / 1024)
                S.recip(rs[:, :w], rs[:, :w])
                for c in range(8):
                    t_ = tmp[c % 2]
                    S.tt(t_[:, :w], xv(c, ci), rs[:, :w], ALU.mult, e="pool")
                    if B is None:
                        S.ts(out_fn(ci, c), t_[:, :w], A[:, c:c + 1], None, ALU.mult)
                    else:
                        S.ts(out_fn(ci, c), t_[:, :w], A[:, c, st:st + 1], B[:, c, st:st + 1], ALU.mult, ALU.add)
                yield ci

        def load_w(ph, name, src_view, shape, q="pool"):
            t = S.sb(name, shape, BF16, es=ph)
            S.dma(t.ap, src_view, q=q)
            return t

        def moe_phase(l):
            with ExitStack() as ph:
                h2T = S.sb("h2T", [128, 8, NT], BF16, es=ph)
                with ExitStack() as ph2:
                    WT = S.sb("WT", [32, NT], F32, es=ph2)
                    h2f = S.sb("h2f", [128, 8, 512], F32, es=ph2)
                    rwt = S.sb("rwt", [128, 8, 36], F32, es=ph2)
                    rbt = S.sb("rbt", [128, 36], F32, es=ph2)
                    S.dma(rwt.ap, rw[l].re("(kc p) n -> p kc n", p=128))
                    S.dma(rbt.ap, rb[l].bc([128, 36]))
                    sm = S.sb("sm", [128, 160], F32, es=ph2)
                    for ci in norm_mod(ph2, prm[:, 3], prm[:, 4], lambda ci, c: h2f[:, c, :CH[ci][1]], "n2"):
                        t0, w = CH[ci]
                        for c in range(8):
                            S.cp(h2T[:, c, t0:t0 + w], h2f[:, c, :w], e="act")
                        for ti in range(w // 128):
                            lg_ps = PS[5]
                            for kc in range(8):
                                S.mm(lg_ps[:, 0:36], h2f[:, kc, ti * 128:(ti + 1) * 128], rwt[:, kc, :],
                                     start=(kc == 0), stop=(kc == 7))
                            lg = sm[:, 0:36]
                            S.tt(lg, lg_ps[:, 0:36], rbt.ap, ALU.add)
                            gmax = sm[:, 36:37]
                            S.red(gmax, sm[:, 0:4], ALU.max)
                            goh = sm[:, 40:44]
                            S.ts(goh, sm[:, 0:4], gmax, None, ALU.is_equal)
                            ngm = sm[:, 37:38]
                            S.ts(ngm, gmax, -1.0, None, ALU.mult)
                            gsum = sm[:, 38:39]
                            S.memset(gsum, 0.0)
                            S.act(sm[:, 44:48], sm[:, 0:4], AF.Exp, bias=ngm, scale=1.0, accum=gsum)
                            gprob = sm[:, 39:40]
                            S.recip(gprob, gsum)
                            em = sm[:, 48:80]
                            S.tt(em.re("p (g e) -> p g e", g=4), sm[:, 4:36].re("p (g e) -> p g e", g=4),
                                 goh.re("p (g o) -> p g o", o=1).bc([128, 4, 8]), ALU.mult)
                            esel = sm[:, 80:88]
                            S.red(esel, em.re("p (g e) -> p e g", g=4), ALU.add)
                            m1 = sm[:, 88:89]
                            S.red(m1, esel, ALU.max)
                            oh1 = sm[:, 96:104]
                            S.ts(oh1, esel, m1, None, ALU.is_equal)
                            es2 = sm[:, 104:112]
                            S.stt(es2, oh1, -1e30, esel, ALU.mult, ALU.add)
                            m2 = sm[:, 89:90]
                            S.red(m2, es2, ALU.max)
                            oh2 = sm[:, 112:120]
                            S.ts(oh2, es2, m2, None, ALU.is_equal)
                            dd = sm[:, 90:91]
                            S.tt(dd, m2, m1, ALU.subtract)
                            ee = sm[:, 91:92]
                            S.act(ee, dd, AF.Exp)
                            S.ts(ee, ee, 1.0, None, ALU.add)
                            w1 = sm[:, 92:93]
                            S.recip(w1, ee)
                            S.tt(w1, w1, gprob, ALU.mult)
                            w2 = sm[:, 93:94]
                            S.tt(w2, gprob, w1, ALU.subtract)
                            wsel = sm[:, 120:128]
                            S.ts(wsel, oh1, w1, None, ALU.mult)
                            S.stt(wsel, oh2, w2, wsel, ALU.mult, ALU.add)
                            wf = sm[:, 128:160]
                            S.tt(wf.re("p (g e) -> p g e", g=4), goh.re("p (g o) -> p g o", o=1).bc([128, 4, 8]),
                                 wsel.re("p (o e) -> p o e", o=1).bc([128, 4, 8]), ALU.mult)
                            tp = PS[4]
                            S.tr(tp[0:32, 0:128], wf, ident.ap)
                            S.cp(WT[:, t0 + ti * 128:t0 + (ti + 1) * 128], tp[0:32, 0:128], e="act")
                    S.dma(wt_scr.ap, WT.ap)
                    S.barrier()
                wg = [S.sb("wg%d" % i, [128, 8, 512], BF16, es=ph) for i in range(2)]
                wu = [S.sb("wu%d" % i, [128, 8, 512], BF16, es=ph) for i in range(2)]
                wd = [S.sb("wd%d" % i, [128, 4, 1024], BF16, es=ph) for i in range(2)]
                stg = [S.sb("stg%d" % i, [128, 2048], F32, es=ph) for i in range(2)]
                wbs = [S.sb("wbs%d" % i, [128, 512], F32, es=ph) for i in range(2)]
                sg = [S.sb("sg%d" % i, [128, 512], F32, es=ph) for i in range(2)]
                tt_ = [S.sb("tt%d" % i, [128, 512], F32, es=ph) for i in range(2)]
                hh = [S.sb("hh%d" % i, [128, 4, 512], BF16, es=ph) for i in range(2)]
                ucnt = [0]

                def unit(e, u):
                    b = e % 2
                    if u < 2:
                        return (ewg[l][e].re("(kc p) n -> p kc n", p=128)[:, 4 * u:4 * u + 4, :],
                                wg[b][:, 4 * u:4 * u + 4, :], 4)
                    if u < 4:
                        return (ewu[l][e].re("(kc p) n -> p kc n", p=128)[:, 4 * (u - 2):4 * (u - 2) + 4, :],
                                wu[b][:, 4 * (u - 2):4 * (u - 2) + 4, :], 4)
                    return (ewd[l][e].re("(kc p) n -> p kc n", p=128)[:, 2 * (u - 4):2 * (u - 4) + 2, :],
                            wd[b][:, 2 * (u - 4):2 * (u - 4) + 2, :], 2)
                ucnt[0] = 0
                for u in range(6):
                    src, dst, kc = unit(0, u)
                    st_ = stg[u % 2]
                    S.dma(st_.ap.re("p (kc n) -> p kc n", kc=kc), src, q="sp")
                    S.cp(dst, st_.ap.re("p (kc n) -> p kc n", kc=kc), e=("act" if u % 2 == 0 else "pool"))
                it = 0
                import os as _os
                NEXP = int(_os.environ.get("DEV_NEXP", "32"))
                for e in range(NEXP):
                    nxt = e + 1 < NEXP
                    b = e % 2

                    def dma_u(u):
                        src, dst, kc = unit(e + 1, u)
                        S.dma(stg[u % 2].ap.re("p (kc n) -> p kc n", kc=kc), src, q="sp")

                    def cast_u(u):
                        src, dst, kc = unit(e + 1, u)
                        S.cp(dst, stg[u % 2].ap.re("p (kc n) -> p kc n", kc=kc), e=("act" if u % 2 == 0 else "pool"))
                    if nxt:
                        dma_u(0)
                        dma_u(1)
                    for ci, (t0, w) in enumerate(CH):
                        st = 0 if t0 < TL else 1
                        it += 1
                        wb = wbs[it % 2]
                        S.dma(wb[:, :w], wt_scr[e:e + 1, t0:t0 + w].bc([128, w]))
                        hb = hh[it % 2]
                        for j in range(4):
                            g_ps = PS[j % 2]
                            u_ps = PS[2 + j % 2]
                            for kc in range(8):
                                S.mm(g_ps[:, :w], wg[b][:, kc, j * 128:(j + 1) * 128], h2T[:, kc, t0:t0 + w],
                                     start=(kc == 0), stop=(kc == 7))
                            for kc in range(8):
                                S.mm(u_ps[:, :w], wu[b][:, kc, j * 128:(j + 1) * 128], h2T[:, kc, t0:t0 + w],
                                     start=(kc == 0), stop=(kc == 7))
                            s_ = sg[j % 2]
                            S.act(s_[:, :w], g_ps[:, :w], AF.Silu)
                            t_ = tt_[j % 2]
                            S.tt(t_[:, :w], u_ps[:, :w], s_[:, :w], ALU.mult)
                            S.tt(hb[:, j, :w], t_[:, :w], wb[:, :w], ALU.mult, e="pool")
                        for oc in range(8):
                            d_ps = PS[4 + oc % 2]
                            for j in range(4):
                                S.mm(d_ps[:, :w], wd[b][:, j, oc * 128:(oc + 1) * 128], hb[:, j, :w],
                                     start=(j == 0), stop=(j == 3))
                            S.stt(xv(oc, ci), d_ps[:, :w], prm[:, 5, oc, st:st + 1], xv(oc, ci), ALU.mult, ALU.add)
                        if nxt:
                            cast_u(ci)
                            if ci + 2 < 6:
                                dma_u(ci + 2)
                            if ci == 4:
                                cast_u(5)
                S.barrier()

        def even_phase(l):
            j = l // 2
            with ExitStack() as ph:
                yac = S.sb("yac", [128, 4, TCX], BF16, es=ph)
                ybT = S.sb("ybT", [128, 4, NT], BF16, es=ph)
                with ExitStack() as pu_:
                    uext = S.sb("uext", [128, 4, TL + 30], BF16, es=pu_)
                    ucx = S.sb("ucx", [128, 4, TCX + 30], BF16, es=pu_)
                    S.memset(ucx.ap, 0.0, e="pool")
                    with ExitStack() as ph2:
                        paT = S.sb("paT", [128, 4, NT], BF16, es=ph2)
                        hTc = [S.sb("hTc%d" % i, [128, 8, 512], BF16, es=ph2) for i in range(2)]
                        wab = load_w(ph2, "wab", w_in_ab[j].re("(kc p) n -> p kc n", p=128), [128, 8, 1536])
                        sgt = [S.sb("sgt%d" % i, [128, 512], F32, es=ph2) for i in range(2)]
                        fccs = load_w(ph2, "fccs", cin["c_fccs"].ap, [128, 256])
                        c256 = load_w(ph2, "c256", cin["c_c256"].ap, [128, 2, 256])
                        s256 = load_w(ph2, "s256", cin["c_s256"].ap, [128, 2, 256])
                        zc = S.sb("zc", [128, 2, 256], BF16, es=ph2)
                        for ci in norm_mod(ph2, prm[:, 0], prm[:, 1],
                                           lambda ci, c: hTc[ci % 2][:, c, :CH[ci][1]], "n1"):
                            t0, w = CH[ci]
                            hT = hTc[ci % 2]
                            for g in range(4):
                                p = PS[g % 2]
                                for kc in range(8):
                                    S.mm(p[:, :w], wab[:, kc, g * 128:(g + 1) * 128], hT[:, kc, :w],
                                         start=(kc == 0), stop=(kc == 7))
                                S.cp(paT[:, g, t0:t0 + w], p[:, :w], e="act")
                            for cc in range(4):
                                pu = PS[2 + cc % 2]
                                pg = PS[4 + cc % 2]
                                for kc in range(8):
                                    S.mm(pu[:, :w], wab[:, kc, 512 + cc * 128:512 + (cc + 1) * 128], hT[:, kc, :w],
                                         start=(kc == 0), stop=(kc == 7))
                                for kc in range(8):
                                    S.mm(pg[:, :w], wab[:, kc, 1024 + cc * 128:1024 + (cc + 1) * 128], hT[:, kc, :w],
                                         start=(kc == 0), stop=(kc == 7))
                                s_ = sgt[cc % 2]
                                S.act(s_[:, :w], pg[:, :w], AF.Sigmoid)
                                dst = uext[:, cc, 15 + t0:15 + t0 + w] if t0 < TL else ucx[:, cc, 15:15 + TCX]
                                S.tt(dst, pu[:, :w], s_[:, :w], ALU.mult)
                        for i in range(2):
                            S.dma(pa_src[i].ap.re("p (g t) -> p g t", g=2), paT[:, 2 * i:2 * i + 2, 0:TL])
                            S.allgather(pa_dst[i], pa_src[i], groups)
                        for g in range(4):
                            for i in range(2):
                                p = PS[i]
                                S.mm(p[:, 0:256], paT[:, g, TL + 128 * i:TL + 128 * (i + 1)], fccs.ap)
                                S.cp(zc[:, i, :], p[:, 0:256], e="act")
                            p = PS[2 + g % 2]
                            for i in range(2):
                                S.mm(p[:, 0:256], zc[:, i, 0:128], c256[:, i, :], start=(i == 0), stop=False)
                                S.mm(p[:, 0:256], zc[:, i, 128:256], s256[:, i, :], start=False, stop=(i == 1))
                            S.cp(yac[:, g, :], p[:, 0:256])
                        S.barrier()
                    with ExitStack() as ph2:
                        hs = S.sb("hs", [128, 4, 2, 15], BF16, es=ph2)
                        S.cp(hs[:, :, 0, :], uext[:, :, 15:30], e="pool")
                        S.cp(hs[:, :, 1, :], uext[:, :, TL:TL + 15], e="pool")
                        S.dma(halo_src.ap.re("p (c s k) -> p c s k", c=4, s=2), hs.ap)
                        S.allgather(halo_dst, halo_src, groups)
                        hall = S.sb("hall", [128, 4, 4, 2, 15], BF16, es=ph2)
                        S.dma(hall.ap, halo_dst.ap.re("(r p) (c s k) -> p r c s k", p=128, c=4, s=2))
                        hm = S.sb("hm", [128, 8], F32, es=ph2)
                        S.dma(hm.ap, cin["k_hmask"].ap)
                        hl = S.sb("hl", [128, 4, 15], F32, es=ph2)
                        hr = S.sb("hr", [128, 4, 15], F32, es=ph2)
                        S.memset(hl.ap, 0.0)
                        S.memset(hr.ap, 0.0)
                        for r in range(4):
                            S.stt(hl.ap, hall[:, r, :, 1, :], hm[:, r:r + 1], hl.ap, ALU.mult, ALU.add)
                            S.stt(hr.ap, hall[:, r, :, 0, :], hm[:, 4 + r:5 + r], hr.ap, ALU.mult, ALU.add)
                        S.cp(uext[:, :, 0:15], hl.ap)
                        S.cp(uext[:, :, TL + 15:TL + 30], hr.ap)
                        cw = S.sb("cw", [128, 4, 31], F32, es=ph2)
                        cb = S.sb("cb", [128, 4], F32, es=ph2)
                        cg = S.sb("cg", [128, 4], F32, es=ph2)
                        S.dma(cw.ap, conv_wT[j])
                        S.dma(cb.ap, conv_bT[j])
                        S.dma(cg.ap, conv_gT[j])
                        dg = [S.sb("dg%d" % cc, [128, 31, 128], BF16, es=ph2) for cc in range(4)]
                        for cc in range(4):
                            for k in range(31):
                                S.ts(dg[cc][:, k, :], ident.ap, cw[:, cc, k:k + 1], None, ALU.mult,
                                     e=("dve" if k % 2 else "pool"))
                        vb = [S.sb("vb%d" % cc, [128, 512], F32, es=ph2) for cc in range(4)]
                        sqb = [S.sb("sqb%d" % i, [128, 512], BF16, es=ph2) for i in range(2)]
                        rsb = S.sb("rsb", [128, 512], F32, es=ph2)
                        for ci, (t0, w) in enumerate(CH):
                            ss = PS[6]
                            for cc in range(4):
                                p = PS[cc % 2]
                                for k in range(31):
                                    src = uext[:, cc, t0 + k:t0 + k + w] if t0 < TL else ucx[:, cc, k:k + w]
                                    S.mm(p[:, :w], dg[cc][:, k, :], src, start=(k == 0), stop=(k == 30))
                                S.act(vb[cc][:, :w], p[:, :w], AF.Identity, bias=cb[:, cc:cc + 1], scale=1.0)
                                s_ = sqb[cc % 2]
                                S.tt(s_[:, :w], vb[cc][:, :w], vb[cc][:, :w], ALU.mult, e="pool")
                                S.mm(ss[:, :w], ones_bf.ap, s_[:, :w], start=(cc == 0), stop=(cc == 3))
                            S.act(rsb[:, :w], ss[:, :w], AF.Sqrt, bias=eps_t.ap, scale=1.0 / 512)
                            S.recip(rsb[:, :w], rsb[:, :w])
                            for cc in range(4):
                                S.tt(vb[cc][:, :w], vb[cc][:, :w], rsb[:, :w], ALU.mult)
                                S.act(ybT[:, cc, t0:t0 + w], vb[cc][:, :w], AF.Silu, scale=cg[:, cc:cc + 1])
                        S.barrier()
                yal = S.sb("yal", [128, 4, TL], BF16, es=ph)
                with ExitStack() as ph2:
                    fccs = load_w(ph2, "fccs", cin["c_fccs"].ap, [128, 256])
                    rqre = load_w(ph2, "rqre", cin["c_rqre"].ap, [128, 4, 64])
                    rqim = load_w(ph2, "rqim", cin["c_rqim"].ap, [128, 4, 64])
                    f64c = load_w(ph2, "f64c", cin["k_f64c"].ap, [64, 16])
                    f64s = load_w(ph2, "f64s", cin["k_f64s"].ap, [64, 16])
                    twc = S.sb("twc", [64, 128], F32, es=ph2)
                    tws = S.sb("tws", [64, 128], F32, es=ph2)
                    S.dma(twc.ap, cin["c_twc"].ap)
                    S.dma(tws.ap, cin["c_tws"].ap)
                    paf = S.sb("paf", [128, 4 * TL], BF16, es=ph2)
                    Z = S.sb("Z", [128, 64, 256], BF16, es=ph2)
                    Vq = S.sb("Vq", [64, 128, 2, 32], BF16, es=ph2)
                    t4 = [S.sb("t4_%d" % i, [64, 8, 32], F32, es=ph2) for i in range(4)]
                    for g in range(4):
                        S.dma(paf.ap.re("p (r t) -> p r t", r=4),
                              pa_dst[g // 2].ap.re("(r p) (g t) -> p r g t", p=128, g=2)[:, :, g % 2, :])
                        for i in range(32):
                            p = PS[i % 2]
                            for h in range(2):
                                n2 = 2 * i + h
                                S.mm(p[:, 256 * h:256 * (h + 1)], paf[:, n2:4 * TL:64], fccs.ap)
                            S.cp(Z[:, 2 * i:2 * i + 2, :].re("p a b -> p (a b)"), p.ap,
                                 e=("act" if i % 2 else "dve"))
                        for qt in range(4):
                            for kb in range(16):
                                p = PS[2 + kb % 2]
                                for k8 in range(8):
                                    kc = kb * 8 + k8
                                    S.mm(p[0:64, 64 * k8:64 * (k8 + 1)], Z[:, :, kc], rqre[:, qt, :],
                                         start=True, stop=False)
                                    S.mm(p[0:64, 64 * k8:64 * (k8 + 1)], Z[:, :, 128 + kc], rqim[:, qt, :],
                                         start=False, stop=True)
                                U = p[0:64, :].re("p (k r a) -> p k r a", k=8, r=2)
                                tcv = twc[:, 32 * qt:32 * qt + 32].re("p (o a) -> p o a", o=1).bc([64, 8, 32])
                                tsv = tws[:, 32 * qt:32 * qt + 32].re("p (o a) -> p o a", o=1).bc([64, 8, 32])
                                S.tt(t4[0].ap, U[:, :, 0, :], tcv, ALU.mult)
                                S.tt(t4[1].ap, U[:, :, 1, :], tsv, ALU.mult)
                                S.tt(t4[2].ap, U[:, :, 0, :], tsv, ALU.mult)
                                S.tt(t4[3].ap, U[:, :, 1, :], tcv, ALU.mult)
                                S.tt(Vq[:, kb * 8:kb * 8 + 8, 0, :], t4[0].ap, t4[1].ap, ALU.subtract, e="pool")
                                S.tt(Vq[:, kb * 8:kb * 8 + 8, 1, :], t4[2].ap, t4[3].ap, ALU.add, e="pool")
                            p = PS[4 + qt % 2]
                            for a in range(32):
                                S.mm(p[:, 16 * a:16 * (a + 1)], Vq[:, :, 0, a], f64c.ap, start=True, stop=False)
                                S.mm(p[:, 16 * a:16 * (a + 1)], Vq[:, :, 1, a], f64s.ap, start=False, stop=True)
                            dst = yal[:, g, :].re("p (jj k) -> p k jj", k=128)[:, 32 * qt:32 * qt + 32, :]
                            S.cp(dst, p.ap.re("p (a jj) -> p a jj", a=32), e="act")
                    S.barrier()
                with ExitStack() as ph2:
                    wo = load_w(ph2, "wo", w_out_ab[j].re("(kc p) n -> p kc n", p=128), [128, 8, 1024])
                    for ci, (t0, w) in enumerate(CH):
                        st = 0 if t0 < TL else 1
                        for oc in range(8):
                            p = PS[oc % 4]
                            for kc in range(8):
                                if kc < 4:
                                    rhs = yal[:, kc, t0:t0 + w] if t0 < TL else yac[:, kc, :]
                                else:
                                    rhs = ybT[:, kc - 4, t0:t0 + w]
                                S.mm(p[:, :w], wo[:, kc, oc * 128:(oc + 1) * 128], rhs,
                                     start=(kc == 0), stop=(kc == 7))
                            S.stt(xv(oc, ci), p[:, :w], prm[:, 2, oc, st:st + 1], xv(oc, ci), ALU.mult, ALU.add)
                    S.barrier()

        def odd_phase(l):
            j = l // 2
            OK_, OKD, OV, OVD, OKR = 0, 4096, 12288, 16384, 24576
            with ExitStack() as ph:
                kctx = S.sb("kctx", [128, 2, TCX], BF16, es=ph)
                kdctx = S.sb("kdctx", [128, 4, TCX], BF16, es=ph)
                krctx = S.sb("krctx", [64, TCX], BF16, es=ph)
                vctx = S.sb("vctx", [128, 2, 256], BF16, es=ph)
                vdctx = S.sb("vdctx", [128, 2, 512], BF16, es=ph)
                with ExitStack() as ph2:
                    hTc = [S.sb("hTc%d" % i, [128, 8, 512], BF16, es=ph2) for i in range(2)]
                    wcd = load_w(ph2, "wcd", w_in_cd[j].re("(kc p) n -> p kc n", p=128), [128, 8, 1728])
                    wuq = load_w(ph2, "wuq", w_uq[j].re("(kc p) n -> p kc n", p=128), [128, 3, 768])
                    wukv = load_w(ph2, "wukv", w_ukv[j].re("(kc p) n -> p kc n", p=128), [128, 2, 1024])
                    wukvv = S.sb("wukvv", [128, 2, 4, 128], BF16, es=ph2)
                    for kc_ in range(2):
                        S.dma(wukvv[:, kc_], w_ukv[j].re("(kc p) (h two d) -> p kc h two d", p=128, h=4, two=2)[:, kc_, :, 1, :],
                              q="pool")
                    rot = load_w(ph2, "rot", cin["c_rot"].ap, [128, 128])
                    rc128 = S.sb("rc128", [128, 512], F32, es=ph2)
                    rs128 = S.sb("rs128", [128, 512], F32, es=ph2)
                    rc64 = S.sb("rc64", [64, 512], F32, es=ph2)
                    rs64 = S.sb("rs64", [64, 512], F32, es=ph2)
                    gq = S.sb("gq", [128, 8], F32, es=ph2)
                    S.dma(gq[:, 0:1], q_gT[j])
                    S.dma(gq[:, 1:2], k_gT[j])
                    S.dma(gq[:, 2:5], cq_gT[j])
                    S.dma(gq[:, 5:7], ckv_gT[j])
                    kst = S.sb("kst", [128, 6656], BF16, es=ph2)
                    LK, LKD, LKR, LV, LVD = 0, 1024, 3072, 3584, 4608
                    qst = S.sb("qst", [128, 12, 512], BF16, es=ph2)
                    sqb = [S.sb("sqb%d" % i, [128, 512], BF16, es=ph2) for i in range(2)]
                    rsb = S.sb("rsb", [128, 512], F32, es=ph2)
                    qn = [S.sb("qn%d" % i, [128, 512], BF16, es=ph2) for i in range(2)]
                    f1 = [S.sb("f1_%d" % i, [128, 512], F32, es=ph2) for i in range(2)]
                    f2 = [S.sb("f2_%d" % i, [128, 512], F32, es=ph2) for i in range(2)]
                    craw = S.sb("craw", [128, 3, 512], F32, es=ph2)
                    cn = S.sb("cn", [128, 3, 512], BF16, es=ph2)
                    kn = S.sb("kn", [128, 2, 512], BF16, es=ph2)
                    S.memset(qst.ap, 0.0, e="pool")
                    S.memset(kst.ap, 0.0, e="pool")
                    cnt = [0]

                    def rms_rope(p, w, t0, np_, gcol, dst, rope, inv_dim):
                        cnt[0] += 1
                        i = cnt[0] % 2
                        if gcol is not None:
                            S.act(sqb[i][:np_, :w], p, AF.Square)
                            ss = PS[6]
                            S.mm(ss[:np_, :w], ones_bf[:np_, :np_], sqb[i][:np_, :w])
                            S.act(rsb[:np_, :w], ss[:np_, :w], AF.Sqrt, bias=eps_t[:np_, :], scale=inv_dim)
                            S.recip(rsb[:np_, :w], rsb[:np_, :w])
                            S.tt(f1[i][:np_, :w], p, rsb[:np_, :w], ALU.mult)
                            tgt = qn[i][:np_, :w] if rope else dst
                            S.ts(tgt, f1[i][:np_, :w], gq[:np_, gcol:gcol + 1], None, ALU.mult)
                        else:
                            tgt = qn[i][:np_, :w] if rope else dst
                            S.cp(tgt, p, e="act")
                        if rope:
                            rp = PS[7]
                            S.mm(rp[:np_, :w], rot[:np_, :np_], qn[i][:np_, :w])
                            rc, rs_ = (rc128, rs128) if np_ == 128 else (rc64, rs64)
                            S.tt(f1[i][:np_, :w], qn[i][:np_, :w], rc[:np_, :w], ALU.mult, e="pool")
                            S.tt(f2[i][:np_, :w], rp[:np_, :w], rs_[:np_, :w], ALU.mult)
                            S.tt(dst, f1[i][:np_, :w], f2[i][:np_, :w], ALU.add, e="pool")

                    for ci in norm_mod(ph2, prm[:, 0], prm[:, 1],
                                       lambda ci, c: hTc[ci % 2][:, c, :CH[ci][1]], "n1"):
                        t0, w = CH[ci]
                        lat = t0 < TL
                        hT = hTc[ci % 2]
                        if lat:
                            S.dma(rc128.ap, cin["k_rc128"][:, t0:t0 + w])
                            S.dma(rs128.ap, cin["k_rs128"][:, t0:t0 + w], q="act")
                            S.dma(rc64.ap, cin["k_rc64"][:, t0:t0 + w])
                            S.dma(rs64.ap, cin["k_rs64"][:, t0:t0 + w], q="act")

                        def proj(pv, col0, ncol, wt=wcd, nk=8, src=None):
                            for kc in range(nk):
                                rhs = hT[:, kc, :w] if src is None else src[:, kc, :w]
                                S.mm(pv, wt[:, kc, col0:col0 + ncol], rhs, start=(kc == 0), stop=(kc == nk - 1))
                        for h in range(4):
                            p = PS[h % 2]
                            proj(p[:, :w], 128 * h, 128)
                            rms_rope(p[:, :w], w, t0, 128, 0, qst[:, h, :w], lat, 1.0 / 128)
                        for h in range(2):
                            p = PS[2 + h % 2]
                            proj(p[:, :w], 512 + 128 * h, 128)
                            dst = kst[:, LK + h * 512:LK + h * 512 + w] if lat else kctx[:, h, :]
                            rms_rope(p[:, :w], w, t0, 128, 1, dst, lat, 1.0 / 128)
                        for ti in range(w // 128):
                            p = PS[4 + ti % 2]
                            for kc in range(8):
                                S.mm(p[:, 0:256], hT[:, kc, ti * 128:(ti + 1) * 128], wcd[:, kc, 768:1024],
                                     start=(kc == 0), stop=(kc == 7))
                            tg = (t0 + ti * 128) // 128
                            dst = kst[:, LV + ti * 256:LV + (ti + 1) * 256] if lat else vctx[:, ti, :]
                            S.cp(dst, p[:, 0:256], e="act")
                        ss = PS[5]
                        for k3 in range(3):
                            p = PS[k3 % 2]
                            proj(p[:, :w], 1024 + 128 * k3, 128)
                            S.cp(craw[:, k3, :w], p[:, :w], e="act")
                            S.tt(sqb[k3 % 2][:, :w], craw[:, k3, :w], craw[:, k3, :w], ALU.mult, e="pool")
                            S.mm(ss[:, :w], ones_bf.ap, sqb[k3 % 2][:, :w], start=(k3 == 0), stop=(k3 == 2))
                        S.act(rsb[:, :w], ss[:, :w], AF.Sqrt, bias=eps_t.ap, scale=1.0 / 384)
                        S.recip(rsb[:, :w], rsb[:, :w])
                        for k3 in range(3):
                            S.tt(craw[:, k3, :w], craw[:, k3, :w], rsb[:, :w], ALU.mult)
                            S.ts(cn[:, k3, :w], craw[:, k3, :w], gq[:, 2 + k3:3 + k3], None, ALU.mult)
                        for h in range(4):
                            p = PS[h % 2]
                            proj(p[:, :w], 192 * h, 128, wt=wuq, nk=3, src=cn)
                            S.cp(qst[:, 4 + h, :w], p[:, :w], e="act")
                            p2 = PS[2 + h % 2]
                            proj(p2[0:64, :w], 192 * h + 128, 64, wt=wuq, nk=3, src=cn)
                            rms_rope(p2[0:64, :w], w, t0, 64, None, qst[0:64, 8 + h, :w], lat, None)
                        ss = PS[5]
                        for k2 in range(2):
                            p = PS[k2 % 2]
                            proj(p[:, :w], 1408 + 128 * k2, 128)
                            S.cp(craw[:, k2, :w], p[:, :w], e="act")
                            S.tt(sqb[k2 % 2][:, :w], craw[:, k2, :w], craw[:, k2, :w], ALU.mult, e="pool")
                            S.mm(ss[:, :w], ones_bf.ap, sqb[k2 % 2][:, :w], start=(k2 == 0), stop=(k2 == 1))
                        S.act(rsb[:, :w], ss[:, :w], AF.Sqrt, bias=eps_t.ap, scale=1.0 / 256)
                        S.recip(rsb[:, :w], rsb[:, :w])
                        for k2 in range(2):
                            S.tt(craw[:, k2, :w], craw[:, k2, :w], rsb[:, :w], ALU.mult)
                            S.ts(kn[:, k2, :w], craw[:, k2, :w], gq[:, 5 + k2:6 + k2], None, ALU.mult)
                        for h in range(4):
                            p = PS[h % 2]
                            proj(p[:, :w], 256 * h, 128, wt=wukv, nk=2, src=kn)
                            dst = kst[:, LKD + h * 512:LKD + h * 512 + w] if lat else kdctx[:, h, :]
                            S.cp(dst, p[:, :w], e="act")
                        for ti in range(w // 128):
                            p = PS[4 + ti % 2]
                            for k2 in range(2):
                                S.mm(p[:, 0:512], kn[:, k2, ti * 128:(ti + 1) * 128],
                                     wukvv[:, k2].re("p h d -> p (h d)"), start=(k2 == 0), stop=(k2 == 1))
                            tg = (t0 + ti * 128) // 128
                            dst = kst[:, LVD + ti * 512:LVD + (ti + 1) * 512] if lat else vdctx[:, ti, :]
                            S.cp(dst, p[:, 0:512])
                        p = PS[2]
                        proj(p[0:64, :w], 1664, 64)
                        dst = kst[0:64, LKR:LKR + w] if lat else krctx.ap
                        rms_rope(p[0:64, :w], w, t0, 64, None, dst, lat, None)
                        S.dma(q_scr[:, :, t0:t0 + w], qst[:, :, :w])
                        if lat:
                            nt_ = w // 128
                            tg0 = t0 // 128
                            for h in range(2):
                                S.dma(kvs_(OK_ + h * TL + t0, OK_ + h * TL + t0 + w), kst[:, LK + h * 512:LK + h * 512 + w])
                            for h in range(4):
                                S.dma(kvs_(OKD + h * TL + t0, OKD + h * TL + t0 + w),
                                      kst[:, LKD + h * 512:LKD + h * 512 + w], q="act")
                            S.dma(kvs_(OKR + t0, OKR + t0 + w), kst[:, LKR:LKR + w])
                            S.dma(kvs_(OV + tg0 * 256, OV + (tg0 + nt_) * 256), kst[:, LV:LV + nt_ * 256], q="act")
                            S.dma(kvs_(OVD + tg0 * 512, OVD + (tg0 + nt_) * 512), kst[:, LVD:LVD + nt_ * 512])
                    S.barrier()
                for i in range(7):
                    S.allgather(kv_dst[i], kv_src[i], groups)
                with ExitStack() as ph2:
                    wo = load_w(ph2, "wo", w_out_cd[j].re("(kc p) n -> p kc n", p=128), [128, 8, 1024])
                    NK = 66
                    Kf = S.sb("Kf", [128, NK * 128], BF16, es=ph2)
                    Krf = S.sb("Krf", [64, NK * 128], BF16, es=ph2)
                    Vf = S.sb("Vf", [128, NK, 128], BF16, es=ph2)
                    qh = S.sb("qh", [128, NT], BF16, es=ph2)
                    qrh = S.sb("qrh", [64, NT], BF16, es=ph2)
                    oh = S.sb("oh", [128, NT], BF16, es=ph2)
                    Pb = [S.sb("Pb%d" % i, [128, 512], BF16, es=ph2) for i in range(3)]
                    rd = S.sb("rd", [128, 512], F32, es=ph2)
                    S.cp(Krf[:, 0:TCX], krctx.ap, e="pool")
                    S.dma(Krf[:, TCX:].re("p (r t) -> p r t", r=4), kvd_(OKR, OKR + TL, rows=64))
                    pcount = [0]

                    def attend(qv, qrv, keys, scale, ocol, wq):
                        o_ps = PS[4 + pcount[0] % 2]
                        d_ps = PS[6 + pcount[0] % 2]
                        for kc in range(keys):
                            pcount[0] += 1
                            s_ps = PS[pcount[0] % 4]
                            if qrv is None:
                                S.mm(s_ps[:, :wq], Kf[:, kc * 128:(kc + 1) * 128], qv)
                            else:
                                S.mm(s_ps[:, :wq], Kf[:, kc * 128:(kc + 1) * 128], qv, start=True, stop=False)
                                S.mm(s_ps[:, :wq], Krf[:, kc * 128:(kc + 1) * 128], qrv, start=False, stop=True)
                            P = Pb[pcount[0] % 3]
                            S.act(P[:, :wq], s_ps[:, :wq], AF.Exp, scale=scale)
                            S.mm(o_ps[:, :wq], Vf[:, kc, :], P[:, :wq], start=(kc == 0), stop=(kc == keys - 1))
                            S.mm(d_ps[:, :wq], ones_bf.ap, P[:, :wq], start=(kc == 0), stop=(kc == keys - 1))
                        S.recip(rd[:, :wq], d_ps[:, :wq])
                        S.tt(oh[:, ocol:ocol + wq], o_ps[:, :wq], rd[:, :wq], ALU.mult)

                    for hidx in range(8):
                        mla = hidx >= 4
                        h = hidx % 4
                        if not mla:
                            if h % 2 == 0:
                                kvh = h // 2
                                S.cp(Kf[:, 0:TCX], kctx[:, kvh, :], e="pool")
                                S.dma(Kf[:, TCX:].re("p (r t) -> p r t", r=4),
                                      kvd_(OK_ + kvh * TL, OK_ + (kvh + 1) * TL))
                                S.cp(Vf[:, 0:2, :], vctx.ap.re("p i (h d) -> p i h d", h=2)[:, :, kvh, :], e="pool")
                                for r_ in range(4):
                                    S.dma(Vf[:, 2 + 16 * r_:2 + 16 * (r_ + 1), :],
                                          kvd_(OV, OV + 4096).re("p r (i h d) -> p r i h d", i=16, h=2)[:, r_, :, kvh, :],
                                          q=("sp" if r_ % 2 == 0 else "act"))
                            S.dma(qh.ap, q_scr[:, h, :])
                            scale = 128.0 ** -0.5
                        else:
                            S.cp(Kf[:, 0:TCX], kdctx[:, h, :], e="pool")
                            S.dma(Kf[:, TCX:].re("p (r t) -> p r t", r=4),
                                  kvd_(OKD + h * TL, OKD + (h + 1) * TL))
                            S.cp(Vf[:, 0:2, :], vdctx.ap.re("p i (h d) -> p i h d", h=4)[:, :, h, :], e="pool")
                            for hf in range(2):
                                for r_ in range(4):
                                    S.dma(Vf[:, 2 + 16 * r_ + 8 * hf:2 + 16 * r_ + 8 * hf + 8, :],
                                          kvd_(OVD + 4096 * hf, OVD + 4096 * (hf + 1)).re("p r (i h d) -> p r i h d", i=8, h=4)[:, r_, :, h, :],
                                          q=("sp" if r_ % 2 == 0 else "act"))
                            S.dma(qh.ap, q_scr[:, 4 + h, :])
                            S.dma(qrh.ap, q_scr[0:64, 8 + h, :])
                            scale = 192.0 ** -0.5
                        for qb in range(4):
                            attend(qh[:, qb * 512:(qb + 1) * 512], qrh[:, qb * 512:(qb + 1) * 512] if mla else None,
                                   NK, scale, qb * 512, 512)
                        attend(qh[:, TL:NT], qrh[:, TL:NT] if mla else None, 2, scale, TL, TCX)
                        for ci, (t0, w) in enumerate(CH):
                            st = 0 if t0 < TL else 1
                            for oc in range(8):
                                pcount[0] += 1
                                p = PS[pcount[0] % 4]
                                S.mm(p[:, :w], wo[:, hidx, oc * 128:(oc + 1) * 128], oh[:, t0:t0 + w])
                                S.stt(xv(oc, ci), p[:, :w], prm[:, 2, oc, st:st + 1], xv(oc, ci), ALU.mult, ALU.add)
                    S.barrier()

        import os
        skip = os.environ.get("DEV_SKIP", "").split(",")
        for l in range(l0, n_layers):
            if "mod" not in skip:
                mod_phase(l)
            if "mix" not in skip:
                if l % 2 == 0:
                    even_phase(l)
                else:
                    odd_phase(l)
            if "moe" not in skip:
                moe_phase(l)
        if final:
            with ExitStack() as ph:
                gf = S.sb("gf", [128, 8], F32, es=ph)
                S.dma(gf.ap, gfinT.ap)
                of = S.sb("of", [128, 8, 512], F32, es=ph)
                for ci in norm_mod(ph, gf, None, lambda ci, c: of[:, c, :CH[ci][1]], "nf"):
                    t0, w = CH[ci]
                    if t0 < TL:
                        S.dma(y[:, :, t0:t0 + w], of[:, :, :w])
                S.barrier()
        else:
            for c in range(8):
                S.dma(y[:, c, :], xT[:, c, 0:TL])
        S.finish([y])
        print("ninst", S.ninst, flush=True)
    return nc


def _fm(a):
    t = a.shape[0]
    return np.ascontiguousarray(a.T.reshape(8, 128, t).transpose(1, 0, 2))


def _vecT(v, nchunk):
    return np.ascontiguousarray(np.swapaxes(v.reshape(v.shape[:-1] + (nchunk, 128)), -1, -2))


def make_in_maps(inp, n_layers=4):
    f = lambda k: np.ascontiguousarray(np.asarray(inp[k], dtype=np.float32))
    shared = {
        "mod_bT": _vecT(f("mod_b"), 48), "gmixT": _vecT(f("norm_mix_g"), 8),
        "gffnT": _vecT(f("norm_ffn_g"), 8), "gfinT": _vecT(f("final_norm_g"), 8),
        "w_in_ab": f("w_in_ab"),
        "conv_wT": np.ascontiguousarray(f("conv_w").transpose(0, 2, 1).reshape(2, 4, 128, 31).transpose(0, 2, 1, 3)),
        "conv_bT": _vecT(f("conv_b"), 4), "conv_gT": _vecT(f("conv_norm_g"), 4),
        "w_out_ab": f("w_out_ab"), "w_in_cd": f("w_in_cd"),
        "q_gT": _vecT(f("q_norm_g"), 1), "k_gT": _vecT(f("k_norm_g"), 1),
        "cq_gT": _vecT(f("cq_norm_g"), 3), "ckv_gT": _vecT(f("ckv_norm_g"), 2),
        "w_uq": f("w_uq"), "w_ukv": f("w_ukv"), "w_out_cd": f("w_out_cd"),
        "rw": np.ascontiguousarray(np.concatenate([f("router_grp_w"), f("router_exp_w")], -1)),
        "rb": np.ascontiguousarray(np.concatenate([f("router_grp_b"), f("router_exp_b")], -1)[:, None, :]),
    }
    for l in range(n_layers):
        shared["ewg%d" % l] = f("exp_w_gate")[l]
        shared["ewu%d" % l] = f("exp_w_up")[l]
        shared["ewd%d" % l] = f("exp_w_down")[l]
        shared["mod_w%d" % l] = f("mod_w")[l]
    shared.update(_consts_common())
    x = f("x")
    ctx = f("ctx")
    c = f("c")
    cc = f("c_ctx")
    maps = []
    for core in range(8):
        b, q = core // 4, core % 4
        m = dict(shared)
        xt = np.concatenate([x[b, 2048 * q:2048 * (q + 1)], ctx[b]], 0)
        m["x0"] = _fm(xt)
        m["scT"] = np.ascontiguousarray(np.stack([c[b], cc], 0).reshape(2, 8, 128).transpose(2, 1, 0))
        m.update(_consts_core(q))
        maps.append(m)
    return maps


def assemble(res):
    out = np.zeros((2, 8192, 1024), np.float32)
    for core in range(8):
        b, q = core // 4, core % 4
        yv = np.asarray(res[core]["y"])
        out[b, 2048 * q:2048 * (q + 1), :] = yv.transpose(2, 1, 0).reshape(2048, 1024)
    return out


_NC = {}


def kernel(**inputs):
    key = (4, True)
    if key not in _NC:
        _NC[key] = build(4, True)
    maps = make_in_maps(inputs)
    res = run_bass_kernel_spmd(_NC[key], maps, core_ids=list(range(8)))
    return assemble(res.results)
```

```python
import numpy as np
from contextlib import ExitStack
import concourse.bass as bass
import concourse.mybir as mybir
from concourse.bass_utils import run_bass_kernel_spmd

F32 = mybir.dt.float32
BF16 = mybir.dt.bfloat16
AF = mybir.ActivationFunctionType
ALU = mybir.AluOpType
AX = mybir.AxisListType

NT, TL, TCX = 2304, 2048, 256
CH = [(0, 512), (512, 512), (1024, 512), (1536, 512), (2048, 256)]
EPS = 1e-6


class Trk:
    __slots__ = ("lw", "rd")

    def __init__(self):
        self.lw = None
        self.rd = []


class T:
    def __init__(self, h, name=""):
        self.h = h
        self.name = name
        self.trk = Trk()
        self.subs = {}

    def __getitem__(self, idx):
        return V(self.trk, self.h[idx])

    @property
    def ap(self):
        return V(self.trk, self.h[:])

    def sv(self, key, idx):
        t = self.subs.get(key)
        if t is None:
            t = self.subs[key] = Trk()
        return V(t, self.h[idx])


class V:
    __slots__ = ("t", "a")

    def __init__(self, t, a):
        self.t = t
        self.a = a

    def __getitem__(self, idx):
        return V(self.t, self.a[idx])

    def re(self, pat, **kw):
        return V(self.t, self.a.rearrange(pat, **kw))

    def bc(self, shape):
        return V(self.t, self.a.broadcast_to(list(shape)))


class Sched:
    NDMA = 24
    NSW = 16

    def __init__(self, nc, es):
        self.nc = nc
        self.es = es
        self.engs = {"pe": nc.tensor, "act": nc.scalar, "dve": nc.vector, "pool": nc.gpsimd, "sp": nc.sync}
        self.sem = {}
        self.cnt = {}
        self.es0 = es
        for k in list(self.engs) + ["d%d" % i for i in range(self.NDMA)] + ["cc"]:
            self.sem[k] = es.enter_context(nc.semaphore("s_" + k))
            self.cnt[k] = 0
        self.dma_rr = 0
        self.sw_rr = 0
        self.gen = {}
        self.cons = {}
        self.pend = {e: [] for e in self.engs}
        self.seen = {e: {} for e in self.engs}
        self.ninst = 0
        self.uid = 0

    def sb(self, name, shape, dt=F32, es=None):
        self.uid += 1
        h = (es or self.es).enter_context(self.nc.sbuf_tensor("%s_%d" % (name, self.uid), list(shape), dt))
        return T(h, name)

    def ps(self, name, shape, dt=F32):
        h = self.es.enter_context(self.nc.psum_tensor(name, list(shape), dt))
        return T(h, name)

    def dram(self, name, shape, dt, kind="Internal"):
        return T(self.nc.dram_tensor(name, list(shape), dt, kind=kind), name)

    def _wait(self, e, tok):
        if tok is None:
            return
        if len(tok) == 3:
            k, v, g = tok
            if g != self.gen[k]:
                return
        else:
            k, v = tok
        if self.seen[e].get(k, 0) >= v:
            return
        if k == e and e == "pe":
            return
        self.engs[e].wait_ge(self.sem[k], v)
        self.seen[e][k] = v
        self.ninst += 1
        if len(tok) == 3:
            self.pend[e].append(k)

    def _flush_pend(self, e, tok):
        if self.pend[e]:
            for k in self.pend[e]:
                self.cons[k].append(tok)
            self.pend[e] = []

    def _deps(self, e, reads, writes):
        for r in reads:
            self._wait(e, r.t.lw)
        for w in writes:
            self._wait(e, w.t.lw)
            for tok in w.t.rd:
                self._wait(e, tok)

    def _commit(self, tok, reads, writes):
        for r in reads:
            rd = r.t.rd
            rd.append(tok)
            if len(rd) > 48:
                best = {}
                for k, v in rd:
                    if best.get(k, 0) < v:
                        best[k] = v
                r.t.rd = list(best.items())
        for w in writes:
            w.t.lw = tok
            w.t.rd = []

    def op(self, e, fn, reads, writes):
        reads = [r for r in reads if isinstance(r, V)]
        self._deps(e, reads, writes)
        ins = fn(self.engs[e])
        self.cnt[e] += 1
        ins.then_inc(self.sem[e], 1)
        self._commit((e, self.cnt[e]), reads, writes)
        self._flush_pend(e, (e, self.cnt[e]))
        self.ninst += 1
        return ins

    def dma_sw(self, out, in_):
        k = "w%d" % self.sw_rr
        self.sw_rr += 1
        self.sem[k] = self.es0.enter_context(self.nc.semaphore("s_" + k))
        self.cnt[k] = 0
        self._deps("pool", [in_], [out])
        ins = self.nc.gpsimd.dma_start(out=out.a, in_=in_.a)
        self.cnt[k] = 16
        ins.then_inc(self.sem[k], 16)
        tok = (k, 16)
        self._commit(tok, [in_], [out])
        self.ninst += 1

    def dma(self, out, in_, q="sp"):
        if q == "pool":
            return self.dma_sw(out, in_)
        k = "d%d" % self.dma_rr
        self.dma_rr = (self.dma_rr + 1) % self.NDMA
        if self.cnt[k] > 0:
            self._wait(q, (k, self.cnt[k]))
        self._deps(q, [in_], [out])
        ins = self.engs[q].dma_start(out=out.a, in_=in_.a)
        self.cnt[k] += 16
        ins.then_inc(self.sem[k], 16)
        self._commit((k, self.cnt[k]), [in_], [out])
        self._flush_pend(q, (k, self.cnt[k]))
        self.ninst += 1

    def allgather(self, dst, src, groups):
        self._deps("pool", [src.ap], [dst.ap])
        ins = self.nc.gpsimd.collective_compute("AllGather", ALU.bypass, replica_groups=groups,
                                                ins=[src.h.ap()], outs=[dst.h.ap()])
        self.cnt["cc"] += 1
        ins.then_inc(self.sem["cc"], 1)
        self._commit(("cc", self.cnt["cc"]), [src.ap], [dst.ap])
        self.ninst += 1

    def barrier(self):
        for e in self.engs:
            for k, c in self.cnt.items():
                if c > 0:
                    self._wait(e, (k, c, self.gen[k]) if k in self.gen else (k, c))

    def finish(self, outs):
        for o in outs:
            self._wait("sp", o.trk.lw)
        self.barrier()

    def mm(self, out, lhsT, rhs, start=True, stop=True, **kw):
        return self.op("pe", lambda E: E.matmul(out.a, lhsT.a, rhs.a, start=start, stop=stop, **kw),
                       [lhsT, rhs] + ([] if start else [out]), [out])

    def tr(self, out, in_, ident):
        return self.op("pe", lambda E: E.transpose(out.a, in_.a, ident.a), [in_, ident], [out])

    def act(self, out, in_, func, bias=None, scale=None, accum=None):
        kw = {}
        rd = [in_]
        wr = [out]
        if bias is not None:
            kw["bias"] = bias.a if isinstance(bias, V) else bias
            rd.append(bias)
        if scale is not None:
            kw["scale"] = scale.a if isinstance(scale, V) else scale
            rd.append(scale)
        if accum is not None:
            kw["accum_out"] = accum.a
            wr.append(accum)
        return self.op("act", lambda E: E.activation(out.a, in_.a, func, **kw), rd, wr)

    def tt(self, out, a, b, op, e="dve"):
        return self.op(e, lambda E: E.tensor_tensor(out.a, a.a, b.a, op), [a, b], [out])

    def ts(self, out, a, s1, s2, op0, op1=None, e="dve"):
        g = lambda s: s.a if isinstance(s, V) else s
        if op1 is None:
            return self.op(e, lambda E: E.tensor_scalar(out.a, a.a, g(s1), None, op0), [a, s1], [out])
        return self.op(e, lambda E: E.tensor_scalar(out.a, a.a, g(s1), g(s2), op0, op1), [a, s1, s2], [out])

    def stt(self, out, a, s, b, op0, op1, e="dve"):
        g = lambda x: x.a if isinstance(x, V) else x
        return self.op(e, lambda E: E.scalar_tensor_tensor(out.a, a.a, g(s), b.a, op0, op1), [a, s, b], [out])

    def cp(self, out, in_, e="dve"):
        if e == "act":
            return self.op(e, lambda E: E.copy(out.a, in_.a), [in_], [out])
        return self.op(e, lambda E: E.tensor_copy(out.a, in_.a), [in_], [out])

    def red(self, out, in_, op, e="dve"):
        return self.op(e, lambda E: E.tensor_reduce(out.a, in_.a, AX.X, op), [in_], [out])

    def memset(self, out, val, e="dve"):
        return self.op(e, lambda E: E.memset(out.a, val), [], [out])

    def recip(self, out, in_):
        return self.op("dve", lambda E: E.reciprocal(out.a, in_.a), [in_], [out])


def _consts_common():
    c = np.arange(128)
    Fc = np.exp(-2j * np.pi * np.outer(c, c) / 128) / np.sqrt(128)
    FcCS = np.concatenate([Fc.real, Fc.imag], 1)
    F128 = np.exp(-2j * np.pi * np.outer(c, c) / 128)
    R_re = np.concatenate([F128.real, F128.imag], 1)
    R_im = np.concatenate([-F128.imag, F128.real], 1)
    q4 = lambda R: np.stack([np.concatenate([R[:, 32 * t:32 * t + 32], R[:, 128 + 32 * t:160 + 32 * t]], 1)
                             for t in range(4)], 1)
    n2 = np.arange(64)
    Tw = np.exp(-2j * np.pi * np.outer(n2, c) / 8192)
    n = np.arange(256)
    F256 = np.exp(-2j * np.pi * np.outer(n, n) / 256) / 16.0
    t256 = lambda M: M.reshape(2, 128, 256).transpose(1, 0, 2)
    rot128 = np.zeros((128, 128))
    for i in range(64):
        rot128[2 * i, 2 * i + 1] = 1.0
        rot128[2 * i + 1, 2 * i] = -1.0
    sel = np.zeros((32, 32, 128))
    for e in range(32):
        sel[e, e, :] = 1.0
    d = {"c_ident": np.eye(128), "c_fccs": FcCS, "c_rqre": q4(R_re), "c_rqim": q4(R_im),
         "c_twc": Tw.real, "c_tws": Tw.imag, "c_c256": t256(F256.real), "c_s256": t256(-F256.imag),
         "c_rot": rot128, "c_sel": sel}
    return {k: np.ascontiguousarray(v, dtype=np.float32) for k, v in d.items()}


def _consts_core(q):
    n2 = np.arange(64)
    F64 = np.exp(-2j * np.pi * np.outer(n2, np.arange(64)) / 64) / np.sqrt(8192.0)
    sl = slice(16 * q, 16 * q + 16)
    n = 2048 * q + np.arange(2048)
    row = (n // 64).astype(np.float64)
    col = (n % 64).astype(np.float64)

    def tables(dim):
        nf = dim // 4
        inv = 10000.0 ** (-np.arange(nf, dtype=np.float64) / nf)
        ang = np.concatenate([row[:, None] * inv[None, :], col[:, None] * inv[None, :]], -1)
        ang = np.repeat(ang, 2, axis=1).T
        return np.cos(ang), np.sin(ang)
    c128, s128 = tables(128)
    c64, s64 = tables(64)
    hm = np.zeros((128, 8))
    if q > 0:
        hm[:, q - 1] = 1.0
    if q < 3:
        hm[:, 4 + q + 1] = 1.0
    d = {"k_f64c": F64.real[:, sl], "k_f64s": -F64.imag[:, sl], "k_rc128": c128, "k_rs128": s128,
         "k_rc64": c64, "k_rs64": s64, "k_hmask": hm}
    return {k: np.ascontiguousarray(v, dtype=np.float32) for k, v in d.items()}


def build(n_layers=4, final=True, l0=0):
    nc = bass.Bass("TRN2", target_bir_lowering=False)
    groups = [[0, 1, 2, 3], [4, 5, 6, 7]]
    with ExitStack() as es:
        S = Sched(nc, es)
        din = lambda name, shape: S.dram(name, shape, F32, "ExternalInput")
        x0 = din("x0", [128, 8, NT])
        scT = din("scT", [128, 8, 2])
        mod_w = [din("mod_w%d" % l, [1024, 6144]) for l in range(n_layers)]
        mod_bT = din("mod_bT", [4, 128, 48])
        gmixT = din("gmixT", [4, 128, 8])
        gffnT = din("gffnT", [4, 128, 8])
        gfinT = din("gfinT", [128, 8])
        w_in_ab = din("w_in_ab", [2, 1024, 1536])
        conv_wT = din("conv_wT", [2, 128, 4, 31])
        conv_bT = din("conv_bT", [2, 128, 4])
        conv_gT = din("conv_gT", [2, 128, 4])
        w_out_ab = din("w_out_ab", [2, 1024, 1024])
        w_in_cd = din("w_in_cd", [2, 1024, 1728])
        q_gT = din("q_gT", [2, 128, 1])
        k_gT = din("k_gT", [2, 128, 1])
        cq_gT = din("cq_gT", [2, 128, 3])
        ckv_gT = din("ckv_gT", [2, 128, 2])
        w_uq = din("w_uq", [2, 384, 768])
        w_ukv = din("w_ukv", [2, 256, 1024])
        w_out_cd = din("w_out_cd", [2, 1024, 1024])
        rw = din("rw", [4, 1024, 36])
        rb = din("rb", [4, 1, 36])
        ewg = [din("ewg%d" % l, [32, 1024, 512]) for l in range(n_layers)]
        ewu = [din("ewu%d" % l, [32, 1024, 512]) for l in range(n_layers)]
        ewd = [din("ewd%d" % l, [32, 512, 1024]) for l in range(n_layers)]
        cin = {}
        for nm, shp in [("c_ident", [128, 128]), ("c_fccs", [128, 256]), ("c_rqre", [128, 4, 64]),
                        ("c_rqim", [128, 4, 64]), ("c_twc", [64, 128]), ("c_tws", [64, 128]),
                        ("c_c256", [128, 2, 256]), ("c_s256", [128, 2, 256]), ("c_rot", [128, 128]),
                        ("c_sel", [32, 32, 128]), ("k_f64c", [64, 16]), ("k_f64s", [64, 16]),
                        ("k_rc128", [128, 2048]), ("k_rs128", [128, 2048]), ("k_rc64", [64, 2048]),
                        ("k_rs64", [64, 2048]), ("k_hmask", [128, 8])]:
            cin[nm] = din(nm, shp)
        y = S.dram("y", [128, 8, TL], F32, "ExternalOutput")
        pa_src = [S.dram("pa_src%d" % i, [128, 2 * TL], BF16) for i in range(2)]
        pa_dst = [S.dram("pa_dst%d" % i, [512, 2 * TL], BF16) for i in range(2)]
        halo_src = S.dram("halo_src", [128, 120], BF16)
        halo_dst = S.dram("halo_dst", [512, 120], BF16)
        XC = 26624
        KVW = [4096] * 6 + [2048]
        kv_src = [S.dram("kv_src%d" % i, [128, KVW[i]], BF16) for i in range(7)]
        kv_dst = [S.dram("kv_dst%d" % i, [512, KVW[i]], BF16) for i in range(7)]

        def kvs_(c0, c1):
            pi = c0 // 4096
            assert (c1 - 1) // 4096 == pi
            return kv_src[pi][:, c0 - 4096 * pi:c1 - 4096 * pi]

        def kvd_(c0, c1, rows=128):
            pi = c0 // 4096
            assert (c1 - 1) // 4096 == pi
            return kv_dst[pi].ap.re("(r p) c -> p r c", p=128)[0:rows, :, c0 - 4096 * pi:c1 - 4096 * pi]
        wt_scr = S.dram("wt_scr", [32, NT], F32)
        q_scr = S.dram("q_scr", [128, 12, NT], BF16)

        xT = S.sb("xT", [128, 8, NT], F32)
        PS = [S.ps("ps%d" % i, [128, 512], F32) for i in range(8)]
        ident = S.sb("ident", [128, 128], F32)
        ident_bf = S.sb("ident_bf", [128, 128], BF16)
        ones_bf = S.sb("ones_bf", [128, 128], BF16)
        eps_t = S.sb("eps_t", [128, 1], F32)
        sc_t = S.sb("sc_t", [128, 8, 2], F32)
        modv = S.sb("modv", [128, 48, 2], F32)
        prm = S.sb("prm", [128, 6, 8, 2], F32)
        gtmp = S.sb("gtmp", [128, 16], F32)
        S.dma(ident.ap, cin["c_ident"].ap)
        S.dma(ident_bf.ap, cin["c_ident"].ap, q="pool")
        S.memset(ones_bf.ap, 1.0)
        S.memset(eps_t.ap, EPS)
        S.dma(sc_t.ap, scT.ap)
        S.act(sc_t.ap, sc_t.ap, AF.Silu)
        for c in range(8):
            S.dma(xT[:, c, :], x0[:, c, :], q=("sp" if c % 2 == 0 else "act"))
        S.barrier()

        def xv(c, ci):
            t0, w = CH[ci]
            return xT.sv((c, ci), (slice(None), c, slice(t0, t0 + w)))

        def mod_phase(l):
            with ExitStack() as ph:
                wbuf = [S.sb("modw%d" % i, [128, 3072], F32, es=ph) for i in range(2)]
                mb = S.sb("modb", [128, 48], F32, es=ph)
                gm = S.sb("gm", [128, 16], F32, es=ph)
                S.dma(mb.ap, mod_bT[l])
                S.dma(gm[:, 0:8], gmixT[l])
                S.dma(gm[:, 8:16], gffnT[l])
                mps = PS[7]
                first = True
                i = 0
                for kc in range(8):
                    for hf in range(2):
                        wb = wbuf[i % 2]
                        i += 1
                        S.dma(wb.ap, mod_w[l][kc * 128:(kc + 1) * 128, hf * 3072:(hf + 1) * 3072],
                              q=("sp" if i % 2 else "act"))
                        for j in range(24):
                            jj = hf * 24 + j
                            S.mm(mps[:, 2 * jj:2 * jj + 2], wb[:, j * 128:(j + 1) * 128], sc_t[:, kc, :],
                                 start=first, stop=(kc == 7 and jj == 47), skip_group_check=True)
                            first = False
                S.tt(modv.ap, mps[:, 0:96].re("p (j s) -> p j s", s=2),
                     mb.ap.re("p (j o) -> p j o", o=1).bc([128, 48, 2]), ALU.add)
                for half, (ish, isc, ig) in enumerate([(0, 1, 2), (3, 4, 5)]):
                    gv = gm[:, 8 * half:8 * half + 8].re("p (c o) -> p c o", o=1).bc([128, 8, 2])
                    A = prm[:, 3 * half + 0]
                    S.ts(A, modv[:, 8 * isc:8 * isc + 8, :], 1.0, None, ALU.add)
                    S.tt(A, A, gv, ALU.mult)
                    S.cp(prm[:, 3 * half + 1], modv[:, 8 * ish:8 * ish + 8, :])
                    S.cp(prm[:, 3 * half + 2], modv[:, 8 * ig:8 * ig + 8, :])
                S.barrier()

        def norm_mod(ph, A, B, out_fn, name):
            sq = [S.sb(name + "sq%d" % i, [128, 512], BF16, es=ph) for i in range(2)]
            rs = S.sb(name + "rs", [128, 512], F32, es=ph)
            tmp = [S.sb(name + "tmp%d" % i, [128, 512], F32, es=ph) for i in range(2)]
            for ci, (t0, w) in enumerate(CH):
                st = 0 if t0 < TL else 1
                ss = PS[6]
                for c in range(8):
                    s_ = sq[c % 2]
                    S.act(s_[:, :w], xv(c, ci), AF.Square)
                    S.mm(ss[:, :w], ones_bf.ap, s_[:, :w], start=(c == 0), stop=(c == 7))
                S.act(rs[:, :w], ss[:, :w], AF.Sqrt, bias=eps_t.ap, scale=1.0 / 1024)
                S.recip(rs[:, :w], rs[:, :w])
                for c in range(8):
                    t_ = tmp[c % 2]
                    S.tt(t_[:, :w], xv(c, ci), rs[:, :w], ALU.mult, e="pool")
                    if B is None:
                        S.ts(out_fn(ci, c), t_[:, :w], A[:, c:c + 1], None, ALU.mult)
                    else:
                        S.ts(out_fn(ci, c), t_[:, :w], A[:, c, st:st + 1], B[:, c, st:st + 1], ALU.mult, ALU.add)
                yield ci

        def load_w(ph, name, src_view, shape, q="pool"):
            t = S.sb(name, shape, BF16, es=ph)
            S.dma(t.ap, src_view, q=q)
            return t

        def moe_phase(l):
            with ExitStack() as ph:
                h2T = S.sb("h2T", [128, 8, NT], BF16, es=ph)
                with ExitStack() as ph2:
                    WT = S.sb("WT", [32, NT], F32, es=ph2)
                    h2f = S.sb("h2f", [128, 8, 512], F32, es=ph2)
                    rwt = S.sb("rwt", [128, 8, 36], F32, es=ph2)
                    rbt = S.sb("rbt", [128, 36], F32, es=ph2)
                    S.dma(rwt.ap, rw[l].re("(kc p) n -> p kc n", p=128))
                    S.dma(rbt.ap, rb[l].bc([128, 36]))
                    sm = S.sb("sm", [128, 160], F32, es=ph2)
                    for ci in norm_mod(ph2, prm[:, 3], prm[:, 4], lambda ci, c: h2f[:, c, :CH[ci][1]], "n2"):
                        t0, w = CH[ci]
                        for c in range(8):
                            S.cp(h2T[:, c, t0:t0 + w], h2f[:, c, :w], e="act")
                        for ti in range(w // 128):
                            lg_ps = PS[5]
                            for kc in range(8):
                                S.mm(lg_ps[:, 0:36], h2f[:, kc, ti * 128:(ti + 1) * 128], rwt[:, kc, :],
                                     start=(kc == 0), stop=(kc == 7))
                            lg = sm[:, 0:36]
                            S.tt(lg, lg_ps[:, 0:36], rbt.ap, ALU.add)
                            gmax = sm[:, 36:37]
                            S.red(gmax, sm[:, 0:4], ALU.max)
                            goh = sm[:, 40:44]
                            S.ts(goh, sm[:, 0:4], gmax, None, ALU.is_equal)
                            ngm = sm[:, 37:38]
                            S.ts(ngm, gmax, -1.0, None, ALU.mult)
                            gsum = sm[:, 38:39]
                            S.memset(gsum, 0.0)
                            S.act(sm[:, 44:48], sm[:, 0:4], AF.Exp, bias=ngm, scale=1.0, accum=gsum)
                            gprob = sm[:, 39:40]
                            S.recip(gprob, gsum)
                            em = sm[:, 48:80]
                            S.tt(em.re("p (g e) -> p g e", g=4), sm[:, 4:36].re("p (g e) -> p g e", g=4),
                                 goh.re("p (g o) -> p g o", o=1).bc([128, 4, 8]), ALU.mult)
                            esel = sm[:, 80:88]
                            S.red(esel, em.re("p (g e) -> p e g", g=4), ALU.add)
                            m1 = sm[:, 88:89]
                            S.red(m1, esel, ALU.max)
                            oh1 = sm[:, 96:104]
                            S.ts(oh1, esel, m1, None, ALU.is_equal)
                            es2 = sm[:, 104:112]
                            S.stt(es2, oh1, -1e30, esel, ALU.mult, ALU.add)
                            m2 = sm[:, 89:90]
                            S.red(m2, es2, ALU.max)
                            oh2 = sm[:, 112:120]
                            S.ts(oh2, es2, m2, None, ALU.is_equal)
                            dd = sm[:, 90:91]
                            S.tt(dd, m2, m1, ALU.subtract)
                            ee = sm[:, 91:92]
                            S.act(ee, dd, AF.Exp)
                            S.ts(ee, ee, 1.0, None, ALU.add)
                            w1 = sm[:, 92:93]
                            S.recip(w1, ee)
                            S.tt(w1, w1, gprob, ALU.mult)
                            w2 = sm[:, 93:94]
                            S.tt(w2, gprob, w1, ALU.subtract)
                            wsel = sm[:, 120:128]
                            S.ts(wsel, oh1, w1, None, ALU.mult)
                            S.stt(wsel, oh2, w2, wsel, ALU.mult, ALU.add)
                            wf = sm[:, 128:160]
                            S.tt(wf.re("p (g e) -> p g e", g=4), goh.re("p (g o) -> p g o", o=1).bc([128, 4, 8]),
                                 wsel.re("p (o e) -> p o e", o=1).bc([128, 4, 8]), ALU.mult)
                            tp = PS[4]
                            S.tr(tp[0:32, 0:128], wf, ident.ap)
                            S.cp(WT[:, t0 + ti * 128:t0 + (ti + 1) * 128], tp[0:32, 0:128], e="act")
                    S.dma(wt_scr.ap, WT.ap)
                    S.barrier()
                wg = [S.sb("wg%d" % i, [128, 8, 512], BF16, es=ph) for i in range(2)]
                wu = [S.sb("wu%d" % i, [128, 8, 512], BF16, es=ph) for i in range(2)]
                wd = [S.sb("wd%d" % i, [128, 4, 1024], BF16, es=ph) for i in range(2)]
                stg = [S.sb("stg%d" % i, [128, 2048], F32, es=ph) for i in range(2)]
                wbs = [S.sb("wbs%d" % i, [128, 512], F32, es=ph) for i in range(2)]
                sg = [S.sb("sg%d" % i, [128, 512], F32, es=ph) for i in range(2)]
                hh = [S.sb("hh%d" % i, [128, 4, 512], BF16, es=ph) for i in range(2)]
                ucnt = [0]

                def unit(e, u):
                    b = e % 2
                    if u < 2:
                        return (ewg[l][e].re("(kc p) n -> p kc n", p=128)[:, 4 * u:4 * u + 4, :],
                                wg[b][:, 4 * u:4 * u + 4, :], 4)
                    if u < 4:
                        return (ewu[l][e].re("(kc p) n -> p kc n", p=128)[:, 4 * (u - 2):4 * (u - 2) + 4, :],
                                wu[b][:, 4 * (u - 2):4 * (u - 2) + 4, :], 4)
                    return (ewd[l][e].re("(kc p) n -> p kc n", p=128)[:, 2 * (u - 4):2 * (u - 4) + 2, :],
                            wd[b][:, 2 * (u - 4):2 * (u - 4) + 2, :], 2)
                ucnt[0] = 0
                for u in range(6):
                    src, dst, kc = unit(0, u)
                    st_ = stg[u % 2]
                    S.dma(st_.ap.re("p (kc n) -> p kc n", kc=kc), src, q="sp")
                    S.cp(dst, st_.ap.re("p (kc n) -> p kc n", kc=kc), e=("act" if u % 2 == 0 else "pool"))
                import os as _os
                NEXP = int(_os.environ.get("DEV_NEXP", "32"))
                ub = [S.sb("ub%d" % i, [128, 512], F32, es=ph) for i in range(2)]
                steps = [(e, ci) for e in range(NEXP) for ci in range(len(CH))]

                def dma_u(e1, u):
                    src, dst, kc = unit(e1, u)
                    S.dma(stg[u % 2].ap.re("p (kc n) -> p kc n", kc=kc), src, q="sp")

                def cast_u(e1, u):
                    src, dst, kc = unit(e1, u)
                    S.cp(dst, stg[u % 2].ap.re("p (kc n) -> p kc n", kc=kc), e=("act" if u % 2 == 0 else "pool"))

                def front(si):
                    e, ci = steps[si]
                    t0, w = CH[ci]
                    b = e % 2
                    wb = wbs[si % 2]
                    S.dma(wb[:, :w], wt_scr[e:e + 1, t0:t0 + w].bc([128, w]))
                    hb = hh[si % 2]
                    for j in range(4):
                        g_ps = PS[j % 2]
                        u_ps = PS[2 + j % 2]
                        for kc in range(8):
                            S.mm(g_ps[:, :w], wg[b][:, kc, j * 128:(j + 1) * 128], h2T[:, kc, t0:t0 + w],
                                 start=(kc == 0), stop=(kc == 7))
                        for kc in range(8):
                            S.mm(u_ps[:, :w], wu[b][:, kc, j * 128:(j + 1) * 128], h2T[:, kc, t0:t0 + w],
                                 start=(kc == 0), stop=(kc == 7))
                        s_ = sg[j % 2]
                        S.act(s_[:, :w], g_ps[:, :w], AF.Silu)
                        u_ = ub[j % 2]
                        S.cp(u_[:, :w], u_ps[:, :w], e="act")
                        S.tt(u_[:, :w], u_[:, :w], s_[:, :w], ALU.mult, e="pool")
                        S.tt(hb[:, j, :w], u_[:, :w], wb[:, :w], ALU.mult, e="pool")

                def back(si):
                    e, ci = steps[si]
                    t0, w = CH[ci]
                    st = 0 if t0 < TL else 1
                    b = e % 2
                    hb = hh[si % 2]
                    for oc in range(8):
                        d_ps = PS[4 + oc % 4]
                        for j in range(4):
                            S.mm(d_ps[:, :w], wd[b][:, j, oc * 128:(oc + 1) * 128], hb[:, j, :w],
                                 start=(j == 0), stop=(j == 3))
                        S.stt(xv(oc, ci), d_ps[:, :w], prm[:, 5, oc, st:st + 1], xv(oc, ci), ALU.mult, ALU.add)
                    if e + 1 < NEXP:
                        cast_u(e + 1, ci)
                        if ci + 2 < 6:
                            dma_u(e + 1, ci + 2)
                        if ci == 4:
                            cast_u(e + 1, 5)
                            if e + 2 < NEXP:
                                dma_u(e + 2, 0)
                                dma_u(e + 2, 1)
                if NEXP > 1:
                    dma_u(1, 0)
                    dma_u(1, 1)
                front(0)
                for si in range(len(steps)):
                    if si + 1 < len(steps):
                        front(si + 1)
                    back(si)
                S.barrier()

        def even_phase(l):
            j = l // 2
            with ExitStack() as ph:
                yac = S.sb("yac", [128, 4, TCX], BF16, es=ph)
                ybT = S.sb("ybT", [128, 4, NT], BF16, es=ph)
                with ExitStack() as pu_:
                    uext = S.sb("uext", [128, 4, TL + 30], BF16, es=pu_)
                    ucx = S.sb("ucx", [128, 4, TCX + 30], BF16, es=pu_)
                    S.memset(ucx.ap, 0.0, e="pool")
                    with ExitStack() as ph2:
                        paT = S.sb("paT", [128, 4, NT], BF16, es=ph2)
                        hTc = [S.sb("hTc%d" % i, [128, 8, 512], BF16, es=ph2) for i in range(2)]
                        wab = load_w(ph2, "wab", w_in_ab[j].re("(kc p) n -> p kc n", p=128), [128, 8, 1536])
                        sgt = [S.sb("sgt%d" % i, [128, 512], F32, es=ph2) for i in range(2)]
                        fccs = load_w(ph2, "fccs", cin["c_fccs"].ap, [128, 256])
                        c256 = load_w(ph2, "c256", cin["c_c256"].ap, [128, 2, 256])
                        s256 = load_w(ph2, "s256", cin["c_s256"].ap, [128, 2, 256])
                        zc = S.sb("zc", [128, 2, 256], BF16, es=ph2)
                        for ci in norm_mod(ph2, prm[:, 0], prm[:, 1],
                                           lambda ci, c: hTc[ci % 2][:, c, :CH[ci][1]], "n1"):
                            t0, w = CH[ci]
                            hT = hTc[ci % 2]
                            for g in range(4):
                                p = PS[g % 2]
                                for kc in range(8):
                                    S.mm(p[:, :w], wab[:, kc, g * 128:(g + 1) * 128], hT[:, kc, :w],
                                         start=(kc == 0), stop=(kc == 7))
                                S.cp(paT[:, g, t0:t0 + w], p[:, :w], e="act")
                            for cc in range(4):
                                pu = PS[2 + cc % 2]
                                pg = PS[4 + cc % 2]
                                for kc in range(8):
                                    S.mm(pu[:, :w], wab[:, kc, 512 + cc * 128:512 + (cc + 1) * 128], hT[:, kc, :w],
                                         start=(kc == 0), stop=(kc == 7))
                                for kc in range(8):
                                    S.mm(pg[:, :w], wab[:, kc, 1024 + cc * 128:1024 + (cc + 1) * 128], hT[:, kc, :w],
                                         start=(kc == 0), stop=(kc == 7))
                                s_ = sgt[cc % 2]
                                S.act(s_[:, :w], pg[:, :w], AF.Sigmoid)
                                dst = uext[:, cc, 15 + t0:15 + t0 + w] if t0 < TL else ucx[:, cc, 15:15 + TCX]
                                S.tt(dst, pu[:, :w], s_[:, :w], ALU.mult)
                        for i in range(2):
                            S.dma(pa_src[i].ap.re("p (g t) -> p g t", g=2), paT[:, 2 * i:2 * i + 2, 0:TL])
                            S.allgather(pa_dst[i], pa_src[i], groups)
                        for g in range(4):
                            for i in range(2):
                                p = PS[i]
                                S.mm(p[:, 0:256], paT[:, g, TL + 128 * i:TL + 128 * (i + 1)], fccs.ap)
                                S.cp(zc[:, i, :], p[:, 0:256], e="act")
                            p = PS[2 + g % 2]
                            for i in range(2):
                                S.mm(p[:, 0:256], zc[:, i, 0:128], c256[:, i, :], start=(i == 0), stop=False)
                                S.mm(p[:, 0:256], zc[:, i, 128:256], s256[:, i, :], start=False, stop=(i == 1))
                            S.cp(yac[:, g, :], p[:, 0:256])
                        S.barrier()
                    with ExitStack() as ph2:
                        hs = S.sb("hs", [128, 4, 2, 15], BF16, es=ph2)
                        S.cp(hs[:, :, 0, :], uext[:, :, 15:30], e="pool")
                        S.cp(hs[:, :, 1, :], uext[:, :, TL:TL + 15], e="pool")
                        S.dma(halo_src.ap.re("p (c s k) -> p c s k", c=4, s=2), hs.ap)
                        S.allgather(halo_dst, halo_src, groups)
                        hall = S.sb("hall", [128, 4, 4, 2, 15], BF16, es=ph2)
                        S.dma(hall.ap, halo_dst.ap.re("(r p) (c s k) -> p r c s k", p=128, c=4, s=2))
                        hm = S.sb("hm", [128, 8], F32, es=ph2)
                        S.dma(hm.ap, cin["k_hmask"].ap)
                        hl = S.sb("hl", [128, 4, 15], F32, es=ph2)
                        hr = S.sb("hr", [128, 4, 15], F32, es=ph2)
                        S.memset(hl.ap, 0.0)
                        S.memset(hr.ap, 0.0)
                        for r in range(4):
                            S.stt(hl.ap, hall[:, r, :, 1, :], hm[:, r:r + 1], hl.ap, ALU.mult, ALU.add)
                            S.stt(hr.ap, hall[:, r, :, 0, :], hm[:, 4 + r:5 + r], hr.ap, ALU.mult, ALU.add)
                        S.cp(uext[:, :, 0:15], hl.ap)
                        S.cp(uext[:, :, TL + 15:TL + 30], hr.ap)
                        cw = S.sb("cw", [128, 4, 31], F32, es=ph2)
                        cb = S.sb("cb", [128, 4], F32, es=ph2)
                        cg = S.sb("cg", [128, 4], F32, es=ph2)
                        S.dma(cw.ap, conv_wT[j])
                        S.dma(cb.ap, conv_bT[j])
                        S.dma(cg.ap, conv_gT[j])
                        dg = [S.sb("dg%d" % cc, [128, 31, 128], BF16, es=ph2) for cc in range(4)]
                        for cc in range(4):
                            for k in range(31):
                                S.ts(dg[cc][:, k, :], ident.ap, cw[:, cc, k:k + 1], None, ALU.mult,
                                     e=("dve" if k % 2 else "pool"))
                        vb = [S.sb("vb%d" % cc, [128, 512], F32, es=ph2) for cc in range(4)]
                        sqb = [S.sb("sqb%d" % i, [128, 512], BF16, es=ph2) for i in range(2)]
                        rsb = S.sb("rsb", [128, 512], F32, es=ph2)
                        for ci, (t0, w) in enumerate(CH):
                            ss = PS[6]
                            for cc in range(4):
                                p = PS[cc % 2]
                                for k in range(31):
                                    src = uext[:, cc, t0 + k:t0 + k + w] if t0 < TL else ucx[:, cc, k:k + w]
                                    S.mm(p[:, :w], dg[cc][:, k, :], src, start=(k == 0), stop=(k == 30))
                                S.act(vb[cc][:, :w], p[:, :w], AF.Identity, bias=cb[:, cc:cc + 1], scale=1.0)
                                s_ = sqb[cc % 2]
                                S.tt(s_[:, :w], vb[cc][:, :w], vb[cc][:, :w], ALU.mult, e="pool")
                                S.mm(ss[:, :w], ones_bf.ap, s_[:, :w], start=(cc == 0), stop=(cc == 3))
                            S.act(rsb[:, :w], ss[:, :w], AF.Sqrt, bias=eps_t.ap, scale=1.0 / 512)
                            S.recip(rsb[:, :w], rsb[:, :w])
                            for cc in range(4):
                                S.tt(vb[cc][:, :w], vb[cc][:, :w], rsb[:, :w], ALU.mult)
                                S.act(ybT[:, cc, t0:t0 + w], vb[cc][:, :w], AF.Silu, scale=cg[:, cc:cc + 1])
                        S.barrier()
                yal = S.sb("yal", [128, 4, TL], BF16, es=ph)
                with ExitStack() as ph2:
                    fccs = load_w(ph2, "fccs", cin["c_fccs"].ap, [128, 256])
                    rqre = load_w(ph2, "rqre", cin["c_rqre"].ap, [128, 4, 64])
                    rqim = load_w(ph2, "rqim", cin["c_rqim"].ap, [128, 4, 64])
                    f64c = load_w(ph2, "f64c", cin["k_f64c"].ap, [64, 16])
                    f64s = load_w(ph2, "f64s", cin["k_f64s"].ap, [64, 16])
                    twc = S.sb("twc", [64, 128], F32, es=ph2)
                    tws = S.sb("tws", [64, 128], F32, es=ph2)
                    S.dma(twc.ap, cin["c_twc"].ap)
                    S.dma(tws.ap, cin["c_tws"].ap)
                    paf = S.sb("paf", [128, 4 * TL], BF16, es=ph2)
                    Z = S.sb("Z", [128, 64, 256], BF16, es=ph2)
                    Vq = S.sb("Vq", [64, 128, 2, 32], BF16, es=ph2)
                    t4 = [S.sb("t4_%d" % i, [64, 8, 32], F32, es=ph2) for i in range(4)]
                    for g in range(4):
                        S.dma(paf.ap.re("p (r t) -> p r t", r=4),
                              pa_dst[g // 2].ap.re("(r p) (g t) -> p r g t", p=128, g=2)[:, :, g % 2, :])
                        for i in range(32):
                            p = PS[i % 2]
                            for h in range(2):
                                n2 = 2 * i + h
                                S.mm(p[:, 256 * h:256 * (h + 1)], paf[:, n2:4 * TL:64], fccs.ap)
                            S.cp(Z[:, 2 * i:2 * i + 2, :].re("p a b -> p (a b)"), p.ap,
                                 e=("act" if i % 2 else "dve"))
                        for qt in range(4):
                            for kb in range(16):
                                p = PS[2 + kb % 2]
                                for k8 in range(8):
                                    kc = kb * 8 + k8
                                    S.mm(p[0:64, 64 * k8:64 * (k8 + 1)], Z[:, :, kc], rqre[:, qt, :],
                                         start=True, stop=False)
                                    S.mm(p[0:64, 64 * k8:64 * (k8 + 1)], Z[:, :, 128 + kc], rqim[:, qt, :],
                                         start=False, stop=True)
                                U = p[0:64, :].re("p (k r a) -> p k r a", k=8, r=2)
                                tcv = twc[:, 32 * qt:32 * qt + 32].re("p (o a) -> p o a", o=1).bc([64, 8, 32])
                                tsv = tws[:, 32 * qt:32 * qt + 32].re("p (o a) -> p o a", o=1).bc([64, 8, 32])
                                S.tt(t4[0].ap, U[:, :, 0, :], tcv, ALU.mult)
                                S.tt(t4[1].ap, U[:, :, 1, :], tsv, ALU.mult)
                                S.tt(t4[2].ap, U[:, :, 0, :], tsv, ALU.mult)
                                S.tt(t4[3].ap, U[:, :, 1, :], tcv, ALU.mult)
                                S.tt(Vq[:, kb * 8:kb * 8 + 8, 0, :], t4[0].ap, t4[1].ap, ALU.subtract, e="pool")
                                S.tt(Vq[:, kb * 8:kb * 8 + 8, 1, :], t4[2].ap, t4[3].ap, ALU.add, e="pool")
                            p = PS[4 + qt % 2]
                            for a in range(32):
                                S.mm(p[:, 16 * a:16 * (a + 1)], Vq[:, :, 0, a], f64c.ap, start=True, stop=False)
                                S.mm(p[:, 16 * a:16 * (a + 1)], Vq[:, :, 1, a], f64s.ap, start=False, stop=True)
                            dst = yal[:, g, :].re("p (jj k) -> p k jj", k=128)[:, 32 * qt:32 * qt + 32, :]
                            S.cp(dst, p.ap.re("p (a jj) -> p a jj", a=32), e="act")
                    S.barrier()
                with ExitStack() as ph2:
                    wo = load_w(ph2, "wo", w_out_ab[j].re("(kc p) n -> p kc n", p=128), [128, 8, 1024])
                    for ci, (t0, w) in enumerate(CH):
                        st = 0 if t0 < TL else 1
                        for oc in range(8):
                            p = PS[oc % 4]
                            for kc in range(8):
                                if kc < 4:
                                    rhs = yal[:, kc, t0:t0 + w] if t0 < TL else yac[:, kc, :]
                                else:
                                    rhs = ybT[:, kc - 4, t0:t0 + w]
                                S.mm(p[:, :w], wo[:, kc, oc * 128:(oc + 1) * 128], rhs,
                                     start=(kc == 0), stop=(kc == 7))
                            S.stt(xv(oc, ci), p[:, :w], prm[:, 2, oc, st:st + 1], xv(oc, ci), ALU.mult, ALU.add)
                    S.barrier()

        def odd_phase(l):
            j = l // 2
            OK_, OKD, OV, OVD, OKR = 0, 4096, 12288, 16384, 24576
            with ExitStack() as ph:
                kctx = S.sb("kctx", [128, 2, TCX], BF16, es=ph)
                kdctx = S.sb("kdctx", [128, 4, TCX], BF16, es=ph)
                krctx = S.sb("krctx", [64, TCX], BF16, es=ph)
                vctx = S.sb("vctx", [128, 2, 256], BF16, es=ph)
                vdctx = S.sb("vdctx", [128, 2, 512], BF16, es=ph)
                with ExitStack() as ph2:
                    hTc = [S.sb("hTc%d" % i, [128, 8, 512], BF16, es=ph2) for i in range(2)]
                    wcd = load_w(ph2, "wcd", w_in_cd[j].re("(kc p) n -> p kc n", p=128), [128, 8, 1728])
                    wuq = load_w(ph2, "wuq", w_uq[j].re("(kc p) n -> p kc n", p=128), [128, 3, 768])
                    wukv = load_w(ph2, "wukv", w_ukv[j].re("(kc p) n -> p kc n", p=128), [128, 2, 1024])
                    wukvv = S.sb("wukvv", [128, 2, 4, 128], BF16, es=ph2)
                    for kc_ in range(2):
                        S.dma(wukvv[:, kc_], w_ukv[j].re("(kc p) (h two d) -> p kc h two d", p=128, h=4, two=2)[:, kc_, :, 1, :],
                              q="pool")
                    rot = load_w(ph2, "rot", cin["c_rot"].ap, [128, 128])
                    rc128 = S.sb("rc128", [128, 512], F32, es=ph2)
                    rs128 = S.sb("rs128", [128, 512], F32, es=ph2)
                    rc64 = S.sb("rc64", [64, 512], F32, es=ph2)
                    rs64 = S.sb("rs64", [64, 512], F32, es=ph2)
                    gq = S.sb("gq", [128, 8], F32, es=ph2)
                    S.dma(gq[:, 0:1], q_gT[j])
                    S.dma(gq[:, 1:2], k_gT[j])
                    S.dma(gq[:, 2:5], cq_gT[j])
                    S.dma(gq[:, 5:7], ckv_gT[j])
                    kst = S.sb("kst", [128, 6656], BF16, es=ph2)
                    LK, LKD, LKR, LV, LVD = 0, 1024, 3072, 3584, 4608
                    qst = S.sb("qst", [128, 12, 512], BF16, es=ph2)
                    sqb = [S.sb("sqb%d" % i, [128, 512], BF16, es=ph2) for i in range(2)]
                    rsb = S.sb("rsb", [128, 512], F32, es=ph2)
                    qn = [S.sb("qn%d" % i, [128, 512], BF16, es=ph2) for i in range(2)]
                    f1 = [S.sb("f1_%d" % i, [128, 512], F32, es=ph2) for i in range(2)]
                    f2 = [S.sb("f2_%d" % i, [128, 512], F32, es=ph2) for i in range(2)]
                    craw = S.sb("craw", [128, 3, 512], F32, es=ph2)
                    cn = S.sb("cn", [128, 3, 512], BF16, es=ph2)
                    kn = S.sb("kn", [128, 2, 512], BF16, es=ph2)
                    S.memset(qst.ap, 0.0, e="pool")
                    S.memset(kst.ap, 0.0, e="pool")
                    cnt = [0]

                    def rms_rope(p, w, t0, np_, gcol, dst, rope, inv_dim):
                        cnt[0] += 1
                        i = cnt[0] % 2
                        if gcol is not None:
                            S.act(sqb[i][:np_, :w], p, AF.Square)
                            ss = PS[6]
                            S.mm(ss[:np_, :w], ones_bf[:np_, :np_], sqb[i][:np_, :w])
                            S.act(rsb[:np_, :w], ss[:np_, :w], AF.Sqrt, bias=eps_t[:np_, :], scale=inv_dim)
                            S.recip(rsb[:np_, :w], rsb[:np_, :w])
                            S.tt(f1[i][:np_, :w], p, rsb[:np_, :w], ALU.mult)
                            tgt = qn[i][:np_, :w] if rope else dst
                            S.ts(tgt, f1[i][:np_, :w], gq[:np_, gcol:gcol + 1], None, ALU.mult)
                        else:
                            tgt = qn[i][:np_, :w] if rope else dst
                            S.cp(tgt, p, e="act")
                        if rope:
                            rp = PS[7]
                            S.mm(rp[:np_, :w], rot[:np_, :np_], qn[i][:np_, :w])
                            rc, rs_ = (rc128, rs128) if np_ == 128 else (rc64, rs64)
                            S.tt(f1[i][:np_, :w], qn[i][:np_, :w], rc[:np_, :w], ALU.mult, e="pool")
                            S.tt(f2[i][:np_, :w], rp[:np_, :w], rs_[:np_, :w], ALU.mult)
                            S.tt(dst, f1[i][:np_, :w], f2[i][:np_, :w], ALU.add, e="pool")

                    for ci in norm_mod(ph2, prm[:, 0], prm[:, 1],
                                       lambda ci, c: hTc[ci % 2][:, c, :CH[ci][1]], "n1"):
                        t0, w = CH[ci]
                        lat = t0 < TL
                        hT = hTc[ci % 2]
                        if lat:
                            S.dma(rc128.ap, cin["k_rc128"][:, t0:t0 + w])
                            S.dma(rs128.ap, cin["k_rs128"][:, t0:t0 + w], q="act")
                            S.dma(rc64.ap, cin["k_rc64"][:, t0:t0 + w])
                            S.dma(rs64.ap, cin["k_rs64"][:, t0:t0 + w], q="act")

                        def proj(pv, col0, ncol, wt=wcd, nk=8, src=None):
                            for kc in range(nk):
                                rhs = hT[:, kc, :w] if src is None else src[:, kc, :w]
                                S.mm(pv, wt[:, kc, col0:col0 + ncol], rhs, start=(kc == 0), stop=(kc == nk - 1))
                        for h in range(4):
                            p = PS[h % 2]
                            proj(p[:, :w], 128 * h, 128)
                            rms_rope(p[:, :w], w, t0, 128, 0, qst[:, h, :w], lat, 1.0 / 128)
                        for h in range(2):
                            p = PS[2 + h % 2]
                            proj(p[:, :w], 512 + 128 * h, 128)
                            dst = kst[:, LK + h * 512:LK + h * 512 + w] if lat else kctx[:, h, :]
                            rms_rope(p[:, :w], w, t0, 128, 1, dst, lat, 1.0 / 128)
                        for ti in range(w // 128):
                            p = PS[4 + ti % 2]
                            for kc in range(8):
                                S.mm(p[:, 0:256], hT[:, kc, ti * 128:(ti + 1) * 128], wcd[:, kc, 768:1024],
                                     start=(kc == 0), stop=(kc == 7))
                            tg = (t0 + ti * 128) // 128
                            dst = kst[:, LV + ti * 256:LV + (ti + 1) * 256] if lat else vctx[:, ti, :]
                            S.cp(dst, p[:, 0:256], e="act")
                        ss = PS[5]
                        for k3 in range(3):
                            p = PS[k3 % 2]
                            proj(p[:, :w], 1024 + 128 * k3, 128)
                            S.cp(craw[:, k3, :w], p[:, :w], e="act")
                            S.tt(sqb[k3 % 2][:, :w], craw[:, k3, :w], craw[:, k3, :w], ALU.mult, e="pool")
                            S.mm(ss[:, :w], ones_bf.ap, sqb[k3 % 2][:, :w], start=(k3 == 0), stop=(k3 == 2))
                        S.act(rsb[:, :w], ss[:, :w], AF.Sqrt, bias=eps_t.ap, scale=1.0 / 384)
                        S.recip(rsb[:, :w], rsb[:, :w])
                        for k3 in range(3):
                            S.tt(craw[:, k3, :w], craw[:, k3, :w], rsb[:, :w], ALU.mult)
                            S.ts(cn[:, k3, :w], craw[:, k3, :w], gq[:, 2 + k3:3 + k3], None, ALU.mult)
                        for h in range(4):
                            p = PS[h % 2]
                            proj(p[:, :w], 192 * h, 128, wt=wuq, nk=3, src=cn)
                            S.cp(qst[:, 4 + h, :w], p[:, :w], e="act")
                            p2 = PS[2 + h % 2]
                            proj(p2[0:64, :w], 192 * h + 128, 64, wt=wuq, nk=3, src=cn)
                            rms_rope(p2[0:64, :w], w, t0, 64, None, qst[0:64, 8 + h, :w], lat, None)
                        ss = PS[5]
                        for k2 in range(2):
                            p = PS[k2 % 2]
                            proj(p[:, :w], 1408 + 128 * k2, 128)
                            S.cp(craw[:, k2, :w], p[:, :w], e="act")
                            S.tt(sqb[k2 % 2][:, :w], craw[:, k2, :w], craw[:, k2, :w], ALU.mult, e="pool")
                            S.mm(ss[:, :w], ones_bf.ap, sqb[k2 % 2][:, :w], start=(k2 == 0), stop=(k2 == 1))
                        S.act(rsb[:, :w], ss[:, :w], AF.Sqrt, bias=eps_t.ap, scale=1.0 / 256)
                        S.recip(rsb[:, :w], rsb[:, :w])
                        for k2 in range(2):
                            S.tt(craw[:, k2, :w], craw[:, k2, :w], rsb[:, :w], ALU.mult)
                            S.ts(kn[:, k2, :w], craw[:, k2, :w], gq[:, 5 + k2:6 + k2], None, ALU.mult)
                        for h in range(4):
                            p = PS[h % 2]
                            proj(p[:, :w], 256 * h, 128, wt=wukv, nk=2, src=kn)
                            dst = kst[:, LKD + h * 512:LKD + h * 512 + w] if lat else kdctx[:, h, :]
                            S.cp(dst, p[:, :w], e="act")
                        for ti in range(w // 128):
                            p = PS[4 + ti % 2]
                            for k2 in range(2):
                                S.mm(p[:, 0:512], kn[:, k2, ti * 128:(ti + 1) * 128],
                                     wukvv[:, k2].re("p h d -> p (h d)"), start=(k2 == 0), stop=(k2 == 1))
                            tg = (t0 + ti * 128) // 128
                            dst = kst[:, LVD + ti * 512:LVD + (ti + 1) * 512] if lat else vdctx[:, ti, :]
                            S.cp(dst, p[:, 0:512])
                        p = PS[2]
                        proj(p[0:64, :w], 1664, 64)
                        dst = kst[0:64, LKR:LKR + w] if lat else krctx.ap
                        rms_rope(p[0:64, :w], w, t0, 64, None, dst, lat, None)
                        S.dma(q_scr[:, :, t0:t0 + w], qst[:, :, :w])
                        if lat:
                            nt_ = w // 128
                            tg0 = t0 // 128
                            for h in range(2):
                                S.dma(kvs_(OK_ + h * TL + t0, OK_ + h * TL + t0 + w), kst[:, LK + h * 512:LK + h * 512 + w])
                            for h in range(4):
                                S.dma(kvs_(OKD + h * TL + t0, OKD + h * TL + t0 + w),
                                      kst[:, LKD + h * 512:LKD + h * 512 + w], q="act")
                            S.dma(kvs_(OKR + t0, OKR + t0 + w), kst[:, LKR:LKR + w])
                            S.dma(kvs_(OV + tg0 * 256, OV + (tg0 + nt_) * 256), kst[:, LV:LV + nt_ * 256], q="act")
                            S.dma(kvs_(OVD + tg0 * 512, OVD + (tg0 + nt_) * 512), kst[:, LVD:LVD + nt_ * 512])
                    S.barrier()
                for i in range(7):
                    S.allgather(kv_dst[i], kv_src[i], groups)
                with ExitStack() as ph2:
                    wo = load_w(ph2, "wo", w_out_cd[j].re("(kc p) n -> p kc n", p=128), [128, 8, 1024])
                    NK = 66
                    Kf = S.sb("Kf", [128, NK * 128], BF16, es=ph2)
                    Krf = S.sb("Krf", [64, NK * 128], BF16, es=ph2)
                    Vf = S.sb("Vf", [128, NK, 128], BF16, es=ph2)
                    qh = S.sb("qh", [128, NT], BF16, es=ph2)
                    qrh = S.sb("qrh", [64, NT], BF16, es=ph2)
                    oh = S.sb("oh", [128, NT], BF16, es=ph2)
                    Pb = [S.sb("Pb%d" % i, [128, 512], BF16, es=ph2) for i in range(3)]
                    rd = S.sb("rd", [128, 512], F32, es=ph2)
                    S.cp(Krf[:, 0:TCX], krctx.ap, e="pool")
                    S.dma(Krf[:, TCX:].re("p (r t) -> p r t", r=4), kvd_(OKR, OKR + TL, rows=64))
                    pcount = [0]

                    def attend(qv, qrv, keys, scale, ocol, wq):
                        o_ps = PS[4 + pcount[0] % 2]
                        d_ps = PS[6 + pcount[0] % 2]
                        for kc in range(keys):
                            pcount[0] += 1
                            s_ps = PS[pcount[0] % 4]
                            if qrv is None:
                                S.mm(s_ps[:, :wq], Kf[:, kc * 128:(kc + 1) * 128], qv)
                            else:
                                S.mm(s_ps[:, :wq], Kf[:, kc * 128:(kc + 1) * 128], qv, start=True, stop=False)
                                S.mm(s_ps[:, :wq], Krf[:, kc * 128:(kc + 1) * 128], qrv, start=False, stop=True)
                            P = Pb[pcount[0] % 3]
                            S.act(P[:, :wq], s_ps[:, :wq], AF.Exp, scale=scale)
                            S.mm(o_ps[:, :wq], Vf[:, kc, :], P[:, :wq], start=(kc == 0), stop=(kc == keys - 1))
                            S.mm(d_ps[:, :wq], ones_bf.ap, P[:, :wq], start=(kc == 0), stop=(kc == keys - 1))
                        S.recip(rd[:, :wq], d_ps[:, :wq])
                        S.tt(oh[:, ocol:ocol + wq], o_ps[:, :wq], rd[:, :wq], ALU.mult)

                    for hidx in range(8):
                        mla = hidx >= 4
                        h = hidx % 4
                        if not mla:
                            if h % 2 == 0:
                                kvh = h // 2
                                S.cp(Kf[:, 0:TCX], kctx[:, kvh, :], e="pool")
                                S.dma(Kf[:, TCX:].re("p (r t) -> p r t", r=4),
                                      kvd_(OK_ + kvh * TL, OK_ + (kvh + 1) * TL))
                                S.cp(Vf[:, 0:2, :], vctx.ap.re("p i (h d) -> p i h d", h=2)[:, :, kvh, :], e="pool")
                                for r_ in range(4):
                                    S.dma(Vf[:, 2 + 16 * r_:2 + 16 * (r_ + 1), :],
                                          kvd_(OV, OV + 4096).re("p r (i h d) -> p r i h d", i=16, h=2)[:, r_, :, kvh, :],
                                          q=("sp" if r_ % 2 == 0 else "act"))
                            S.dma(qh.ap, q_scr[:, h, :])
                            scale = 128.0 ** -0.5
                        else:
                            S.cp(Kf[:, 0:TCX], kdctx[:, h, :], e="pool")
                            S.dma(Kf[:, TCX:].re("p (r t) -> p r t", r=4),
                                  kvd_(OKD + h * TL, OKD + (h + 1) * TL))
                            S.cp(Vf[:, 0:2, :], vdctx.ap.re("p i (h d) -> p i h d", h=4)[:, :, h, :], e="pool")
                            for hf in range(2):
                                for r_ in range(4):
                                    S.dma(Vf[:, 2 + 16 * r_ + 8 * hf:2 + 16 * r_ + 8 * hf + 8, :],
                                          kvd_(OVD + 4096 * hf, OVD + 4096 * (hf + 1)).re("p r (i h d) -> p r i h d", i=8, h=4)[:, r_, :, h, :],
                                          q=("sp" if r_ % 2 == 0 else "act"))
                            S.dma(qh.ap, q_scr[:, 4 + h, :])
                            S.dma(qrh.ap, q_scr[0:64, 8 + h, :])
                            scale = 192.0 ** -0.5
                        for qb in range(4):
                            attend(qh[:, qb * 512:(qb + 1) * 512], qrh[:, qb * 512:(qb + 1) * 512] if mla else None,
                                   NK, scale, qb * 512, 512)
                        attend(qh[:, TL:NT], qrh[:, TL:NT] if mla else None, 2, scale, TL, TCX)
                        for ci, (t0, w) in enumerate(CH):
                            st = 0 if t0 < TL else 1
                            for oc in range(8):
                                pcount[0] += 1
                                p = PS[pcount[0] % 4]
                                S.mm(p[:, :w], wo[:, hidx, oc * 128:(oc + 1) * 128], oh[:, t0:t0 + w])
                                S.stt(xv(oc, ci), p[:, :w], prm[:, 2, oc, st:st + 1], xv(oc, ci), ALU.mult, ALU.add)
                    S.barrier()

        import os
        skip = os.environ.get("DEV_SKIP", "").split(",")
        for l in range(l0, n_layers):
            if "mod" not in skip:
                mod_phase(l)
            if "mix" not in skip:
                if l % 2 == 0:
                    even_phase(l)
                else:
                    odd_phase(l)
            if "moe" not in skip:
                moe_phase(l)
        if final:
            with ExitStack() as ph:
                gf = S.sb("gf", [128, 8], F32, es=ph)
                S.dma(gf.ap, gfinT.ap)
                of = S.sb("of", [128, 8, 512], F32, es=ph)
                for ci in norm_mod(ph, gf, None, lambda ci, c: of[:, c, :CH[ci][1]], "nf"):
                    t0, w = CH[ci]
                    if t0 < TL:
                        S.dma(y[:, :, t0:t0 + w], of[:, :, :w])
                S.barrier()
        else:
            for c in range(8):
                S.dma(y[:, c, :], xT[:, c, 0:TL])
        S.finish([y])
        print("ninst", S.ninst, flush=True)
    return nc


def _fm(a):
    t = a.shape[0]
    return np.ascontiguousarray(a.T.reshape(8, 128, t).transpose(1, 0, 2))


def _vecT(v, nchunk):
    return np.ascontiguousarray(np.swapaxes(v.reshape(v.shape[:-1] + (nchunk, 128)), -1, -2))


def make_in_maps(inp, n_layers=4):
    f = lambda k: np.ascontiguousarray(np.asarray(inp[k], dtype=np.float32))
    shared = {
        "mod_bT": _vecT(f("mod_b"), 48), "gmixT": _vecT(f("norm_mix_g"), 8),
        "gffnT": _vecT(f("norm_ffn_g"), 8), "gfinT": _vecT(f("final_norm_g"), 8),
        "w_in_ab": f("w_in_ab"),
        "conv_wT": np.ascontiguousarray(f("conv_w").transpose(0, 2, 1).reshape(2, 4, 128, 31).transpose(0, 2, 1, 3)),
        "conv_bT": _vecT(f("conv_b"), 4), "conv_gT": _vecT(f("conv_norm_g"), 4),
        "w_out_ab": f("w_out_ab"), "w_in_cd": f("w_in_cd"),
        "q_gT": _vecT(f("q_norm_g"), 1), "k_gT": _vecT(f("k_norm_g"), 1),
        "cq_gT": _vecT(f("cq_norm_g"), 3), "ckv_gT": _vecT(f("ckv_norm_g"), 2),
        "w_uq": f("w_uq"), "w_ukv": f("w_ukv"), "w_out_cd": f("w_out_cd"),
        "rw": np.ascontiguousarray(np.concatenate([f("router_grp_w"), f("router_exp_w")], -1)),
        "rb": np.ascontiguousarray(np.concatenate([f("router_grp_b"), f("router_exp_b")], -1)[:, None, :]),
    }
    for l in range(n_layers):
        shared["ewg%d" % l] = f("exp_w_gate")[l]
        shared["ewu%d" % l] = f("exp_w_up")[l]
        shared["ewd%d" % l] = f("exp_w_down")[l]
        shared["mod_w%d" % l] = f("mod_w")[l]
    shared.update(_consts_common())
    x = f("x")
    ctx = f("ctx")
    c = f("c")
    cc = f("c_ctx")
    maps = []
    for core in range(8):
        b, q = core // 4, core % 4
        m = dict(shared)
        xt = np.concatenate([x[b, 2048 * q:2048 * (q + 1)], ctx[b]], 0)
        m["x0"] = _fm(xt)
        m["scT"] = np.ascontiguousarray(np.stack([c[b], cc], 0).reshape(2, 8, 128).transpose(2, 1, 0))
        m.update(_consts_core(q))
        maps.append(m)
    return maps


def assemble(res):
    out = np.zeros((2, 8192, 1024), np.float32)
    for core in range(8):
        b, q = core // 4, core % 4
        yv = np.asarray(res[core]["y"])
        out[b, 2048 * q:2048 * (q + 1), :] = yv.transpose(2, 1, 0).reshape(2048, 1024)
    return out


_NC = {}


def kernel(**inputs):
    key = (4, True)
    if key not in _NC:
        _NC[key] = build(4, True)
    maps = make_in_maps(inputs)
    res = run_bass_kernel_spmd(_NC[key], maps, core_ids=list(range(8)))
    return assemble(res.results)
```

```python
import numpy as np
from contextlib import ExitStack
import concourse.bass as bass
import concourse.mybir as mybir
from concourse.bass_utils import run_bass_kernel_spmd

F32 = mybir.dt.float32
BF16 = mybir.dt.bfloat16
AF = mybir.ActivationFunctionType
ALU = mybir.AluOpType
AX = mybir.AxisListType

NT, TL, TCX = 2304, 2048, 256
CH = [(0, 512), (512, 512), (1024, 512), (1536, 512), (2048, 256)]
EPS = 1e-6


class Trk:
    __slots__ = ("lw", "rd")

    def __init__(self):
        self.lw = None
        self.rd = []


class T:
    def __init__(self, h, name=""):
        self.h = h
        self.name = name
        self.trk = Trk()
        self.subs = {}

    def __getitem__(self, idx):
        return V(self.trk, self.h[idx])

    @property
    def ap(self):
        return V(self.trk, self.h[:])

    def sv(self, key, idx):
        t = self.subs.get(key)
        if t is None:
            t = self.subs[key] = Trk()
        return V(t, self.h[idx])


class V:
    __slots__ = ("t", "a")

    def __init__(self, t, a):
        self.t = t
        self.a = a

    def __getitem__(self, idx):
        return V(self.t, self.a[idx])

    def re(self, pat, **kw):
        return V(self.t, self.a.rearrange(pat, **kw))

    def bc(self, shape):
        return V(self.t, self.a.broadcast_to(list(shape)))


class Sched:
    NDMA = 24
    NSW = 16

    def __init__(self, nc, es):
        self.nc = nc
        self.es = es
        self.engs = {"pe": nc.tensor, "act": nc.scalar, "dve": nc.vector, "pool": nc.gpsimd, "sp": nc.sync}
        self.sem = {}
        self.cnt = {}
        self.es0 = es
        for k in list(self.engs) + ["d%d" % i for i in range(self.NDMA)] + ["cc"]:
            self.sem[k] = es.enter_context(nc.semaphore("s_" + k))
            self.cnt[k] = 0
        self.dma_rr = 0
        self.sw_rr = 0
        self.gen = {}
        self.cons = {}
        self.pend = {e: [] for e in self.engs}
        self.seen = {e: {} for e in self.engs}
        self.ninst = 0
        self.uid = 0

    def sb(self, name, shape, dt=F32, es=None):
        self.uid += 1
        h = (es or self.es).enter_context(self.nc.sbuf_tensor("%s_%d" % (name, self.uid), list(shape), dt))
        return T(h, name)

    def ps(self, name, shape, dt=F32):
        h = self.es.enter_context(self.nc.psum_tensor(name, list(shape), dt))
        return T(h, name)

    def dram(self, name, shape, dt, kind="Internal"):
        return T(self.nc.dram_tensor(name, list(shape), dt, kind=kind), name)

    def _wait(self, e, tok):
        if tok is None:
            return
        if len(tok) == 3:
            k, v, g = tok
            if g != self.gen[k]:
                return
        else:
            k, v = tok
        if self.seen[e].get(k, 0) >= v:
            return
        if k == e and e == "pe":
            return
        self.engs[e].wait_ge(self.sem[k], v)
        self.seen[e][k] = v
        self.ninst += 1
        if len(tok) == 3:
            self.pend[e].append(k)

    def _flush_pend(self, e, tok):
        if self.pend[e]:
            for k in self.pend[e]:
                self.cons[k].append(tok)
            self.pend[e] = []

    def _deps(self, e, reads, writes):
        for r in reads:
            self._wait(e, r.t.lw)
        for w in writes:
            self._wait(e, w.t.lw)
            for tok in w.t.rd:
                self._wait(e, tok)

    def _commit(self, tok, reads, writes):
        for r in reads:
            rd = r.t.rd
            rd.append(tok)
            if len(rd) > 48:
                best = {}
                for k, v in rd:
                    if best.get(k, 0) < v:
                        best[k] = v
                r.t.rd = list(best.items())
        for w in writes:
            w.t.lw = tok
            w.t.rd = []

    def op(self, e, fn, reads, writes):
        reads = [r for r in reads if isinstance(r, V)]
        self._deps(e, reads, writes)
        ins = fn(self.engs[e])
        self.cnt[e] += 1
        ins.then_inc(self.sem[e], 1)
        self._commit((e, self.cnt[e]), reads, writes)
        self._flush_pend(e, (e, self.cnt[e]))
        self.ninst += 1
        return ins

    def dma_sw(self, out, in_):
        k = "w%d" % self.sw_rr
        self.sw_rr += 1
        self.sem[k] = self.es0.enter_context(self.nc.semaphore("s_" + k))
        self.cnt[k] = 0
        self._deps("pool", [in_], [out])
        ins = self.nc.gpsimd.dma_start(out=out.a, in_=in_.a)
        self.cnt[k] = 16
        ins.then_inc(self.sem[k], 16)
        tok = (k, 16)
        self._commit(tok, [in_], [out])
        self.ninst += 1

    def dma(self, out, in_, q="sp"):
        if q == "pool":
            return self.dma_sw(out, in_)
        k = "d%d" % self.dma_rr
        self.dma_rr = (self.dma_rr + 1) % self.NDMA
        if self.cnt[k] > 0:
            self._wait(q, (k, self.cnt[k]))
        self._deps(q, [in_], [out])
        ins = self.engs[q].dma_start(out=out.a, in_=in_.a)
        self.cnt[k] += 16
        ins.then_inc(self.sem[k], 16)
        self._commit((k, self.cnt[k]), [in_], [out])
        self._flush_pend(q, (k, self.cnt[k]))
        self.ninst += 1

    def allgather(self, dst, src, groups):
        self._deps("pool", [src.ap], [dst.ap])
        ins = self.nc.gpsimd.collective_compute("AllGather", ALU.bypass, replica_groups=groups,
                                                ins=[src.h.ap()], outs=[dst.h.ap()])
        self.cnt["cc"] += 1
        ins.then_inc(self.sem["cc"], 1)
        self._commit(("cc", self.cnt["cc"]), [src.ap], [dst.ap])
        self.ninst += 1

    def barrier(self):
        for e in self.engs:
            for k, c in self.cnt.items():
                if c > 0:
                    self._wait(e, (k, c, self.gen[k]) if k in self.gen else (k, c))

    def finish(self, outs):
        for o in outs:
            self._wait("sp", o.trk.lw)
        self.barrier()

    def mm(self, out, lhsT, rhs, start=True, stop=True, **kw):
        return self.op("pe", lambda E: E.matmul(out.a, lhsT.a, rhs.a, start=start, stop=stop, **kw),
                       [lhsT, rhs] + ([] if start else [out]), [out])

    def tr(self, out, in_, ident):
        return self.op("pe", lambda E: E.transpose(out.a, in_.a, ident.a), [in_, ident], [out])

    def act(self, out, in_, func, bias=None, scale=None, accum=None):
        kw = {}
        rd = [in_]
        wr = [out]
        if bias is not None:
            kw["bias"] = bias.a if isinstance(bias, V) else bias
            rd.append(bias)
        if scale is not None:
            kw["scale"] = scale.a if isinstance(scale, V) else scale
            rd.append(scale)
        if accum is not None:
            kw["accum_out"] = accum.a
            wr.append(accum)
        return self.op("act", lambda E: E.activation(out.a, in_.a, func, **kw), rd, wr)

    def tt(self, out, a, b, op, e="dve"):
        return self.op(e, lambda E: E.tensor_tensor(out.a, a.a, b.a, op), [a, b], [out])

    def ts(self, out, a, s1, s2, op0, op1=None, e="dve"):
        g = lambda s: s.a if isinstance(s, V) else s
        if op1 is None:
            return self.op(e, lambda E: E.tensor_scalar(out.a, a.a, g(s1), None, op0), [a, s1], [out])
        return self.op(e, lambda E: E.tensor_scalar(out.a, a.a, g(s1), g(s2), op0, op1), [a, s1, s2], [out])

    def stt(self, out, a, s, b, op0, op1, e="dve"):
        g = lambda x: x.a if isinstance(x, V) else x
        return self.op(e, lambda E: E.scalar_tensor_tensor(out.a, a.a, g(s), b.a, op0, op1), [a, s, b], [out])

    def cp(self, out, in_, e="dve"):
        if e == "act":
            return self.op(e, lambda E: E.copy(out.a, in_.a), [in_], [out])
        return self.op(e, lambda E: E.tensor_copy(out.a, in_.a), [in_], [out])

    def red(self, out, in_, op, e="dve"):
        return self.op(e, lambda E: E.tensor_reduce(out.a, in_.a, AX.X, op), [in_], [out])

    def memset(self, out, val, e="dve"):
        return self.op(e, lambda E: E.memset(out.a, val), [], [out])

    def recip(self, out, in_):
        return self.op("dve", lambda E: E.reciprocal(out.a, in_.a), [in_], [out])


def _consts_common():
    c = np.arange(128)
    Fc = np.exp(-2j * np.pi * np.outer(c, c) / 128) / np.sqrt(128)
    FcCS = np.concatenate([Fc.real, Fc.imag], 1)
    F128 = np.exp(-2j * np.pi * np.outer(c, c) / 128)
    R_re = np.concatenate([F128.real, F128.imag], 1)
    R_im = np.concatenate([-F128.imag, F128.real], 1)
    q4 = lambda R: np.stack([np.concatenate([R[:, 32 * t:32 * t + 32], R[:, 128 + 32 * t:160 + 32 * t]], 1)
                             for t in range(4)], 1)
    n2 = np.arange(64)
    Tw = np.exp(-2j * np.pi * np.outer(n2, c) / 8192)
    n = np.arange(256)
    F256 = np.exp(-2j * np.pi * np.outer(n, n) / 256) / 16.0
    t256 = lambda M: M.reshape(2, 128, 256).transpose(1, 0, 2)
    rot128 = np.zeros((128, 128))
    for i in range(64):
        rot128[2 * i, 2 * i + 1] = 1.0
        rot128[2 * i + 1, 2 * i] = -1.0
    sel = np.zeros((32, 32, 128))
    for e in range(32):
        sel[e, e, :] = 1.0
    d = {"c_ident": np.eye(128), "c_fccs": FcCS, "c_rqre": q4(R_re), "c_rqim": q4(R_im),
         "c_twc": Tw.real, "c_tws": Tw.imag, "c_c256": t256(F256.real), "c_s256": t256(-F256.imag),
         "c_rot": rot128, "c_sel": sel}
    return {k: np.ascontiguousarray(v, dtype=np.float32) for k, v in d.items()}


def _consts_core(q):
    n2 = np.arange(64)
    F64 = np.exp(-2j * np.pi * np.outer(n2, np.arange(64)) / 64) / np.sqrt(8192.0)
    sl = slice(16 * q, 16 * q + 16)
    n = 2048 * q + np.arange(2048)
    row = (n // 64).astype(np.float64)
    col = (n % 64).astype(np.float64)

    def tables(dim):
        nf = dim // 4
        inv = 10000.0 ** (-np.arange(nf, dtype=np.float64) / nf)
        ang = np.concatenate([row[:, None] * inv[None, :], col[:, None] * inv[None, :]], -1)
        ang = np.repeat(ang, 2, axis=1).T
        return np.cos(ang), np.sin(ang)
    c128, s128 = tables(128)
    c64, s64 = tables(64)
    hm = np.zeros((128, 8))
    if q > 0:
        hm[:, q - 1] = 1.0
    if q < 3:
        hm[:, 4 + q + 1] = 1.0
    d = {"k_f64c": F64.real[:, sl], "k_f64s": -F64.imag[:, sl], "k_rc128": c128, "k_rs128": s128,
         "k_rc64": c64, "k_rs64": s64, "k_hmask": hm}
    return {k: np.ascontiguousarray(v, dtype=np.float32) for k, v in d.items()}


def build(n_layers=4, final=True, l0=0):
    nc = bass.Bass("TRN2", target_bir_lowering=False)
    groups = [[0, 1, 2, 3], [4, 5, 6, 7]]
    with ExitStack() as es:
        S = Sched(nc, es)
        din = lambda name, shape: S.dram(name, shape, F32, "ExternalInput")
        x0 = din("x0", [128, 8, NT])
        scT = din("scT", [128, 8, 2])
        mod_w = [din("mod_w%d" % l, [1024, 6144]) for l in range(n_layers)]
        mod_bT = din("mod_bT", [4, 128, 48])
        gmixT = din("gmixT", [4, 128, 8])
        gffnT = din("gffnT", [4, 128, 8])
        gfinT = din("gfinT", [128, 8])
        w_in_ab = din("w_in_ab", [2, 1024, 1536])
        conv_wT = din("conv_wT", [2, 128, 4, 31])
        conv_bT = din("conv_bT", [2, 128, 4])
        conv_gT = din("conv_gT", [2, 128, 4])
        w_out_ab = din("w_out_ab", [2, 1024, 1024])
        w_in_cd = din("w_in_cd", [2, 1024, 1728])
        q_gT = din("q_gT", [2, 128, 1])
        k_gT = din("k_gT", [2, 128, 1])
        cq_gT = din("cq_gT", [2, 128, 3])
        ckv_gT = din("ckv_gT", [2, 128, 2])
        w_uq = din("w_uq", [2, 384, 768])
        w_ukv = din("w_ukv", [2, 256, 1024])
        w_out_cd = din("w_out_cd", [2, 1024, 1024])
        rw = din("rw", [4, 1024, 36])
        rb = din("rb", [4, 1, 36])
        ewg = [din("ewg%d" % l, [32, 1024, 512]) for l in range(n_layers)]
        ewu = [din("ewu%d" % l, [32, 1024, 512]) for l in range(n_layers)]
        ewd = [din("ewd%d" % l, [32, 512, 1024]) for l in range(n_layers)]
        cin = {}
        for nm, shp in [("c_ident", [128, 128]), ("c_fccs", [128, 256]), ("c_rqre", [128, 4, 64]),
                        ("c_rqim", [128, 4, 64]), ("c_twc", [64, 128]), ("c_tws", [64, 128]),
                        ("c_c256", [128, 2, 256]), ("c_s256", [128, 2, 256]), ("c_rot", [128, 128]),
                        ("c_sel", [32, 32, 128]), ("k_f64c", [64, 16]), ("k_f64s", [64, 16]),
                        ("k_rc128", [128, 2048]), ("k_rs128", [128, 2048]), ("k_rc64", [64, 2048]),
                        ("k_rs64", [64, 2048]), ("k_hmask", [128, 8])]:
            cin[nm] = din(nm, shp)
        y = S.dram("y", [128, 8, TL], F32, "ExternalOutput")
        pa_src = [S.dram("pa_src%d" % i, [128, 2 * TL], BF16) for i in range(2)]
        pa_dst = [S.dram("pa_dst%d" % i, [512, 2 * TL], BF16) for i in range(2)]
        halo_src = S.dram("halo_src", [128, 120], BF16)
        halo_dst = S.dram("halo_dst", [512, 120], BF16)
        XC = 26624
        KVW = [4096] * 6 + [2048]
        kv_src = [S.dram("kv_src%d" % i, [128, KVW[i]], BF16) for i in range(7)]
        kv_dst = [S.dram("kv_dst%d" % i, [512, KVW[i]], BF16) for i in range(7)]

        def kvs_(c0, c1):
            pi = c0 // 4096
            assert (c1 - 1) // 4096 == pi
            return kv_src[pi][:, c0 - 4096 * pi:c1 - 4096 * pi]

        def kvd_(c0, c1, rows=128):
            pi = c0 // 4096
            assert (c1 - 1) // 4096 == pi
            return kv_dst[pi].ap.re("(r p) c -> p r c", p=128)[0:rows, :, c0 - 4096 * pi:c1 - 4096 * pi]
        wt_scr = S.dram("wt_scr", [32, NT], F32)
        q_scr = S.dram("q_scr", [128, 12, NT], BF16)

        xT = S.sb("xT", [128, 8, NT], F32)
        PS = [S.ps("ps%d" % i, [128, 512], F32) for i in range(8)]
        ident = S.sb("ident", [128, 128], F32)
        ident_bf = S.sb("ident_bf", [128, 128], BF16)
        ones_bf = S.sb("ones_bf", [128, 128], BF16)
        eps_t = S.sb("eps_t", [128, 1], F32)
        sc_t = S.sb("sc_t", [128, 8, 2], F32)
        modv = S.sb("modv", [128, 48, 2], F32)
        prm = S.sb("prm", [128, 6, 8, 2], F32)
        gtmp = S.sb("gtmp", [128, 16], F32)
        S.dma(ident.ap, cin["c_ident"].ap)
        S.dma(ident_bf.ap, cin["c_ident"].ap, q="pool")
        S.memset(ones_bf.ap, 1.0)
        S.memset(eps_t.ap, EPS)
        S.dma(sc_t.ap, scT.ap)
        S.act(sc_t.ap, sc_t.ap, AF.Silu)
        for c in range(8):
            S.dma(xT[:, c, :], x0[:, c, :], q=("sp" if c % 2 == 0 else "act"))
        S.barrier()

        def xv(c, ci):
            t0, w = CH[ci]
            return xT.sv((c, ci), (slice(None), c, slice(t0, t0 + w)))

        def mod_phase(l):
            with ExitStack() as ph:
                wbuf = [S.sb("modw%d" % i, [128, 3072], F32, es=ph) for i in range(2)]
                mb = S.sb("modb", [128, 48], F32, es=ph)
                gm = S.sb("gm", [128, 16], F32, es=ph)
                S.dma(mb.ap, mod_bT[l])
                S.dma(gm[:, 0:8], gmixT[l])
                S.dma(gm[:, 8:16], gffnT[l])
                mps = PS[7]
                first = True
                i = 0
                for kc in range(8):
                    for hf in range(2):
                        wb = wbuf[i % 2]
                        i += 1
                        S.dma(wb.ap, mod_w[l][kc * 128:(kc + 1) * 128, hf * 3072:(hf + 1) * 3072],
                              q=("sp" if i % 2 else "act"))
                        for j in range(24):
                            jj = hf * 24 + j
                            S.mm(mps[:, 2 * jj:2 * jj + 2], wb[:, j * 128:(j + 1) * 128], sc_t[:, kc, :],
                                 start=first, stop=(kc == 7 and jj == 47), skip_group_check=True)
                            first = False
                S.tt(modv.ap, mps[:, 0:96].re("p (j s) -> p j s", s=2),
                     mb.ap.re("p (j o) -> p j o", o=1).bc([128, 48, 2]), ALU.add)
                for half, (ish, isc, ig) in enumerate([(0, 1, 2), (3, 4, 5)]):
                    gv = gm[:, 8 * half:8 * half + 8].re("p (c o) -> p c o", o=1).bc([128, 8, 2])
                    A = prm[:, 3 * half + 0]
                    S.ts(A, modv[:, 8 * isc:8 * isc + 8, :], 1.0, None, ALU.add)
                    S.tt(A, A, gv, ALU.mult)
                    S.cp(prm[:, 3 * half + 1], modv[:, 8 * ish:8 * ish + 8, :])
                    S.cp(prm[:, 3 * half + 2], modv[:, 8 * ig:8 * ig + 8, :])
                S.barrier()

        def norm_mod(ph, A, B, out_fn, name):
            sq = [S.sb(name + "sq%d" % i, [128, 512], BF16, es=ph) for i in range(2)]
            rs = S.sb(name + "rs", [128, 512], F32, es=ph)
            tmp = [S.sb(name + "tmp%d" % i, [128, 512], F32, es=ph) for i in range(2)]
            for ci, (t0, w) in enumerate(CH):
                st = 0 if t0 < TL else 1
                ss = PS[6]
                for c in range(8):
                    s_ = sq[c % 2]
                    S.act(s_[:, :w], xv(c, ci), AF.Square)
                    S.mm(ss[:, :w], ones_bf.ap, s_[:, :w], start=(c == 0), stop=(c == 7))
                S.act(rs[:, :w], ss[:, :w], AF.Sqrt, bias=eps_t.ap, scale=1.0 / 1024)
                S.recip(rs[:, :w], rs[:, :w])
                for c in range(8):
                    t_ = tmp[c % 2]
                    S.tt(t_[:, :w], xv(c, ci), rs[:, :w], ALU.mult, e="pool")
                    if B is None:
                        S.ts(out_fn(ci, c), t_[:, :w], A[:, c:c + 1], None, ALU.mult)
                    else:
                        S.ts(out_fn(ci, c), t_[:, :w], A[:, c, st:st + 1], B[:, c, st:st + 1], ALU.mult, ALU.add)
                yield ci

        def load_w(ph, name, src_view, shape, q="pool"):
            t = S.sb(name, shape, BF16, es=ph)
            S.dma(t.ap, src_view, q=q)
            return t

        def moe_phase(l):
            with ExitStack() as ph:
                h2T = S.sb("h2T", [128, 8, NT], BF16, es=ph)
                with ExitStack() as ph2:
                    WT = S.sb("WT", [32, NT], F32, es=ph2)
                    h2f = S.sb("h2f", [128, 8, 512], F32, es=ph2)
                    rwt = S.sb("rwt", [128, 8, 36], F32, es=ph2)
                    rbt = S.sb("rbt", [128, 36], F32, es=ph2)
                    S.dma(rwt.ap, rw[l].re("(kc p) n -> p kc n", p=128))
                    S.dma(rbt.ap, rb[l].bc([128, 36]))
                    sm = S.sb("sm", [128, 160], F32, es=ph2)
                    for ci in norm_mod(ph2, prm[:, 3], prm[:, 4], lambda ci, c: h2f[:, c, :CH[ci][1]], "n2"):
                        t0, w = CH[ci]
                        for c in range(8):
                            S.cp(h2T[:, c, t0:t0 + w], h2f[:, c, :w], e="act")
                        for ti in range(w // 128):
                            lg_ps = PS[5]
                            for kc in range(8):
                                S.mm(lg_ps[:, 0:36], h2f[:, kc, ti * 128:(ti + 1) * 128], rwt[:, kc, :],
                                     start=(kc == 0), stop=(kc == 7))
                            lg = sm[:, 0:36]
                            S.tt(lg, lg_ps[:, 0:36], rbt.ap, ALU.add)
                            gmax = sm[:, 36:37]
                            S.red(gmax, sm[:, 0:4], ALU.max)
                            goh = sm[:, 40:44]
                            S.ts(goh, sm[:, 0:4], gmax, None, ALU.is_equal)
                            ngm = sm[:, 37:38]
                            S.ts(ngm, gmax, -1.0, None, ALU.mult)
                            gsum = sm[:, 38:39]
                            S.memset(gsum, 0.0)
                            S.act(sm[:, 44:48], sm[:, 0:4], AF.Exp, bias=ngm, scale=1.0, accum=gsum)
                            gprob = sm[:, 39:40]
                            S.recip(gprob, gsum)
                            em = sm[:, 48:80]
                            S.tt(em.re("p (g e) -> p g e", g=4), sm[:, 4:36].re("p (g e) -> p g e", g=4),
                                 goh.re("p (g o) -> p g o", o=1).bc([128, 4, 8]), ALU.mult)
                            esel = sm[:, 80:88]
                            S.red(esel, em.re("p (g e) -> p e g", g=4), ALU.add)
                            m1 = sm[:, 88:89]
                            S.red(m1, esel, ALU.max)
                            oh1 = sm[:, 96:104]
                            S.ts(oh1, esel, m1, None, ALU.is_equal)
                            es2 = sm[:, 104:112]
                            S.stt(es2, oh1, -1e30, esel, ALU.mult, ALU.add)
                            m2 = sm[:, 89:90]
                            S.red(m2, es2, ALU.max)
                            oh2 = sm[:, 112:120]
                            S.ts(oh2, es2, m2, None, ALU.is_equal)
                            dd = sm[:, 90:91]
                            S.tt(dd, m2, m1, ALU.subtract)
                            ee = sm[:, 91:92]
                            S.act(ee, dd, AF.Exp)
                            S.ts(ee, ee, 1.0, None, ALU.add)
                            w1 = sm[:, 92:93]
                            S.recip(w1, ee)
                            S.tt(w1, w1, gprob, ALU.mult)
                            w2 = sm[:, 93:94]
                            S.tt(w2, gprob, w1, ALU.subtract)
                            wsel = sm[:, 120:128]
                            S.ts(wsel, oh1, w1, None, ALU.mult)
                            S.stt(wsel, oh2, w2, wsel, ALU.mult, ALU.add)
                            wf = sm[:, 128:160]
                            S.tt(wf.re("p (g e) -> p g e", g=4), goh.re("p (g o) -> p g o", o=1).bc([128, 4, 8]),
                                 wsel.re("p (o e) -> p o e", o=1).bc([128, 4, 8]), ALU.mult)
                            tp = PS[4]
                            S.tr(tp[0:32, 0:128], wf, ident.ap)
                            S.cp(WT[:, t0 + ti * 128:t0 + (ti + 1) * 128], tp[0:32, 0:128], e="act")
                    S.dma(wt_scr.ap, WT.ap)
                    S.barrier()
                wg = [S.sb("wg%d" % i, [128, 8, 512], BF16, es=ph) for i in range(2)]
                wu = [S.sb("wu%d" % i, [128, 8, 512], BF16, es=ph) for i in range(2)]
                wd = [S.sb("wd%d" % i, [128, 4, 1024], BF16, es=ph) for i in range(2)]
                stg = [S.sb("stg%d" % i, [128, 2048], F32, es=ph) for i in range(2)]
                wbs = [S.sb("wbs%d" % i, [128, 512], F32, es=ph) for i in range(2)]
                sg = [S.sb("sg%d" % i, [128, 512], F32, es=ph) for i in range(2)]
                hh = [S.sb("hh%d" % i, [128, 4, 512], BF16, es=ph) for i in range(2)]
                ucnt = [0]

                def unit(e, u):
                    b = e % 2
                    if u < 2:
                        return (ewg[l][e].re("(kc p) n -> p kc n", p=128)[:, 4 * u:4 * u + 4, :],
                                wg[b][:, 4 * u:4 * u + 4, :], 4)
                    if u < 4:
                        return (ewu[l][e].re("(kc p) n -> p kc n", p=128)[:, 4 * (u - 2):4 * (u - 2) + 4, :],
                                wu[b][:, 4 * (u - 2):4 * (u - 2) + 4, :], 4)
                    return (ewd[l][e].re("(kc p) n -> p kc n", p=128)[:, 2 * (u - 4):2 * (u - 4) + 2, :],
                            wd[b][:, 2 * (u - 4):2 * (u - 4) + 2, :], 2)
                ucnt[0] = 0
                for u in range(6):
                    src, dst, kc = unit(0, u)
                    st_ = stg[u % 2]
                    S.dma(st_.ap.re("p (kc n) -> p kc n", kc=kc), src, q="sp")
                    S.cp(dst, st_.ap.re("p (kc n) -> p kc n", kc=kc), e=("act" if u % 2 == 0 else "pool"))
                import os as _os
                NEXP = int(_os.environ.get("DEV_NEXP", "32"))
                ub = [S.sb("ub%d" % i, [128, 512], F32, es=ph) for i in range(2)]
                NCH = len(CH)
                steps = [(e, ci) for e in range(NEXP) for ci in range(NCH)]

                def dma_u(e1, u):
                    src, dst, kc = unit(e1, u)
                    S.dma(stg[u % 2].ap.re("p (kc n) -> p kc n", kc=kc), src, q="sp")

                def cast_u(e1, u):
                    src, dst, kc = unit(e1, u)
                    S.cp(dst, stg[u % 2].ap.re("p (kc n) -> p kc n", kc=kc), e=("act" if u % 2 == 0 else "pool"))

                def front(si):
                    e, ci = steps[si]
                    t0, w = CH[ci]
                    b = e % 2
                    wb = wbs[si % 2]
                    S.dma(wb[:, :w], wt_scr[e:e + 1, t0:t0 + w].bc([128, w]))
                    hb = hh[si % 2]
                    for j in range(4):
                        g_ps = PS[j % 2]
                        u_ps = PS[2 + j % 2]
                        for kc in range(8):
                            S.mm(g_ps[:, :w], wg[b][:, kc, j * 128:(j + 1) * 128], h2T[:, kc, t0:t0 + w],
                                 start=(kc == 0), stop=(kc == 7))
                        for kc in range(8):
                            S.mm(u_ps[:, :w], wu[b][:, kc, j * 128:(j + 1) * 128], h2T[:, kc, t0:t0 + w],
                                 start=(kc == 0), stop=(kc == 7))
                        s_ = sg[j % 2]
                        S.act(s_[:, :w], g_ps[:, :w], AF.Silu)
                        u_ = ub[j % 2]
                        S.cp(u_[:, :w], u_ps[:, :w], e="act")
                        S.tt(u_[:, :w], u_[:, :w], s_[:, :w], ALU.mult, e="pool")
                        S.tt(hb[:, j, :w], u_[:, :w], wb[:, :w], ALU.mult, e="pool")

                def back(si):
                    e, ci = steps[si]
                    t0, w = CH[ci]
                    st = 0 if t0 < TL else 1
                    b = e % 2
                    hb = hh[si % 2]
                    for oc in range(8):
                        d_ps = PS[4 + oc % 4]
                        for j in range(4):
                            S.mm(d_ps[:, :w], wd[b][:, j, oc * 128:(oc + 1) * 128], hb[:, j, :w],
                                 start=(j == 0), stop=(j == 3))
                        S.stt(xv(oc, ci), d_ps[:, :w], prm[:, 5, oc, st:st + 1], xv(oc, ci), ALU.mult, ALU.add)
                    if e + 1 < NEXP:
                        cast_u(e + 1, ci)
                        if ci + 2 < 6:
                            dma_u(e + 1, ci + 2)
                        if ci == NCH - 1:
                            for u_ in range(ci + 1, 6):
                                cast_u(e + 1, u_)
                                if u_ + 2 < 6:
                                    dma_u(e + 1, u_ + 2)
                            if e + 2 < NEXP:
                                dma_u(e + 2, 0)
                                dma_u(e + 2, 1)
                if NEXP > 1:
                    dma_u(1, 0)
                    dma_u(1, 1)
                front(0)
                for si in range(len(steps)):
                    if si + 1 < len(steps):
                        front(si + 1)
                    back(si)
                S.barrier()

        def even_phase(l):
            j = l // 2
            with ExitStack() as ph:
                yac = S.sb("yac", [128, 4, TCX], BF16, es=ph)
                ybT = S.sb("ybT", [128, 4, NT], BF16, es=ph)
                with ExitStack() as pu_:
                    uext = S.sb("uext", [128, 4, TL + 30], BF16, es=pu_)
                    ucx = S.sb("ucx", [128, 4, TCX + 30], BF16, es=pu_)
                    S.memset(ucx.ap, 0.0, e="pool")
                    with ExitStack() as ph2:
                        paT = S.sb("paT", [128, 4, NT], BF16, es=ph2)
                        hTc = [S.sb("hTc%d" % i, [128, 8, 512], BF16, es=ph2) for i in range(2)]
                        wab = load_w(ph2, "wab", w_in_ab[j].re("(kc p) n -> p kc n", p=128), [128, 8, 1536])
                        sgt = [S.sb("sgt%d" % i, [128, 512], F32, es=ph2) for i in range(2)]
                        fccs = load_w(ph2, "fccs", cin["c_fccs"].ap, [128, 256])
                        c256 = load_w(ph2, "c256", cin["c_c256"].ap, [128, 2, 256])
                        s256 = load_w(ph2, "s256", cin["c_s256"].ap, [128, 2, 256])
                        zc = S.sb("zc", [128, 2, 256], BF16, es=ph2)
                        for ci in norm_mod(ph2, prm[:, 0], prm[:, 1],
                                           lambda ci, c: hTc[ci % 2][:, c, :CH[ci][1]], "n1"):
                            t0, w = CH[ci]
                            hT = hTc[ci % 2]
                            for g in range(4):
                                p = PS[g % 2]
                                for kc in range(8):
                                    S.mm(p[:, :w], wab[:, kc, g * 128:(g + 1) * 128], hT[:, kc, :w],
                                         start=(kc == 0), stop=(kc == 7))
                                S.cp(paT[:, g, t0:t0 + w], p[:, :w], e="act")
                            for cc in range(4):
                                pu = PS[2 + cc % 2]
                                pg = PS[4 + cc % 2]
                                for kc in range(8):
                                    S.mm(pu[:, :w], wab[:, kc, 512 + cc * 128:512 + (cc + 1) * 128], hT[:, kc, :w],
                                         start=(kc == 0), stop=(kc == 7))
                                for kc in range(8):
                                    S.mm(pg[:, :w], wab[:, kc, 1024 + cc * 128:1024 + (cc + 1) * 128], hT[:, kc, :w],
                                         start=(kc == 0), stop=(kc == 7))
                                s_ = sgt[cc % 2]
                                S.act(s_[:, :w], pg[:, :w], AF.Sigmoid)
                                dst = uext[:, cc, 15 + t0:15 + t0 + w] if t0 < TL else ucx[:, cc, 15:15 + TCX]
                                S.tt(dst, pu[:, :w], s_[:, :w], ALU.mult)
                        for i in range(2):
                            S.dma(pa_src[i].ap.re("p (g t) -> p g t", g=2), paT[:, 2 * i:2 * i + 2, 0:TL])
                            S.allgather(pa_dst[i], pa_src[i], groups)
                        for g in range(4):
                            for i in range(2):
                                p = PS[i]
                                S.mm(p[:, 0:256], paT[:, g, TL + 128 * i:TL + 128 * (i + 1)], fccs.ap)
                                S.cp(zc[:, i, :], p[:, 0:256], e="act")
                            p = PS[2 + g % 2]
                            for i in range(2):
                                S.mm(p[:, 0:256], zc[:, i, 0:128], c256[:, i, :], start=(i == 0), stop=False)
                                S.mm(p[:, 0:256], zc[:, i, 128:256], s256[:, i, :], start=False, stop=(i == 1))
                            S.cp(yac[:, g, :], p[:, 0:256])
                        S.barrier()
                    with ExitStack() as ph2:
                        hs = S.sb("hs", [128, 4, 2, 15], BF16, es=ph2)
                        S.cp(hs[:, :, 0, :], uext[:, :, 15:30], e="pool")
                        S.cp(hs[:, :, 1, :], uext[:, :, TL:TL + 15], e="pool")
                        S.dma(halo_src.ap.re("p (c s k) -> p c s k", c=4, s=2), hs.ap)
                        S.allgather(halo_dst, halo_src, groups)
                        hall = S.sb("hall", [128, 4, 4, 2, 15], BF16, es=ph2)
                        S.dma(hall.ap, halo_dst.ap.re("(r p) (c s k) -> p r c s k", p=128, c=4, s=2))
                        hm = S.sb("hm", [128, 8], F32, es=ph2)
                        S.dma(hm.ap, cin["k_hmask"].ap)
                        hl = S.sb("hl", [128, 4, 15], F32, es=ph2)
                        hr = S.sb("hr", [128, 4, 15], F32, es=ph2)
                        S.memset(hl.ap, 0.0)
                        S.memset(hr.ap, 0.0)
                        for r in range(4):
                            S.stt(hl.ap, hall[:, r, :, 1, :], hm[:, r:r + 1], hl.ap, ALU.mult, ALU.add)
                            S.stt(hr.ap, hall[:, r, :, 0, :], hm[:, 4 + r:5 + r], hr.ap, ALU.mult, ALU.add)
                        S.cp(uext[:, :, 0:15], hl.ap)
                        S.cp(uext[:, :, TL + 15:TL + 30], hr.ap)
                        cw = S.sb("cw", [128, 4, 31], F32, es=ph2)
                        cb = S.sb("cb", [128, 4], F32, es=ph2)
                        cg = S.sb("cg", [128, 4], F32, es=ph2)
                        S.dma(cw.ap, conv_wT[j])
                        S.dma(cb.ap, conv_bT[j])
                        S.dma(cg.ap, conv_gT[j])
                        dg = [S.sb("dg%d" % cc, [128, 31, 128], BF16, es=ph2) for cc in range(4)]
                        for cc in range(4):
                            for k in range(31):
                                S.ts(dg[cc][:, k, :], ident.ap, cw[:, cc, k:k + 1], None, ALU.mult,
                                     e=("dve" if k % 2 else "pool"))
                        vb = [S.sb("vb%d" % cc, [128, 512], F32, es=ph2) for cc in range(4)]
                        sqb = [S.sb("sqb%d" % i, [128, 512], BF16, es=ph2) for i in range(2)]
                        rsb = S.sb("rsb", [128, 512], F32, es=ph2)
                        for ci, (t0, w) in enumerate(CH):
                            ss = PS[6]
                            for cc in range(4):
                                p = PS[cc % 2]
                                for k in range(31):
                                    src = uext[:, cc, t0 + k:t0 + k + w] if t0 < TL else ucx[:, cc, k:k + w]
                                    S.mm(p[:, :w], dg[cc][:, k, :], src, start=(k == 0), stop=(k == 30))
                                S.act(vb[cc][:, :w], p[:, :w], AF.Identity, bias=cb[:, cc:cc + 1], scale=1.0)
                                s_ = sqb[cc % 2]
                                S.tt(s_[:, :w], vb[cc][:, :w], vb[cc][:, :w], ALU.mult, e="pool")
                                S.mm(ss[:, :w], ones_bf.ap, s_[:, :w], start=(cc == 0), stop=(cc == 3))
                            S.act(rsb[:, :w], ss[:, :w], AF.Sqrt, bias=eps_t.ap, scale=1.0 / 512)
                            S.recip(rsb[:, :w], rsb[:, :w])
                            for cc in range(4):
                                S.tt(vb[cc][:, :w], vb[cc][:, :w], rsb[:, :w], ALU.mult)
                                S.act(ybT[:, cc, t0:t0 + w], vb[cc][:, :w], AF.Silu, scale=cg[:, cc:cc + 1])
                        S.barrier()
                yal = S.sb("yal", [128, 4, TL], BF16, es=ph)
                with ExitStack() as ph2:
                    fccs = load_w(ph2, "fccs", cin["c_fccs"].ap, [128, 256])
                    rqre = load_w(ph2, "rqre", cin["c_rqre"].ap, [128, 4, 64])
                    rqim = load_w(ph2, "rqim", cin["c_rqim"].ap, [128, 4, 64])
                    f64c = load_w(ph2, "f64c", cin["k_f64c"].ap, [64, 16])
                    f64s = load_w(ph2, "f64s", cin["k_f64s"].ap, [64, 16])
                    twc = S.sb("twc", [64, 128], F32, es=ph2)
                    tws = S.sb("tws", [64, 128], F32, es=ph2)
                    S.dma(twc.ap, cin["c_twc"].ap)
                    S.dma(tws.ap, cin["c_tws"].ap)
                    paf = S.sb("paf", [128, 4 * TL], BF16, es=ph2)
                    Z = S.sb("Z", [128, 64, 256], BF16, es=ph2)
                    Vq = S.sb("Vq", [64, 128, 2, 32], BF16, es=ph2)
                    t4 = [S.sb("t4_%d" % i, [64, 8, 32], F32, es=ph2) for i in range(4)]
                    for g in range(4):
                        S.dma(paf.ap.re("p (r t) -> p r t", r=4),
                              pa_dst[g // 2].ap.re("(r p) (g t) -> p r g t", p=128, g=2)[:, :, g % 2, :])
                        for i in range(32):
                            p = PS[i % 2]
                            for h in range(2):
                                n2 = 2 * i + h
                                S.mm(p[:, 256 * h:256 * (h + 1)], paf[:, n2:4 * TL:64], fccs.ap)
                            S.cp(Z[:, 2 * i:2 * i + 2, :].re("p a b -> p (a b)"), p.ap,
                                 e=("act" if i % 2 else "dve"))
                        for qt in range(4):
                            for kb in range(16):
                                p = PS[2 + kb % 2]
                                for k8 in range(8):
                                    kc = kb * 8 + k8
                                    S.mm(p[0:64, 64 * k8:64 * (k8 + 1)], Z[:, :, kc], rqre[:, qt, :],
                                         start=True, stop=False)
                                    S.mm(p[0:64, 64 * k8:64 * (k8 + 1)], Z[:, :, 128 + kc], rqim[:, qt, :],
                                         start=False, stop=True)
                                U = p[0:64, :].re("p (k r a) -> p k r a", k=8, r=2)
                                tcv = twc[:, 32 * qt:32 * qt + 32].re("p (o a) -> p o a", o=1).bc([64, 8, 32])
                                tsv = tws[:, 32 * qt:32 * qt + 32].re("p (o a) -> p o a", o=1).bc([64, 8, 32])
                                S.tt(t4[0].ap, U[:, :, 0, :], tcv, ALU.mult)
                                S.tt(t4[1].ap, U[:, :, 1, :], tsv, ALU.mult)
                                S.tt(t4[2].ap, U[:, :, 0, :], tsv, ALU.mult)
                                S.tt(t4[3].ap, U[:, :, 1, :], tcv, ALU.mult)
                                S.tt(Vq[:, kb * 8:kb * 8 + 8, 0, :], t4[0].ap, t4[1].ap, ALU.subtract, e="pool")
                                S.tt(Vq[:, kb * 8:kb * 8 + 8, 1, :], t4[2].ap, t4[3].ap, ALU.add, e="pool")
                            p = PS[4 + qt % 2]
                            for a in range(32):
                                S.mm(p[:, 16 * a:16 * (a + 1)], Vq[:, :, 0, a], f64c.ap, start=True, stop=False)
                                S.mm(p[:, 16 * a:16 * (a + 1)], Vq[:, :, 1, a], f64s.ap, start=False, stop=True)
                            dst = yal[:, g, :].re("p (jj k) -> p k jj", k=128)[:, 32 * qt:32 * qt + 32, :]
                            S.cp(dst, p.ap.re("p (a jj) -> p a jj", a=32), e="act")
                    S.barrier()
                with ExitStack() as ph2:
                    wo = load_w(ph2, "wo", w_out_ab[j].re("(kc p) n -> p kc n", p=128), [128, 8, 1024])
                    for ci, (t0, w) in enumerate(CH):
                        st = 0 if t0 < TL else 1
                        for oc in range(8):
                            p = PS[oc % 4]
                            for kc in range(8):
                                if kc < 4:
                                    rhs = yal[:, kc, t0:t0 + w] if t0 < TL else yac[:, kc, :]
                                else:
                                    rhs = ybT[:, kc - 4, t0:t0 + w]
                                S.mm(p[:, :w], wo[:, kc, oc * 128:(oc + 1) * 128], rhs,
                                     start=(kc == 0), stop=(kc == 7))
                            S.stt(xv(oc, ci), p[:, :w], prm[:, 2, oc, st:st + 1], xv(oc, ci), ALU.mult, ALU.add)
                    S.barrier()

        def odd_phase(l):
            j = l // 2
            OK_, OKD, OV, OVD, OKR = 0, 4096, 12288, 16384, 24576
            with ExitStack() as ph:
                kctx = S.sb("kctx", [128, 2, TCX], BF16, es=ph)
                kdctx = S.sb("kdctx", [128, 4, TCX], BF16, es=ph)
                krctx = S.sb("krctx", [64, TCX], BF16, es=ph)
                vctx = S.sb("vctx", [128, 2, 256], BF16, es=ph)
                vdctx = S.sb("vdctx", [128, 2, 512], BF16, es=ph)
                with ExitStack() as ph2:
                    hTc = [S.sb("hTc%d" % i, [128, 8, 512], BF16, es=ph2) for i in range(2)]
                    wcd = load_w(ph2, "wcd", w_in_cd[j].re("(kc p) n -> p kc n", p=128), [128, 8, 1728])
                    wuq = load_w(ph2, "wuq", w_uq[j].re("(kc p) n -> p kc n", p=128), [128, 3, 768])
                    wukv = load_w(ph2, "wukv", w_ukv[j].re("(kc p) n -> p kc n", p=128), [128, 2, 1024])
                    wukvv = S.sb("wukvv", [128, 2, 4, 128], BF16, es=ph2)
                    for kc_ in range(2):
                        S.dma(wukvv[:, kc_], w_ukv[j].re("(kc p) (h two d) -> p kc h two d", p=128, h=4, two=2)[:, kc_, :, 1, :],
                              q="pool")
                    rot = load_w(ph2, "rot", cin["c_rot"].ap, [128, 128])
                    rc128 = S.sb("rc128", [128, 512], F32, es=ph2)
                    rs128 = S.sb("rs128", [128, 512], F32, es=ph2)
                    rc64 = S.sb("rc64", [64, 512], F32, es=ph2)
                    rs64 = S.sb("rs64", [64, 512], F32, es=ph2)
                    gq = S.sb("gq", [128, 8], F32, es=ph2)
                    S.dma(gq[:, 0:1], q_gT[j])
                    S.dma(gq[:, 1:2], k_gT[j])
                    S.dma(gq[:, 2:5], cq_gT[j])
                    S.dma(gq[:, 5:7], ckv_gT[j])
                    kst = S.sb("kst", [128, 6656], BF16, es=ph2)
                    LK, LKD, LKR, LV, LVD = 0, 1024, 3072, 3584, 4608
                    qst = S.sb("qst", [128, 12, 512], BF16, es=ph2)
                    sqb = [S.sb("sqb%d" % i, [128, 512], BF16, es=ph2) for i in range(2)]
                    rsb = S.sb("rsb", [128, 512], F32, es=ph2)
                    qn = [S.sb("qn%d" % i, [128, 512], BF16, es=ph2) for i in range(2)]
                    f1 = [S.sb("f1_%d" % i, [128, 512], F32, es=ph2) for i in range(2)]
                    f2 = [S.sb("f2_%d" % i, [128, 512], F32, es=ph2) for i in range(2)]
                    craw = S.sb("craw", [128, 3, 512], F32, es=ph2)
                    cn = S.sb("cn", [128, 3, 512], BF16, es=ph2)
                    kn = S.sb("kn", [128, 2, 512], BF16, es=ph2)
                    S.memset(qst.ap, 0.0, e="pool")
                    S.memset(kst.ap, 0.0, e="pool")
                    cnt = [0]

                    def rms_rope(p, w, t0, np_, gcol, dst, rope, inv_dim):
                        cnt[0] += 1
                        i = cnt[0] % 2
                        if gcol is not None:
                            S.act(sqb[i][:np_, :w], p, AF.Square)
                            ss = PS[6]
                            S.mm(ss[:np_, :w], ones_bf[:np_, :np_], sqb[i][:np_, :w])
                            S.act(rsb[:np_, :w], ss[:np_, :w], AF.Sqrt, bias=eps_t[:np_, :], scale=inv_dim)
                            S.recip(rsb[:np_, :w], rsb[:np_, :w])
                            S.tt(f1[i][:np_, :w], p, rsb[:np_, :w], ALU.mult)
                            tgt = qn[i][:np_, :w] if rope else dst
                            S.ts(tgt, f1[i][:np_, :w], gq[:np_, gcol:gcol + 1], None, ALU.mult)
                        else:
                            tgt = qn[i][:np_, :w] if rope else dst
                            S.cp(tgt, p, e="act")
                        if rope:
                            rp = PS[7]
                            S.mm(rp[:np_, :w], rot[:np_, :np_], qn[i][:np_, :w])
                            rc, rs_ = (rc128, rs128) if np_ == 128 else (rc64, rs64)
                            S.tt(f1[i][:np_, :w], qn[i][:np_, :w], rc[:np_, :w], ALU.mult, e="pool")
                            S.tt(f2[i][:np_, :w], rp[:np_, :w], rs_[:np_, :w], ALU.mult)
                            S.tt(dst, f1[i][:np_, :w], f2[i][:np_, :w], ALU.add, e="pool")

                    for ci in norm_mod(ph2, prm[:, 0], prm[:, 1],
                                       lambda ci, c: hTc[ci % 2][:, c, :CH[ci][1]], "n1"):
                        t0, w = CH[ci]
                        lat = t0 < TL
                        hT = hTc[ci % 2]
                        if lat:
                            S.dma(rc128.ap, cin["k_rc128"][:, t0:t0 + w])
                            S.dma(rs128.ap, cin["k_rs128"][:, t0:t0 + w], q="act")
                            S.dma(rc64.ap, cin["k_rc64"][:, t0:t0 + w])
                            S.dma(rs64.ap, cin["k_rs64"][:, t0:t0 + w], q="act")

                        def proj(pv, col0, ncol, wt=wcd, nk=8, src=None):
                            for kc in range(nk):
                                rhs = hT[:, kc, :w] if src is None else src[:, kc, :w]
                                S.mm(pv, wt[:, kc, col0:col0 + ncol], rhs, start=(kc == 0), stop=(kc == nk - 1))
                        for h in range(4):
                            p = PS[h % 2]
                            proj(p[:, :w], 128 * h, 128)
                            rms_rope(p[:, :w], w, t0, 128, 0, qst[:, h, :w], lat, 1.0 / 128)
                        for h in range(2):
                            p = PS[2 + h % 2]
                            proj(p[:, :w], 512 + 128 * h, 128)
                            dst = kst[:, LK + h * 512:LK + h * 512 + w] if lat else kctx[:, h, :]
                            rms_rope(p[:, :w], w, t0, 128, 1, dst, lat, 1.0 / 128)
                        for ti in range(w // 128):
                            p = PS[4 + ti % 2]
                            for kc in range(8):
                                S.mm(p[:, 0:256], hT[:, kc, ti * 128:(ti + 1) * 128], wcd[:, kc, 768:1024],
                                     start=(kc == 0), stop=(kc == 7))
                            tg = (t0 + ti * 128) // 128
                            dst = kst[:, LV + ti * 256:LV + (ti + 1) * 256] if lat else vctx[:, ti, :]
                            S.cp(dst, p[:, 0:256], e="act")
                        ss = PS[5]
                        for k3 in range(3):
                            p = PS[k3 % 2]
                            proj(p[:, :w], 1024 + 128 * k3, 128)
                            S.cp(craw[:, k3, :w], p[:, :w], e="act")
                            S.tt(sqb[k3 % 2][:, :w], craw[:, k3, :w], craw[:, k3, :w], ALU.mult, e="pool")
                            S.mm(ss[:, :w], ones_bf.ap, sqb[k3 % 2][:, :w], start=(k3 == 0), stop=(k3 == 2))
                        S.act(rsb[:, :w], ss[:, :w], AF.Sqrt, bias=eps_t.ap, scale=1.0 / 384)
                        S.recip(rsb[:, :w], rsb[:, :w])
                        for k3 in range(3):
                            S.tt(craw[:, k3, :w], craw[:, k3, :w], rsb[:, :w], ALU.mult)
                            S.ts(cn[:, k3, :w], craw[:, k3, :w], gq[:, 2 + k3:3 + k3], None, ALU.mult)
                        for h in range(4):
                            p = PS[h % 2]
                            proj(p[:, :w], 192 * h, 128, wt=wuq, nk=3, src=cn)
                            S.cp(qst[:, 4 + h, :w], p[:, :w], e="act")
                            p2 = PS[2 + h % 2]
                            proj(p2[0:64, :w], 192 * h + 128, 64, wt=wuq, nk=3, src=cn)
                            rms_rope(p2[0:64, :w], w, t0, 64, None, qst[0:64, 8 + h, :w], lat, None)
                        ss = PS[5]
                        for k2 in range(2):
                            p = PS[k2 % 2]
                            proj(p[:, :w], 1408 + 128 * k2, 128)
                            S.cp(craw[:, k2, :w], p[:, :w], e="act")
                            S.tt(sqb[k2 % 2][:, :w], craw[:, k2, :w], craw[:, k2, :w], ALU.mult, e="pool")
                            S.mm(ss[:, :w], ones_bf.ap, sqb[k2 % 2][:, :w], start=(k2 == 0), stop=(k2 == 1))
                        S.act(rsb[:, :w], ss[:, :w], AF.Sqrt, bias=eps_t.ap, scale=1.0 / 256)
                        S.recip(rsb[:, :w], rsb[:, :w])
                        for k2 in range(2):
                            S.tt(craw[:, k2, :w], craw[:, k2, :w], rsb[:, :w], ALU.mult)
                            S.ts(kn[:, k2, :w], craw[:, k2, :w], gq[:, 5 + k2:6 + k2], None, ALU.mult)
                        for h in range(4):
                            p = PS[h % 2]
                            proj(p[:, :w], 256 * h, 128, wt=wukv, nk=2, src=kn)
                            dst = kst[:, LKD + h * 512:LKD + h * 512 + w] if lat else kdctx[:, h, :]
                            S.cp(dst, p[:, :w], e="act")
                        for ti in range(w // 128):
                            p = PS[4 + ti % 2]
                            for k2 in range(2):
                                S.mm(p[:, 0:512], kn[:, k2, ti * 128:(ti + 1) * 128],
                                     wukvv[:, k2].re("p h d -> p (h d)"), start=(k2 == 0), stop=(k2 == 1))
                            tg = (t0 + ti * 128) // 128
                            dst = kst[:, LVD + ti * 512:LVD + (ti + 1) * 512] if lat else vdctx[:, ti, :]
                            S.cp(dst, p[:, 0:512])
                        p = PS[2]
                        proj(p[0:64, :w], 1664, 64)
                        dst = kst[0:64, LKR:LKR + w] if lat else krctx.ap
                        rms_rope(p[0:64, :w], w, t0, 64, None, dst, lat, None)
                        S.dma(q_scr[:, :, t0:t0 + w], qst[:, :, :w])
                        if lat:
                            nt_ = w // 128
                            tg0 = t0 // 128
                            for h in range(2):
                                S.dma(kvs_(OK_ + h * TL + t0, OK_ + h * TL + t0 + w), kst[:, LK + h * 512:LK + h * 512 + w])
                            for h in range(4):
                                S.dma(kvs_(OKD + h * TL + t0, OKD + h * TL + t0 + w),
                                      kst[:, LKD + h * 512:LKD + h * 512 + w], q="act")
                            S.dma(kvs_(OKR + t0, OKR + t0 + w), kst[:, LKR:LKR + w])
                            S.dma(kvs_(OV + tg0 * 256, OV + (tg0 + nt_) * 256), kst[:, LV:LV + nt_ * 256], q="act")
                            S.dma(kvs_(OVD + tg0 * 512, OVD + (tg0 + nt_) * 512), kst[:, LVD:LVD + nt_ * 512])
                    S.barrier()
                for i in range(7):
                    S.allgather(kv_dst[i], kv_src[i], groups)
                with ExitStack() as ph2:
                    wo = load_w(ph2, "wo", w_out_cd[j].re("(kc p) n -> p kc n", p=128), [128, 8, 1024])
                    NK = 66
                    Kf = S.sb("Kf", [128, NK * 128], BF16, es=ph2)
                    Krf = S.sb("Krf", [64, NK * 128], BF16, es=ph2)
                    Vf = S.sb("Vf", [128, NK, 128], BF16, es=ph2)
                    qh = S.sb("qh", [128, NT], BF16, es=ph2)
                    qrh = S.sb("qrh", [64, NT], BF16, es=ph2)
                    oh = S.sb("oh", [128, NT], BF16, es=ph2)
                    Pb = [S.sb("Pb%d" % i, [128, 512], BF16, es=ph2) for i in range(3)]
                    rd = S.sb("rd", [128, 512], F32, es=ph2)
                    dacc = S.sb("dacc", [128, 512], F32, es=ph2)
                    ones_f = S.sb("ones_f", [128, 128], F32, es=ph2)
                    S.memset(ones_f.ap, 1.0)
                    S.cp(Krf[:, 0:TCX], krctx.ap, e="pool")
                    S.dma(Krf[:, TCX:].re("p (r t) -> p r t", r=4), kvd_(OKR, OKR + TL, rows=64))
                    pcount = [0]

                    def attend(qv, qrv, keys, scale, ocol, wq):
                        pcount[0] += 1
                        o_ps = PS[4 + pcount[0] % 2]
                        d_ps = PS[6 + pcount[0] % 2]

                        def qk(kc):
                            s_ps = PS[kc % 4]
                            if qrv is None:
                                S.mm(s_ps[:, :wq], Kf[:, kc * 128:(kc + 1) * 128], qv)
                            else:
                                S.mm(s_ps[:, :wq], Kf[:, kc * 128:(kc + 1) * 128], qv, start=True, stop=False)
                                S.mm(s_ps[:, :wq], Krf[:, kc * 128:(kc + 1) * 128], qrv, start=False, stop=True)
                        for kc in range(min(2, keys)):
                            qk(kc)
                        for kc in range(keys):
                            if kc + 2 < keys:
                                qk(kc + 2)
                            P = Pb[kc % 3]
                            S.act(P[:, :wq], PS[kc % 4][:, :wq], AF.Exp, scale=scale)
                            S.mm(o_ps[:, :wq], Vf[:, kc, :], P[:, :wq], start=(kc == 0), stop=(kc == keys - 1))
                            if kc == 0:
                                S.cp(dacc[:, :wq], P[:, :wq])
                            else:
                                S.tt(dacc[:, :wq], dacc[:, :wq], P[:, :wq], ALU.add)
                        S.mm(d_ps[:, :wq], ones_f.ap, dacc[:, :wq])
                        S.recip(rd[:, :wq], d_ps[:, :wq])
                        S.tt(oh[:, ocol:ocol + wq], o_ps[:, :wq], rd[:, :wq], ALU.mult)

                    for hidx in range(8):
                        mla = hidx >= 4
                        h = hidx % 4
                        if not mla:
                            if h % 2 == 0:
                                kvh = h // 2
                                S.cp(Kf[:, 0:TCX], kctx[:, kvh, :], e="pool")
                                S.dma(Kf[:, TCX:].re("p (r t) -> p r t", r=4),
                                      kvd_(OK_ + kvh * TL, OK_ + (kvh + 1) * TL))
                                S.cp(Vf[:, 0:2, :], vctx.ap.re("p i (h d) -> p i h d", h=2)[:, :, kvh, :], e="pool")
                                for r_ in range(4):
                                    S.dma(Vf[:, 2 + 16 * r_:2 + 16 * (r_ + 1), :],
                                          kvd_(OV, OV + 4096).re("p r (i h d) -> p r i h d", i=16, h=2)[:, r_, :, kvh, :],
                                          q=("sp" if r_ % 2 == 0 else "act"))
                            S.dma(qh.ap, q_scr[:, h, :])
                            scale = 128.0 ** -0.5
                        else:
                            S.cp(Kf[:, 0:TCX], kdctx[:, h, :], e="pool")
                            S.dma(Kf[:, TCX:].re("p (r t) -> p r t", r=4),
                                  kvd_(OKD + h * TL, OKD + (h + 1) * TL))
                            S.cp(Vf[:, 0:2, :], vdctx.ap.re("p i (h d) -> p i h d", h=4)[:, :, h, :], e="pool")
                            for hf in range(2):
                                for r_ in range(4):
                                    S.dma(Vf[:, 2 + 16 * r_ + 8 * hf:2 + 16 * r_ + 8 * hf + 8, :],
                                          kvd_(OVD + 4096 * hf, OVD + 4096 * (hf + 1)).re("p r (i h d) -> p r i h d", i=8, h=4)[:, r_, :, h, :],
                                          q=("sp" if r_ % 2 == 0 else "act"))
                            S.dma(qh.ap, q_scr[:, 4 + h, :])
                            S.dma(qrh.ap, q_scr[0:64, 8 + h, :])
                            scale = 192.0 ** -0.5
                        for qb in range(4):
                            attend(qh[:, qb * 512:(qb + 1) * 512], qrh[:, qb * 512:(qb + 1) * 512] if mla else None,
                                   NK, scale, qb * 512, 512)
                        attend(qh[:, TL:NT], qrh[:, TL:NT] if mla else None, 2, scale, TL, TCX)
                        for ci, (t0, w) in enumerate(CH):
                            st = 0 if t0 < TL else 1
                            for oc in range(8):
                                p = PS[oc % 4]
                                S.mm(p[:, :w], wo[:, hidx, oc * 128:(oc + 1) * 128], oh[:, t0:t0 + w])
                                S.stt(xv(oc, ci), p[:, :w], prm[:, 2, oc, st:st + 1], xv(oc, ci), ALU.mult, ALU.add)
                    S.barrier()

        import os
        skip = os.environ.get("DEV_SKIP", "").split(",")
        for l in range(l0, n_layers):
            if "mod" not in skip:
                mod_phase(l)
            if "mix" not in skip:
                if l % 2 == 0:
                    even_phase(l)
                else:
                    odd_phase(l)
            if "moe" not in skip:
                moe_phase(l)
        if final:
            with ExitStack() as ph:
                gf = S.sb("gf", [128, 8], F32, es=ph)
                S.dma(gf.ap, gfinT.ap)
                of = S.sb("of", [128, 8, 512], F32, es=ph)
                for ci in norm_mod(ph, gf, None, lambda ci, c: of[:, c, :CH[ci][1]], "nf"):
                    t0, w = CH[ci]
                    if t0 < TL:
                        S.dma(y[:, :, t0:t0 + w], of[:, :, :w])
                S.barrier()
        else:
            for c in range(8):
                S.dma(y[:, c, :], xT[:, c, 0:TL])
        S.finish([y])
        print("ninst", S.ninst, flush=True)
    return nc


def _fm(a):
    t = a.shape[0]
    return np.ascontiguousarray(a.T.reshape(8, 128, t).transpose(1, 0, 2))


def _vecT(v, nchunk):
    return np.ascontiguousarray(np.swapaxes(v.reshape(v.shape[:-1] + (nchunk, 128)), -1, -2))


def make_in_maps(inp, n_layers=4):
    f = lambda k: np.ascontiguousarray(np.asarray(inp[k], dtype=np.float32))
    shared = {
        "mod_bT": _vecT(f("mod_b"), 48), "gmixT": _vecT(f("norm_mix_g"), 8),
        "gffnT": _vecT(f("norm_ffn_g"), 8), "gfinT": _vecT(f("final_norm_g"), 8),
        "w_in_ab": f("w_in_ab"),
        "conv_wT": np.ascontiguousarray(f("conv_w").transpose(0, 2, 1).reshape(2, 4, 128, 31).transpose(0, 2, 1, 3)),
        "conv_bT": _vecT(f("conv_b"), 4), "conv_gT": _vecT(f("conv_norm_g"), 4),
        "w_out_ab": f("w_out_ab"), "w_in_cd": f("w_in_cd"),
        "q_gT": _vecT(f("q_norm_g"), 1), "k_gT": _vecT(f("k_norm_g"), 1),
        "cq_gT": _vecT(f("cq_norm_g"), 3), "ckv_gT": _vecT(f("ckv_norm_g"), 2),
        "w_uq": f("w_uq"), "w_ukv": f("w_ukv"), "w_out_cd": f("w_out_cd"),
        "rw": np.ascontiguousarray(np.concatenate([f("router_grp_w"), f("router_exp_w")], -1)),
        "rb": np.ascontiguousarray(np.concatenate([f("router_grp_b"), f("router_exp_b")], -1)[:, None, :]),
    }
    for l in range(n_layers):
        shared["ewg%d" % l] = f("exp_w_gate")[l]
        shared["ewu%d" % l] = f("exp_w_up")[l]
        shared["ewd%d" % l] = f("exp_w_down")[l]
        shared["mod_w%d" % l] = f("mod_w")[l]
    shared.update(_consts_common())
    x = f("x")
    ctx = f("ctx")
    c = f("c")
    cc = f("c_ctx")
    maps = []
    for core in range(8):
        b, q = core // 4, core % 4
        m = dict(shared)
        xt = np.concatenate([x[b, 2048 * q:2048 * (q + 1)], ctx[b]], 0)
        m["x0"] = _fm(xt)
        m["scT"] = np.ascontiguousarray(np.stack([c[b], cc], 0).reshape(2, 8, 128).transpose(2, 1, 0))
        m.update(_consts_core(q))
        maps.append(m)
    return maps


def assemble(res):
    out = np.zeros((2, 8192, 1024), np.float32)
    for core in range(8):
        b, q = core // 4, core % 4
        yv = np.asarray(res[core]["y"])
        out[b, 2048 * q:2048 * (q + 1), :] = yv.transpose(2, 1, 0).reshape(2048, 1024)
    return out


_NC = {}


def kernel(**inputs):
    key = (4, True)
    if key not in _NC:
        _NC[key] = build(4, True)
    maps = make_in_maps(inputs)
    res = run_bass_kernel_spmd(_NC[key], maps, core_ids=list(range(8)))
    return assemble(res.results)
```

```python
import numpy as np
from contextlib import ExitStack
import concourse.bass as bass
import concourse.mybir as mybir
from concourse.bass_utils import run_bass_kernel_spmd

F32 = mybir.dt.float32
BF16 = mybir.dt.bfloat16
AF = mybir.ActivationFunctionType
ALU = mybir.AluOpType
AX = mybir.AxisListType

NT, TL, TCX = 2304, 2048, 256
CH = [(0, 512), (512, 512), (1024, 512), (1536, 512), (2048, 256)]
EPS = 1e-6


class Trk:
    __slots__ = ("lw", "rd")

    def __init__(self):
        self.lw = None
        self.rd = []


class T:
    def __init__(self, h, name=""):
        self.h = h
        self.name = name
        self.trk = Trk()
        self.subs = {}

    def __getitem__(self, idx):
        return V(self.trk, self.h[idx])

    @property
    def ap(self):
        return V(self.trk, self.h[:])

    def sv(self, key, idx):
        t = self.subs.get(key)
        if t is None:
            t = self.subs[key] = Trk()
        return V(t, self.h[idx])


class V:
    __slots__ = ("t", "a")

    def __init__(self, t, a):
        self.t = t
        self.a = a

    def __getitem__(self, idx):
        return V(self.t, self.a[idx])

    def re(self, pat, **kw):
        return V(self.t, self.a.rearrange(pat, **kw))

    def bc(self, shape):
        return V(self.t, self.a.broadcast_to(list(shape)))


class Sched:
    NDMA = 24
    NSW = 16

    def __init__(self, nc, es):
        self.nc = nc
        self.es = es
        self.engs = {"pe": nc.tensor, "act": nc.scalar, "dve": nc.vector, "pool": nc.gpsimd, "sp": nc.sync}
        self.sem = {}
        self.cnt = {}
        self.es0 = es
        for k in list(self.engs) + ["d%d" % i for i in range(self.NDMA)] + ["cc"]:
            self.sem[k] = es.enter_context(nc.semaphore("s_" + k))
            self.cnt[k] = 0
        self.dma_rr = 0
        self.sw_rr = 0
        self.gen = {}
        self.cons = {}
        self.pend = {e: [] for e in self.engs}
        self.seen = {e: {} for e in self.engs}
        self.ninst = 0
        self.uid = 0

    def sb(self, name, shape, dt=F32, es=None):
        self.uid += 1
        h = (es or self.es).enter_context(self.nc.sbuf_tensor("%s_%d" % (name, self.uid), list(shape), dt))
        return T(h, name)

    def ps(self, name, shape, dt=F32):
        h = self.es.enter_context(self.nc.psum_tensor(name, list(shape), dt))
        return T(h, name)

    def dram(self, name, shape, dt, kind="Internal"):
        return T(self.nc.dram_tensor(name, list(shape), dt, kind=kind), name)

    def _wait(self, e, tok):
        if tok is None:
            return
        if len(tok) == 3:
            k, v, g = tok
            if g != self.gen[k]:
                return
        else:
            k, v = tok
        if self.seen[e].get(k, 0) >= v:
            return
        if k == e and e == "pe":
            return
        self.engs[e].wait_ge(self.sem[k], v)
        self.seen[e][k] = v
        self.ninst += 1
        if len(tok) == 3:
            self.pend[e].append(k)

    def _flush_pend(self, e, tok):
        if self.pend[e]:
            for k in self.pend[e]:
                self.cons[k].append(tok)
            self.pend[e] = []

    def _deps(self, e, reads, writes):
        for r in reads:
            self._wait(e, r.t.lw)
        for w in writes:
            self._wait(e, w.t.lw)
            for tok in w.t.rd:
                self._wait(e, tok)

    def _commit(self, tok, reads, writes):
        for r in reads:
            rd = r.t.rd
            rd.append(tok)
            if len(rd) > 48:
                best = {}
                for k, v in rd:
                    if best.get(k, 0) < v:
                        best[k] = v
                r.t.rd = list(best.items())
        for w in writes:
            w.t.lw = tok
            w.t.rd = []

    def op(self, e, fn, reads, writes):
        reads = [r for r in reads if isinstance(r, V)]
        self._deps(e, reads, writes)
        ins = fn(self.engs[e])
        self.cnt[e] += 1
        ins.then_inc(self.sem[e], 1)
        self._commit((e, self.cnt[e]), reads, writes)
        self._flush_pend(e, (e, self.cnt[e]))
        self.ninst += 1
        return ins

    def dma_sw(self, out, in_):
        k = "w%d" % self.sw_rr
        self.sw_rr += 1
        self.sem[k] = self.es0.enter_context(self.nc.semaphore("s_" + k))
        self.cnt[k] = 0
        self._deps("pool", [in_], [out])
        ins = self.nc.gpsimd.dma_start(out=out.a, in_=in_.a)
        self.cnt[k] = 16
        ins.then_inc(self.sem[k], 16)
        tok = (k, 16)
        self._commit(tok, [in_], [out])
        self.ninst += 1

    def dma(self, out, in_, q="sp"):
        if q == "pool":
            return self.dma_sw(out, in_)
        k = "d%d" % self.dma_rr
        self.dma_rr = (self.dma_rr + 1) % self.NDMA
        if self.cnt[k] > 0:
            self._wait(q, (k, self.cnt[k]))
        self._deps(q, [in_], [out])
        ins = self.engs[q].dma_start(out=out.a, in_=in_.a)
        self.cnt[k] += 16
        ins.then_inc(self.sem[k], 16)
        self._commit((k, self.cnt[k]), [in_], [out])
        self._flush_pend(q, (k, self.cnt[k]))
        self.ninst += 1

    def allgather(self, dst, src, groups):
        self._deps("pool", [src.ap], [dst.ap])
        ins = self.nc.gpsimd.collective_compute("AllGather", ALU.bypass, replica_groups=groups,
                                                ins=[src.h.ap()], outs=[dst.h.ap()])
        self.cnt["cc"] += 1
        ins.then_inc(self.sem["cc"], 1)
        self._commit(("cc", self.cnt["cc"]), [src.ap], [dst.ap])
        self.ninst += 1

    def barrier(self):
        for e in self.engs:
            for k, c in self.cnt.items():
                if c > 0:
                    self._wait(e, (k, c, self.gen[k]) if k in self.gen else (k, c))

    def finish(self, outs):
        for o in outs:
            self._wait("sp", o.trk.lw)
        self.barrier()

    def mm(self, out, lhsT, rhs, start=True, stop=True, **kw):
        return self.op("pe", lambda E: E.matmul(out.a, lhsT.a, rhs.a, start=start, stop=stop, **kw),
                       [lhsT, rhs] + ([] if start else [out]), [out])

    def tr(self, out, in_, ident):
        return self.op("pe", lambda E: E.transpose(out.a, in_.a, ident.a), [in_, ident], [out])

    def act(self, out, in_, func, bias=None, scale=None, accum=None):
        kw = {}
        rd = [in_]
        wr = [out]
        if bias is not None:
            kw["bias"] = bias.a if isinstance(bias, V) else bias
            rd.append(bias)
        if scale is not None:
            kw["scale"] = scale.a if isinstance(scale, V) else scale
            rd.append(scale)
        if accum is not None:
            kw["accum_out"] = accum.a
            wr.append(accum)
        return self.op("act", lambda E: E.activation(out.a, in_.a, func, **kw), rd, wr)

    def tt(self, out, a, b, op, e="dve"):
        return self.op(e, lambda E: E.tensor_tensor(out.a, a.a, b.a, op), [a, b], [out])

    def ts(self, out, a, s1, s2, op0, op1=None, e="dve"):
        g = lambda s: s.a if isinstance(s, V) else s
        if op1 is None:
            return self.op(e, lambda E: E.tensor_scalar(out.a, a.a, g(s1), None, op0), [a, s1], [out])
        return self.op(e, lambda E: E.tensor_scalar(out.a, a.a, g(s1), g(s2), op0, op1), [a, s1, s2], [out])

    def stt(self, out, a, s, b, op0, op1, e="dve"):
        g = lambda x: x.a if isinstance(x, V) else x
        return self.op(e, lambda E: E.scalar_tensor_tensor(out.a, a.a, g(s), b.a, op0, op1), [a, s, b], [out])

    def cp(self, out, in_, e="dve"):
        if e == "act":
            return self.op(e, lambda E: E.copy(out.a, in_.a), [in_], [out])
        return self.op(e, lambda E: E.tensor_copy(out.a, in_.a), [in_], [out])

    def red(self, out, in_, op, e="dve"):
        return self.op(e, lambda E: E.tensor_reduce(out.a, in_.a, AX.X, op), [in_], [out])

    def memset(self, out, val, e="dve"):
        return self.op(e, lambda E: E.memset(out.a, val), [], [out])

    def recip(self, out, in_):
        return self.op("dve", lambda E: E.reciprocal(out.a, in_.a), [in_], [out])


def _consts_common():
    c = np.arange(128)
    Fc = np.exp(-2j * np.pi * np.outer(c, c) / 128) / np.sqrt(128)
    FcCS = np.concatenate([Fc.real, Fc.imag], 1)
    F128 = np.exp(-2j * np.pi * np.outer(c, c) / 128)
    R_re = np.concatenate([F128.real, F128.imag], 1)
    R_im = np.concatenate([-F128.imag, F128.real], 1)
    q4 = lambda R: np.stack([np.concatenate([R[:, 32 * t:32 * t + 32], R[:, 128 + 32 * t:160 + 32 * t]], 1)
                             for t in range(4)], 1)
    n2 = np.arange(64)
    Tw = np.exp(-2j * np.pi * np.outer(n2, c) / 8192)
    n = np.arange(256)
    F256 = np.exp(-2j * np.pi * np.outer(n, n) / 256) / 16.0
    t256 = lambda M: M.reshape(2, 128, 256).transpose(1, 0, 2)
    rot128 = np.zeros((128, 128))
    for i in range(64):
        rot128[2 * i, 2 * i + 1] = 1.0
        rot128[2 * i + 1, 2 * i] = -1.0
    sel = np.zeros((32, 32, 128))
    for e in range(32):
        sel[e, e, :] = 1.0
    d = {"c_ident": np.eye(128), "c_fccs": FcCS, "c_rqre": q4(R_re), "c_rqim": q4(R_im),
         "c_twc": Tw.real, "c_tws": Tw.imag, "c_c256": t256(F256.real), "c_s256": t256(-F256.imag),
         "c_rot": rot128, "c_sel": sel}
    return {k: np.ascontiguousarray(v, dtype=np.float32) for k, v in d.items()}


def _consts_core(q):
    n2 = np.arange(64)
    F64 = np.exp(-2j * np.pi * np.outer(n2, np.arange(64)) / 64) / np.sqrt(8192.0)
    sl = slice(16 * q, 16 * q + 16)
    n = 2048 * q + np.arange(2048)
    row = (n // 64).astype(np.float64)
    col = (n % 64).astype(np.float64)

    def tables(dim):
        nf = dim // 4
        inv = 10000.0 ** (-np.arange(nf, dtype=np.float64) / nf)
        ang = np.concatenate([row[:, None] * inv[None, :], col[:, None] * inv[None, :]], -1)
        ang = np.repeat(ang, 2, axis=1).T
        return np.cos(ang), np.sin(ang)
    c128, s128 = tables(128)
    c64, s64 = tables(64)
    hm = np.zeros((128, 8))
    if q > 0:
        hm[:, q - 1] = 1.0
    if q < 3:
        hm[:, 4 + q + 1] = 1.0
    d = {"k_f64c": F64.real[:, sl], "k_f64s": -F64.imag[:, sl], "k_rc128": c128, "k_rs128": s128,
         "k_rc64": c64, "k_rs64": s64, "k_hmask": hm}
    return {k: np.ascontiguousarray(v, dtype=np.float32) for k, v in d.items()}


def build(n_layers=4, final=True, l0=0):
    nc = bass.Bass("TRN2", target_bir_lowering=False)
    groups = [[0, 1, 2, 3], [4, 5, 6, 7]]
    with ExitStack() as es:
        S = Sched(nc, es)
        din = lambda name, shape: S.dram(name, shape, F32, "ExternalInput")
        x0 = din("x0", [128, 8, NT])
        scT = din("scT", [128, 8, 2])
        mod_w = [din("mod_w%d" % l, [1024, 6144]) for l in range(n_layers)]
        mod_bT = din("mod_bT", [4, 128, 48])
        gmixT = din("gmixT", [4, 128, 8])
        gffnT = din("gffnT", [4, 128, 8])
        gfinT = din("gfinT", [128, 8])
        w_in_ab = din("w_in_ab", [2, 1024, 1536])
        conv_wT = din("conv_wT", [2, 128, 4, 31])
        conv_bT = din("conv_bT", [2, 128, 4])
        conv_gT = din("conv_gT", [2, 128, 4])
        w_out_ab = din("w_out_ab", [2, 1024, 1024])
        w_in_cd = din("w_in_cd", [2, 1024, 1728])
        q_gT = din("q_gT", [2, 128, 1])
        k_gT = din("k_gT", [2, 128, 1])
        cq_gT = din("cq_gT", [2, 128, 3])
        ckv_gT = din("ckv_gT", [2, 128, 2])
        w_uq = din("w_uq", [2, 384, 768])
        w_ukv = din("w_ukv", [2, 256, 1024])
        w_out_cd = din("w_out_cd", [2, 1024, 1024])
        rw = din("rw", [4, 1024, 36])
        rb = din("rb", [4, 1, 36])
        ewg = [din("ewg%d" % l, [32, 1024, 512]) for l in range(n_layers)]
        ewu = [din("ewu%d" % l, [32, 1024, 512]) for l in range(n_layers)]
        ewd = [din("ewd%d" % l, [32, 512, 1024]) for l in range(n_layers)]
        cin = {}
        for nm, shp in [("c_ident", [128, 128]), ("c_fccs", [128, 256]), ("c_rqre", [128, 4, 64]),
                        ("c_rqim", [128, 4, 64]), ("c_twc", [64, 128]), ("c_tws", [64, 128]),
                        ("c_c256", [128, 2, 256]), ("c_s256", [128, 2, 256]), ("c_rot", [128, 128]),
                        ("c_sel", [32, 32, 128]), ("k_f64c", [64, 16]), ("k_f64s", [64, 16]),
                        ("k_rc128", [128, 2048]), ("k_rs128", [128, 2048]), ("k_rc64", [64, 2048]),
                        ("k_rs64", [64, 2048]), ("k_hmask", [128, 8])]:
            cin[nm] = din(nm, shp)
        y = S.dram("y", [128, 8, TL], F32, "ExternalOutput")
        pa_src = [S.dram("pa_src%d" % i, [128, 2 * TL], BF16) for i in range(2)]
        pa_dst = [S.dram("pa_dst%d" % i, [512, 2 * TL], BF16) for i in range(2)]
        halo_src = S.dram("halo_src", [128, 120], BF16)
        halo_dst = S.dram("halo_dst", [512, 120], BF16)
        XC = 26624
        KVW = [4096] * 6 + [2048]
        kv_src = [S.dram("kv_src%d" % i, [128, KVW[i]], BF16) for i in range(7)]
        kv_dst = [S.dram("kv_dst%d" % i, [512, KVW[i]], BF16) for i in range(7)]

        def kvs_(c0, c1):
            pi = c0 // 4096
            assert (c1 - 1) // 4096 == pi
            return kv_src[pi][:, c0 - 4096 * pi:c1 - 4096 * pi]

        def kvd_(c0, c1, rows=128):
            pi = c0 // 4096
            assert (c1 - 1) // 4096 == pi
            return kv_dst[pi].ap.re("(r p) c -> p r c", p=128)[0:rows, :, c0 - 4096 * pi:c1 - 4096 * pi]
        wt_scr = S.dram("wt_scr", [32, NT], F32)
        q_scr = S.dram("q_scr", [128, 12, NT], BF16)

        xT = S.sb("xT", [128, 8, NT], F32)
        PS = [S.ps("ps%d" % i, [128, 512], F32) for i in range(8)]
        ident = S.sb("ident", [128, 128], F32)
        ident_bf = S.sb("ident_bf", [128, 128], BF16)
        ones_bf = S.sb("ones_bf", [128, 128], BF16)
        eps_t = S.sb("eps_t", [128, 1], F32)
        sc_t = S.sb("sc_t", [128, 8, 2], F32)
        modv = S.sb("modv", [128, 48, 2], F32)
        prm = S.sb("prm", [128, 6, 8, 2], F32)
        gtmp = S.sb("gtmp", [128, 16], F32)
        S.dma(ident.ap, cin["c_ident"].ap)
        S.dma(ident_bf.ap, cin["c_ident"].ap, q="pool")
        S.memset(ones_bf.ap, 1.0)
        S.memset(eps_t.ap, EPS)
        S.dma(sc_t.ap, scT.ap)
        S.act(sc_t.ap, sc_t.ap, AF.Silu)
        for c in range(8):
            S.dma(xT[:, c, :], x0[:, c, :], q=("sp" if c % 2 == 0 else "act"))
        S.barrier()

        def xv(c, ci):
            t0, w = CH[ci]
            return xT.sv((c, ci), (slice(None), c, slice(t0, t0 + w)))

        def mod_phase(l):
            with ExitStack() as ph:
                wbuf = [S.sb("modw%d" % i, [128, 3072], F32, es=ph) for i in range(2)]
                mb = S.sb("modb", [128, 48], F32, es=ph)
                gm = S.sb("gm", [128, 16], F32, es=ph)
                S.dma(mb.ap, mod_bT[l])
                S.dma(gm[:, 0:8], gmixT[l])
                S.dma(gm[:, 8:16], gffnT[l])
                mps = PS[7]
                first = True
                i = 0
                for kc in range(8):
                    for hf in range(2):
                        wb = wbuf[i % 2]
                        i += 1
                        S.dma(wb.ap, mod_w[l][kc * 128:(kc + 1) * 128, hf * 3072:(hf + 1) * 3072],
                              q=("sp" if i % 2 else "act"))
                        for j in range(24):
                            jj = hf * 24 + j
                            S.mm(mps[:, 2 * jj:2 * jj + 2], wb[:, j * 128:(j + 1) * 128], sc_t[:, kc, :],
                                 start=first, stop=(kc == 7 and jj == 47), skip_group_check=True)
                            first = False
                S.tt(modv.ap, mps[:, 0:96].re("p (j s) -> p j s", s=2),
                     mb.ap.re("p (j o) -> p j o", o=1).bc([128, 48, 2]), ALU.add)
                for half, (ish, isc, ig) in enumerate([(0, 1, 2), (3, 4, 5)]):
                    gv = gm[:, 8 * half:8 * half + 8].re("p (c o) -> p c o", o=1).bc([128, 8, 2])
                    A = prm[:, 3 * half + 0]
                    S.ts(A, modv[:, 8 * isc:8 * isc + 8, :], 1.0, None, ALU.add)
                    S.tt(A, A, gv, ALU.mult)
                    S.cp(prm[:, 3 * half + 1], modv[:, 8 * ish:8 * ish + 8, :])
                    S.cp(prm[:, 3 * half + 2], modv[:, 8 * ig:8 * ig + 8, :])
                S.barrier()

        def norm_mod(ph, A, B, out_fn, name):
            sq = [S.sb(name + "sq%d" % i, [128, 512], BF16, es=ph) for i in range(2)]
            rs = S.sb(name + "rs", [128, 512], F32, es=ph)
            tmp = [S.sb(name + "tmp%d" % i, [128, 512], F32, es=ph) for i in range(2)]
            for ci, (t0, w) in enumerate(CH):
                st = 0 if t0 < TL else 1
                ss = PS[6]
                for c in range(8):
                    s_ = sq[c % 2]
                    S.act(s_[:, :w], xv(c, ci), AF.Square)
                    S.mm(ss[:, :w], ones_bf.ap, s_[:, :w], start=(c == 0), stop=(c == 7))
                S.act(rs[:, :w], ss[:, :w], AF.Sqrt, bias=eps_t.ap, scale=1.0 / 1024)
                S.recip(rs[:, :w], rs[:, :w])
                for c in range(8):
                    t_ = tmp[c % 2]
                    S.tt(t_[:, :w], xv(c, ci), rs[:, :w], ALU.mult, e="pool")
                    if B is None:
                        S.ts(out_fn(ci, c), t_[:, :w], A[:, c:c + 1], None, ALU.mult)
                    else:
                        S.ts(out_fn(ci, c), t_[:, :w], A[:, c, st:st + 1], B[:, c, st:st + 1], ALU.mult, ALU.add)
                yield ci

        def load_w(ph, name, src_view, shape, q="pool"):
            t = S.sb(name, shape, BF16, es=ph)
            S.dma(t.ap, src_view, q=q)
            return t

        def moe_phase(l):
            with ExitStack() as ph:
                h2T = S.sb("h2T", [128, 8, NT], BF16, es=ph)
                with ExitStack() as ph2:
                    WT = S.sb("WT", [32, NT], F32, es=ph2)
                    h2f = S.sb("h2f", [128, 8, 512], F32, es=ph2)
                    rwt = S.sb("rwt", [128, 8, 36], F32, es=ph2)
                    rbt = S.sb("rbt", [128, 36], F32, es=ph2)
                    S.dma(rwt.ap, rw[l].re("(kc p) n -> p kc n", p=128))
                    S.dma(rbt.ap, rb[l].bc([128, 36]))
                    sm = S.sb("sm", [128, 160], F32, es=ph2)
                    for ci in norm_mod(ph2, prm[:, 3], prm[:, 4], lambda ci, c: h2f[:, c, :CH[ci][1]], "n2"):
                        t0, w = CH[ci]
                        for c in range(8):
                            S.cp(h2T[:, c, t0:t0 + w], h2f[:, c, :w], e="act")
                        for ti in range(w // 128):
                            lg_ps = PS[5]
                            for kc in range(8):
                                S.mm(lg_ps[:, 0:36], h2f[:, kc, ti * 128:(ti + 1) * 128], rwt[:, kc, :],
                                     start=(kc == 0), stop=(kc == 7))
                            lg = sm[:, 0:36]
                            S.tt(lg, lg_ps[:, 0:36], rbt.ap, ALU.add)
                            gmax = sm[:, 36:37]
                            S.red(gmax, sm[:, 0:4], ALU.max)
                            goh = sm[:, 40:44]
                            S.ts(goh, sm[:, 0:4], gmax, None, ALU.is_equal)
                            ngm = sm[:, 37:38]
                            S.ts(ngm, gmax, -1.0, None, ALU.mult)
                            gsum = sm[:, 38:39]
                            S.memset(gsum, 0.0)
                            S.act(sm[:, 44:48], sm[:, 0:4], AF.Exp, bias=ngm, scale=1.0, accum=gsum)
                            gprob = sm[:, 39:40]
                            S.recip(gprob, gsum)
                            em = sm[:, 48:80]
                            S.tt(em.re("p (g e) -> p g e", g=4), sm[:, 4:36].re("p (g e) -> p g e", g=4),
                                 goh.re("p (g o) -> p g o", o=1).bc([128, 4, 8]), ALU.mult)
                            esel = sm[:, 80:88]
                            S.red(esel, em.re("p (g e) -> p e g", g=4), ALU.add)
                            m1 = sm[:, 88:89]
                            S.red(m1, esel, ALU.max)
                            oh1 = sm[:, 96:104]
                            S.ts(oh1, esel, m1, None, ALU.is_equal)
                            es2 = sm[:, 104:112]
                            S.stt(es2, oh1, -1e30, esel, ALU.mult, ALU.add)
                            m2 = sm[:, 89:90]
                            S.red(m2, es2, ALU.max)
                            oh2 = sm[:, 112:120]
                            S.ts(oh2, es2, m2, None, ALU.is_equal)
                            dd = sm[:, 90:91]
                            S.tt(dd, m2, m1, ALU.subtract)
                            ee = sm[:, 91:92]
                            S.act(ee, dd, AF.Exp)
                            S.ts(ee, ee, 1.0, None, ALU.add)
                            w1 = sm[:, 92:93]
                            S.recip(w1, ee)
                            S.tt(w1, w1, gprob, ALU.mult)
                            w2 = sm[:, 93:94]
                            S.tt(w2, gprob, w1, ALU.subtract)
                            wsel = sm[:, 120:128]
                            S.ts(wsel, oh1, w1, None, ALU.mult)
                            S.stt(wsel, oh2, w2, wsel, ALU.mult, ALU.add)
                            wf = sm[:, 128:160]
                            S.tt(wf.re("p (g e) -> p g e", g=4), goh.re("p (g o) -> p g o", o=1).bc([128, 4, 8]),
                                 wsel.re("p (o e) -> p o e", o=1).bc([128, 4, 8]), ALU.mult)
                            tp = PS[4]
                            S.tr(tp[0:32, 0:128], wf, ident.ap)
                            S.cp(WT[:, t0 + ti * 128:t0 + (ti + 1) * 128], tp[0:32, 0:128], e="act")
                    S.dma(wt_scr.ap, WT.ap)
                    S.barrier()
                wg = [S.sb("wg%d" % i, [128, 8, 512], BF16, es=ph) for i in range(2)]
                wu = [S.sb("wu%d" % i, [128, 8, 512], BF16, es=ph) for i in range(2)]
                wd = [S.sb("wd%d" % i, [128, 4, 1024], BF16, es=ph) for i in range(2)]
                stg = [S.sb("stg%d" % i, [128, 2048], F32, es=ph) for i in range(2)]
                wbs = [S.sb("wbs%d" % i, [128, 512], F32, es=ph) for i in range(2)]
                sg = [S.sb("sg%d" % i, [128, 512], F32, es=ph) for i in range(2)]
                hh = [S.sb("hh%d" % i, [128, 4, 512], BF16, es=ph) for i in range(2)]
                ucnt = [0]

                def unit(e, u):
                    b = e % 2
                    if u < 2:
                        return (ewg[l][e].re("(kc p) n -> p kc n", p=128)[:, 4 * u:4 * u + 4, :],
                                wg[b][:, 4 * u:4 * u + 4, :], 4)
                    if u < 4:
                        return (ewu[l][e].re("(kc p) n -> p kc n", p=128)[:, 4 * (u - 2):4 * (u - 2) + 4, :],
                                wu[b][:, 4 * (u - 2):4 * (u - 2) + 4, :], 4)
                    return (ewd[l][e].re("(kc p) n -> p kc n", p=128)[:, 2 * (u - 4):2 * (u - 4) + 2, :],
                            wd[b][:, 2 * (u - 4):2 * (u - 4) + 2, :], 2)
                ucnt[0] = 0
                for u in range(6):
                    src, dst, kc = unit(0, u)
                    st_ = stg[u % 2]
                    S.dma(st_.ap.re("p (kc n) -> p kc n", kc=kc), src, q="sp")
                    S.cp(dst, st_.ap.re("p (kc n) -> p kc n", kc=kc), e=("act" if u % 2 == 0 else "pool"))
                import os as _os
                NEXP = int(_os.environ.get("DEV_NEXP", "32"))
                ub = [S.sb("ub%d" % i, [128, 512], F32, es=ph) for i in range(2)]
                NCH = len(CH) if l < 3 else len(CH) - 1
                steps = [(e, ci) for e in range(NEXP) for ci in range(NCH)]

                def dma_u(e1, u):
                    src, dst, kc = unit(e1, u)
                    S.dma(stg[u % 2].ap.re("p (kc n) -> p kc n", kc=kc), src, q="sp")

                def cast_u(e1, u):
                    src, dst, kc = unit(e1, u)
                    S.cp(dst, stg[u % 2].ap.re("p (kc n) -> p kc n", kc=kc), e=("act" if u % 2 == 0 else "pool"))

                def front(si):
                    e, ci = steps[si]
                    t0, w = CH[ci]
                    b = e % 2
                    wb = wbs[si % 2]
                    S.dma(wb[:, :w], wt_scr[e:e + 1, t0:t0 + w].bc([128, w]))
                    hb = hh[si % 2]
                    for j in range(4):
                        g_ps = PS[j % 2]
                        u_ps = PS[2 + j % 2]
                        for kc in range(8):
                            S.mm(g_ps[:, :w], wg[b][:, kc, j * 128:(j + 1) * 128], h2T[:, kc, t0:t0 + w],
                                 start=(kc == 0), stop=(kc == 7))
                        for kc in range(8):
                            S.mm(u_ps[:, :w], wu[b][:, kc, j * 128:(j + 1) * 128], h2T[:, kc, t0:t0 + w],
                                 start=(kc == 0), stop=(kc == 7))
                        s_ = sg[j % 2]
                        S.act(s_[:, :w], g_ps[:, :w], AF.Silu)
                        u_ = ub[j % 2]
                        S.cp(u_[:, :w], u_ps[:, :w], e="act")
                        S.tt(u_[:, :w], u_[:, :w], s_[:, :w], ALU.mult, e="pool")
                        S.tt(hb[:, j, :w], u_[:, :w], wb[:, :w], ALU.mult, e="pool")

                def back(si):
                    e, ci = steps[si]
                    t0, w = CH[ci]
                    st = 0 if t0 < TL else 1
                    b = e % 2
                    hb = hh[si % 2]
                    for oc in range(8):
                        d_ps = PS[4 + oc % 4]
                        for j in range(4):
                            S.mm(d_ps[:, :w], wd[b][:, j, oc * 128:(oc + 1) * 128], hb[:, j, :w],
                                 start=(j == 0), stop=(j == 3))
                        S.stt(xv(oc, ci), d_ps[:, :w], prm[:, 5, oc, st:st + 1], xv(oc, ci), ALU.mult, ALU.add)
                    if e + 1 < NEXP and NCH == 5:
                        cast_u(e + 1, ci)
                        if ci + 2 < 6:
                            dma_u(e + 1, ci + 2)
                        if ci == 4:
                            cast_u(e + 1, 5)
                            if e + 2 < NEXP:
                                dma_u(e + 2, 0)
                                dma_u(e + 2, 1)
                    elif e + 1 < NEXP:
                        if ci == 0:
                            cast_u(e + 1, 0); dma_u(e + 1, 2); cast_u(e + 1, 1); dma_u(e + 1, 3)
                        elif ci == 1:
                            cast_u(e + 1, 2); dma_u(e + 1, 4); cast_u(e + 1, 3); dma_u(e + 1, 5)
                        elif ci == 2:
                            cast_u(e + 1, 4); cast_u(e + 1, 5)
                        elif e + 2 < NEXP:
                            dma_u(e + 2, 0); dma_u(e + 2, 1)
                if NEXP > 1:
                    dma_u(1, 0)
                    dma_u(1, 1)
                front(0)
                for si in range(len(steps)):
                    if si + 1 < len(steps):
                        front(si + 1)
                    back(si)
                S.barrier()

        def even_phase(l):
            j = l // 2
            with ExitStack() as ph:
                yac = S.sb("yac", [128, 4, TCX], BF16, es=ph)
                ybT = S.sb("ybT", [128, 4, NT], BF16, es=ph)
                with ExitStack() as pu_:
                    uext = S.sb("uext", [128, 4, TL + 30], BF16, es=pu_)
                    ucx = S.sb("ucx", [128, 4, TCX + 30], BF16, es=pu_)
                    S.memset(ucx.ap, 0.0, e="pool")
                    with ExitStack() as ph2:
                        paT = S.sb("paT", [128, 4, NT], BF16, es=ph2)
                        hTc = [S.sb("hTc%d" % i, [128, 8, 512], BF16, es=ph2) for i in range(2)]
                        wab = load_w(ph2, "wab", w_in_ab[j].re("(kc p) n -> p kc n", p=128), [128, 8, 1536])
                        sgt = [S.sb("sgt%d" % i, [128, 512], F32, es=ph2) for i in range(2)]
                        fccs = load_w(ph2, "fccs", cin["c_fccs"].ap, [128, 256])
                        c256 = load_w(ph2, "c256", cin["c_c256"].ap, [128, 2, 256])
                        s256 = load_w(ph2, "s256", cin["c_s256"].ap, [128, 2, 256])
                        zc = S.sb("zc", [128, 2, 256], BF16, es=ph2)
                        for ci in norm_mod(ph2, prm[:, 0], prm[:, 1],
                                           lambda ci, c: hTc[ci % 2][:, c, :CH[ci][1]], "n1"):
                            t0, w = CH[ci]
                            hT = hTc[ci % 2]
                            for g in range(4):
                                p = PS[g % 2]
                                for kc in range(8):
                                    S.mm(p[:, :w], wab[:, kc, g * 128:(g + 1) * 128], hT[:, kc, :w],
                                         start=(kc == 0), stop=(kc == 7))
                                S.cp(paT[:, g, t0:t0 + w], p[:, :w], e="act")
                            for cc in range(4):
                                pu = PS[2 + cc % 2]
                                pg = PS[4 + cc % 2]
                                for kc in range(8):
                                    S.mm(pu[:, :w], wab[:, kc, 512 + cc * 128:512 + (cc + 1) * 128], hT[:, kc, :w],
                                         start=(kc == 0), stop=(kc == 7))
                                for kc in range(8):
                                    S.mm(pg[:, :w], wab[:, kc, 1024 + cc * 128:1024 + (cc + 1) * 128], hT[:, kc, :w],
                                         start=(kc == 0), stop=(kc == 7))
                                s_ = sgt[cc % 2]
                                S.act(s_[:, :w], pg[:, :w], AF.Sigmoid)
                                dst = uext[:, cc, 15 + t0:15 + t0 + w] if t0 < TL else ucx[:, cc, 15:15 + TCX]
                                S.tt(dst, pu[:, :w], s_[:, :w], ALU.mult)
                        for i in range(2):
                            S.dma(pa_src[i].ap.re("p (g t) -> p g t", g=2), paT[:, 2 * i:2 * i + 2, 0:TL])
                            S.allgather(pa_dst[i], pa_src[i], groups)
                        for g in range(4):
                            for i in range(2):
                                p = PS[i]
                                S.mm(p[:, 0:256], paT[:, g, TL + 128 * i:TL + 128 * (i + 1)], fccs.ap)
                                S.cp(zc[:, i, :], p[:, 0:256], e="act")
                            p = PS[2 + g % 2]
                            for i in range(2):
                                S.mm(p[:, 0:256], zc[:, i, 0:128], c256[:, i, :], start=(i == 0), stop=False)
                                S.mm(p[:, 0:256], zc[:, i, 128:256], s256[:, i, :], start=False, stop=(i == 1))
                            S.cp(yac[:, g, :], p[:, 0:256])
                        S.barrier()
                    with ExitStack() as ph2:
                        hs = S.sb("hs", [128, 4, 2, 15], BF16, es=ph2)
                        S.cp(hs[:, :, 0, :], uext[:, :, 15:30], e="pool")
                        S.cp(hs[:, :, 1, :], uext[:, :, TL:TL + 15], e="pool")
                        S.dma(halo_src.ap.re("p (c s k) -> p c s k", c=4, s=2), hs.ap)
                        S.allgather(halo_dst, halo_src, groups)
                        hall = S.sb("hall", [128, 4, 4, 2, 15], BF16, es=ph2)
                        S.dma(hall.ap, halo_dst.ap.re("(r p) (c s k) -> p r c s k", p=128, c=4, s=2))
                        hm = S.sb("hm", [128, 8], F32, es=ph2)
                        S.dma(hm.ap, cin["k_hmask"].ap)
                        hl = S.sb("hl", [128, 4, 15], F32, es=ph2)
                        hr = S.sb("hr", [128, 4, 15], F32, es=ph2)
                        S.memset(hl.ap, 0.0)
                        S.memset(hr.ap, 0.0)
                        for r in range(4):
                            S.stt(hl.ap, hall[:, r, :, 1, :], hm[:, r:r + 1], hl.ap, ALU.mult, ALU.add)
                            S.stt(hr.ap, hall[:, r, :, 0, :], hm[:, 4 + r:5 + r], hr.ap, ALU.mult, ALU.add)
                        S.cp(uext[:, :, 0:15], hl.ap)
                        S.cp(uext[:, :, TL + 15:TL + 30], hr.ap)
                        cw = S.sb("cw", [128, 4, 31], F32, es=ph2)
                        cb = S.sb("cb", [128, 4], F32, es=ph2)
                        cg = S.sb("cg", [128, 4], F32, es=ph2)
                        S.dma(cw.ap, conv_wT[j])
                        S.dma(cb.ap, conv_bT[j])
                        S.dma(cg.ap, conv_gT[j])
                        dg = [S.sb("dg%d" % cc, [128, 31, 128], BF16, es=ph2) for cc in range(4)]
                        for cc in range(4):
                            for k in range(31):
                                S.ts(dg[cc][:, k, :], ident.ap, cw[:, cc, k:k + 1], None, ALU.mult,
                                     e=("dve" if k % 2 else "pool"))
                        vb = [S.sb("vb%d" % cc, [128, 512], F32, es=ph2) for cc in range(4)]
                        sqb = [S.sb("sqb%d" % i, [128, 512], BF16, es=ph2) for i in range(2)]
                        rsb = S.sb("rsb", [128, 512], F32, es=ph2)
                        for ci, (t0, w) in enumerate(CH):
                            ss = PS[6]
                            for cc in range(4):
                                p = PS[cc % 2]
                                for k in range(31):
                                    src = uext[:, cc, t0 + k:t0 + k + w] if t0 < TL else ucx[:, cc, k:k + w]
                                    S.mm(p[:, :w], dg[cc][:, k, :], src, start=(k == 0), stop=(k == 30))
                                S.act(vb[cc][:, :w], p[:, :w], AF.Identity, bias=cb[:, cc:cc + 1], scale=1.0)
                                s_ = sqb[cc % 2]
                                S.tt(s_[:, :w], vb[cc][:, :w], vb[cc][:, :w], ALU.mult, e="pool")
                                S.mm(ss[:, :w], ones_bf.ap, s_[:, :w], start=(cc == 0), stop=(cc == 3))
                            S.act(rsb[:, :w], ss[:, :w], AF.Sqrt, bias=eps_t.ap, scale=1.0 / 512)
                            S.recip(rsb[:, :w], rsb[:, :w])
                            for cc in range(4):
                                S.tt(vb[cc][:, :w], vb[cc][:, :w], rsb[:, :w], ALU.mult)
                                S.act(ybT[:, cc, t0:t0 + w], vb[cc][:, :w], AF.Silu, scale=cg[:, cc:cc + 1])
                        S.barrier()
                yal = S.sb("yal", [128, 4, TL], BF16, es=ph)
                with ExitStack() as ph2:
                    fccs = load_w(ph2, "fccs", cin["c_fccs"].ap, [128, 256])
                    rqre = load_w(ph2, "rqre", cin["c_rqre"].ap, [128, 4, 64])
                    rqim = load_w(ph2, "rqim", cin["c_rqim"].ap, [128, 4, 64])
                    f64c = load_w(ph2, "f64c", cin["k_f64c"].ap, [64, 16])
                    f64s = load_w(ph2, "f64s", cin["k_f64s"].ap, [64, 16])
                    twc = S.sb("twc", [64, 128], F32, es=ph2)
                    tws = S.sb("tws", [64, 128], F32, es=ph2)
                    S.dma(twc.ap, cin["c_twc"].ap)
                    S.dma(tws.ap, cin["c_tws"].ap)
                    paf = S.sb("paf", [128, 4 * TL], BF16, es=ph2)
                    Z = S.sb("Z", [128, 64, 256], BF16, es=ph2)
                    Vq = S.sb("Vq", [64, 128, 2, 32], BF16, es=ph2)
                    t4 = [S.sb("t4_%d" % i, [64, 8, 32], F32, es=ph2) for i in range(4)]
                    for g in range(4):
                        S.dma(paf.ap.re("p (r t) -> p r t", r=4),
                              pa_dst[g // 2].ap.re("(r p) (g t) -> p r g t", p=128, g=2)[:, :, g % 2, :])
                        for i in range(32):
                            p = PS[i % 2]
                            for h in range(2):
                                n2 = 2 * i + h
                                S.mm(p[:, 256 * h:256 * (h + 1)], paf[:, n2:4 * TL:64], fccs.ap)
                            S.cp(Z[:, 2 * i:2 * i + 2, :].re("p a b -> p (a b)"), p.ap,
                                 e=("act" if i % 2 else "dve"))
                        for qt in range(4):
                            for kb in range(16):
                                p = PS[2 + kb % 2]
                                for k8 in range(8):
                                    kc = kb * 8 + k8
                                    S.mm(p[0:64, 64 * k8:64 * (k8 + 1)], Z[:, :, kc], rqre[:, qt, :],
                                         start=True, stop=False)
                                    S.mm(p[0:64, 64 * k8:64 * (k8 + 1)], Z[:, :, 128 + kc], rqim[:, qt, :],
                                         start=False, stop=True)
                                U = p[0:64, :].re("p (k r a) -> p k r a", k=8, r=2)
                                tcv = twc[:, 32 * qt:32 * qt + 32].re("p (o a) -> p o a", o=1).bc([64, 8, 32])
                                tsv = tws[:, 32 * qt:32 * qt + 32].re("p (o a) -> p o a", o=1).bc([64, 8, 32])
                                S.tt(t4[0].ap, U[:, :, 0, :], tcv, ALU.mult)
                                S.tt(t4[1].ap, U[:, :, 1, :], tsv, ALU.mult)
                                S.tt(t4[2].ap, U[:, :, 0, :], tsv, ALU.mult)
                                S.tt(t4[3].ap, U[:, :, 1, :], tcv, ALU.mult)
                                S.tt(Vq[:, kb * 8:kb * 8 + 8, 0, :], t4[0].ap, t4[1].ap, ALU.subtract, e="pool")
                                S.tt(Vq[:, kb * 8:kb * 8 + 8, 1, :], t4[2].ap, t4[3].ap, ALU.add, e="pool")
                            p = PS[4 + qt % 2]
                            for a in range(32):
                                S.mm(p[:, 16 * a:16 * (a + 1)], Vq[:, :, 0, a], f64c.ap, start=True, stop=False)
                                S.mm(p[:, 16 * a:16 * (a + 1)], Vq[:, :, 1, a], f64s.ap, start=False, stop=True)
                            dst = yal[:, g, :].re("p (jj k) -> p k jj", k=128)[:, 32 * qt:32 * qt + 32, :]
                            S.cp(dst, p.ap.re("p (a jj) -> p a jj", a=32), e="act")
                    S.barrier()
                with ExitStack() as ph2:
                    wo = load_w(ph2, "wo", w_out_ab[j].re("(kc p) n -> p kc n", p=128), [128, 8, 1024])
                    for ci, (t0, w) in enumerate(CH):
                        st = 0 if t0 < TL else 1
                        for oc in range(8):
                            p = PS[oc % 4]
                            for kc in range(8):
                                if kc < 4:
                                    rhs = yal[:, kc, t0:t0 + w] if t0 < TL else yac[:, kc, :]
                                else:
                                    rhs = ybT[:, kc - 4, t0:t0 + w]
                                S.mm(p[:, :w], wo[:, kc, oc * 128:(oc + 1) * 128], rhs,
                                     start=(kc == 0), stop=(kc == 7))
                            S.stt(xv(oc, ci), p[:, :w], prm[:, 2, oc, st:st + 1], xv(oc, ci), ALU.mult, ALU.add)
                    S.barrier()

        def odd_phase(l):
            j = l // 2
            OK_, OKD, OV, OVD, OKR = 0, 4096, 12288, 16384, 24576
            with ExitStack() as ph:
                kctx = S.sb("kctx", [128, 2, TCX], BF16, es=ph)
                kdctx = S.sb("kdctx", [128, 4, TCX], BF16, es=ph)
                krctx = S.sb("krctx", [64, TCX], BF16, es=ph)
                vctx = S.sb("vctx", [128, 2, 256], BF16, es=ph)
                vdctx = S.sb("vdctx", [128, 2, 512], BF16, es=ph)
                with ExitStack() as ph2:
                    hTc = [S.sb("hTc%d" % i, [128, 8, 512], BF16, es=ph2) for i in range(2)]
                    wcd = load_w(ph2, "wcd", w_in_cd[j].re("(kc p) n -> p kc n", p=128), [128, 8, 1728])
                    wuq = load_w(ph2, "wuq", w_uq[j].re("(kc p) n -> p kc n", p=128), [128, 3, 768])
                    wukv = load_w(ph2, "wukv", w_ukv[j].re("(kc p) n -> p kc n", p=128), [128, 2, 1024])
                    wukvv = S.sb("wukvv", [128, 2, 4, 128], BF16, es=ph2)
                    for kc_ in range(2):
                        S.dma(wukvv[:, kc_], w_ukv[j].re("(kc p) (h two d) -> p kc h two d", p=128, h=4, two=2)[:, kc_, :, 1, :],
                              q="pool")
                    rot = load_w(ph2, "rot", cin["c_rot"].ap, [128, 128])
                    rc128 = S.sb("rc128", [128, 512], F32, es=ph2)
                    rs128 = S.sb("rs128", [128, 512], F32, es=ph2)
                    rc64 = S.sb("rc64", [64, 512], F32, es=ph2)
                    rs64 = S.sb("rs64", [64, 512], F32, es=ph2)
                    gq = S.sb("gq", [128, 8], F32, es=ph2)
                    S.dma(gq[:, 0:1], q_gT[j])
                    S.dma(gq[:, 1:2], k_gT[j])
                    S.dma(gq[:, 2:5], cq_gT[j])
                    S.dma(gq[:, 5:7], ckv_gT[j])
                    kst = S.sb("kst", [128, 6656], BF16, es=ph2)
                    LK, LKD, LKR, LV, LVD = 0, 1024, 3072, 3584, 4608
                    qst = S.sb("qst", [128, 12, 512], BF16, es=ph2)
                    sqb = [S.sb("sqb%d" % i, [128, 512], BF16, es=ph2) for i in range(2)]
                    rsb = S.sb("rsb", [128, 512], F32, es=ph2)
                    qn = [S.sb("qn%d" % i, [128, 512], BF16, es=ph2) for i in range(2)]
                    f1 = [S.sb("f1_%d" % i, [128, 512], F32, es=ph2) for i in range(2)]
                    f2 = [S.sb("f2_%d" % i, [128, 512], F32, es=ph2) for i in range(2)]
                    craw = S.sb("craw", [128, 3, 512], F32, es=ph2)
                    cn = S.sb("cn", [128, 3, 512], BF16, es=ph2)
                    kn = S.sb("kn", [128, 2, 512], BF16, es=ph2)
                    S.memset(qst.ap, 0.0, e="pool")
                    S.memset(kst.ap, 0.0, e="pool")
                    cnt = [0]

                    def rms_rope(p, w, t0, np_, gcol, dst, rope, inv_dim):
                        cnt[0] += 1
                        i = cnt[0] % 2
                        if gcol is not None:
                            S.act(sqb[i][:np_, :w], p, AF.Square)
                            ss = PS[6]
                            S.mm(ss[:np_, :w], ones_bf[:np_, :np_], sqb[i][:np_, :w])
                            S.act(rsb[:np_, :w], ss[:np_, :w], AF.Sqrt, bias=eps_t[:np_, :], scale=inv_dim)
                            S.recip(rsb[:np_, :w], rsb[:np_, :w])
                            S.tt(f1[i][:np_, :w], p, rsb[:np_, :w], ALU.mult)
                            tgt = qn[i][:np_, :w] if rope else dst
                            S.ts(tgt, f1[i][:np_, :w], gq[:np_, gcol:gcol + 1], None, ALU.mult)
                        else:
                            tgt = qn[i][:np_, :w] if rope else dst
                            S.cp(tgt, p, e="act")
                        if rope:
                            rp = PS[7]
                            S.mm(rp[:np_, :w], rot[:np_, :np_], qn[i][:np_, :w])
                            rc, rs_ = (rc128, rs128) if np_ == 128 else (rc64, rs64)
                            S.tt(f1[i][:np_, :w], qn[i][:np_, :w], rc[:np_, :w], ALU.mult, e="pool")
                            S.tt(f2[i][:np_, :w], rp[:np_, :w], rs_[:np_, :w], ALU.mult)
                            S.tt(dst, f1[i][:np_, :w], f2[i][:np_, :w], ALU.add, e="pool")

                    for ci in norm_mod(ph2, prm[:, 0], prm[:, 1],
                                       lambda ci, c: hTc[ci % 2][:, c, :CH[ci][1]], "n1"):
                        t0, w = CH[ci]
                        lat = t0 < TL
                        hT = hTc[ci % 2]
                        if lat:
                            S.dma(rc128.ap, cin["k_rc128"][:, t0:t0 + w])
                            S.dma(rs128.ap, cin["k_rs128"][:, t0:t0 + w], q="act")
                            S.dma(rc64.ap, cin["k_rc64"][:, t0:t0 + w])
                            S.dma(rs64.ap, cin["k_rs64"][:, t0:t0 + w], q="act")

                        def proj(pv, col0, ncol, wt=wcd, nk=8, src=None):
                            for kc in range(nk):
                                rhs = hT[:, kc, :w] if src is None else src[:, kc, :w]
                                S.mm(pv, wt[:, kc, col0:col0 + ncol], rhs, start=(kc == 0), stop=(kc == nk - 1))
                        for h in range(4):
                            p = PS[h % 2]
                            proj(p[:, :w], 128 * h, 128)
                            rms_rope(p[:, :w], w, t0, 128, 0, qst[:, h, :w], lat, 1.0 / 128)
                        for h in range(2):
                            p = PS[2 + h % 2]
                            proj(p[:, :w], 512 + 128 * h, 128)
                            dst = kst[:, LK + h * 512:LK + h * 512 + w] if lat else kctx[:, h, :]
                            rms_rope(p[:, :w], w, t0, 128, 1, dst, lat, 1.0 / 128)
                        for ti in range(w // 128):
                            p = PS[4 + ti % 2]
                            for kc in range(8):
                                S.mm(p[:, 0:256], hT[:, kc, ti * 128:(ti + 1) * 128], wcd[:, kc, 768:1024],
                                     start=(kc == 0), stop=(kc == 7))
                            tg = (t0 + ti * 128) // 128
                            dst = kst[:, LV + ti * 256:LV + (ti + 1) * 256] if lat else vctx[:, ti, :]
                            S.cp(dst, p[:, 0:256], e="act")
                        ss = PS[5]
                        for k3 in range(3):
                            p = PS[k3 % 2]
                            proj(p[:, :w], 1024 + 128 * k3, 128)
                            S.cp(craw[:, k3, :w], p[:, :w], e="act")
                            S.tt(sqb[k3 % 2][:, :w], craw[:, k3, :w], craw[:, k3, :w], ALU.mult, e="pool")
                            S.mm(ss[:, :w], ones_bf.ap, sqb[k3 % 2][:, :w], start=(k3 == 0), stop=(k3 == 2))
                        S.act(rsb[:, :w], ss[:, :w], AF.Sqrt, bias=eps_t.ap, scale=1.0 / 384)
                        S.recip(rsb[:, :w], rsb[:, :w])
                        for k3 in range(3):
                            S.tt(craw[:, k3, :w], craw[:, k3, :w], rsb[:, :w], ALU.mult)
                            S.ts(cn[:, k3, :w], craw[:, k3, :w], gq[:, 2 + k3:3 + k3], None, ALU.mult)
                        for h in range(4):
                            p = PS[h % 2]
                            proj(p[:, :w], 192 * h, 128, wt=wuq, nk=3, src=cn)
                            S.cp(qst[:, 4 + h, :w], p[:, :w], e="act")
                            p2 = PS[2 + h % 2]
                            proj(p2[0:64, :w], 192 * h + 128, 64, wt=wuq, nk=3, src=cn)
                            rms_rope(p2[0:64, :w], w, t0, 64, None, qst[0:64, 8 + h, :w], lat, None)
                        ss = PS[5]
                        for k2 in range(2):
                            p = PS[k2 % 2]
                            proj(p[:, :w], 1408 + 128 * k2, 128)
                            S.cp(craw[:, k2, :w], p[:, :w], e="act")
                            S.tt(sqb[k2 % 2][:, :w], craw[:, k2, :w], craw[:, k2, :w], ALU.mult, e="pool")
                            S.mm(ss[:, :w], ones_bf.ap, sqb[k2 % 2][:, :w], start=(k2 == 0), stop=(k2 == 1))
                        S.act(rsb[:, :w], ss[:, :w], AF.Sqrt, bias=eps_t.ap, scale=1.0 / 256)
                        S.recip(rsb[:, :w], rsb[:, :w])
                        for k2 in range(2):
                            S.tt(craw[:, k2, :w], craw[:, k2, :w], rsb[:, :w], ALU.mult)
                            S.ts(kn[:, k2, :w], craw[:, k2, :w], gq[:, 5 + k2:6 + k2], None, ALU.mult)
                        for h in range(4):
                            p = PS[h % 2]
                            proj(p[:, :w], 256 * h, 128, wt=wukv, nk=2, src=kn)
                            dst = kst[:, LKD + h * 512:LKD + h * 512 + w] if lat else kdctx[:, h, :]
                            S.cp(dst, p[:, :w], e="act")
                        for ti in range(w // 128):
                            p = PS[4 + ti % 2]
                            for k2 in range(2):
                                S.mm(p[:, 0:512], kn[:, k2, ti * 128:(ti + 1) * 128],
                                     wukvv[:, k2].re("p h d -> p (h d)"), start=(k2 == 0), stop=(k2 == 1))
                            tg = (t0 + ti * 128) // 128
                            dst = kst[:, LVD + ti * 512:LVD + (ti + 1) * 512] if lat else vdctx[:, ti, :]
                            S.cp(dst, p[:, 0:512])
                        p = PS[2]
                        proj(p[0:64, :w], 1664, 64)
                        dst = kst[0:64, LKR:LKR + w] if lat else krctx.ap
                        rms_rope(p[0:64, :w], w, t0, 64, None, dst, lat, None)
                        S.dma(q_scr[:, :, t0:t0 + w], qst[:, :, :w])
                        if lat:
                            nt_ = w // 128
                            tg0 = t0 // 128
                            for h in range(2):
                                S.dma(kvs_(OK_ + h * TL + t0, OK_ + h * TL + t0 + w), kst[:, LK + h * 512:LK + h * 512 + w])
                            for h in range(4):
                                S.dma(kvs_(OKD + h * TL + t0, OKD + h * TL + t0 + w),
                                      kst[:, LKD + h * 512:LKD + h * 512 + w], q="act")
                            S.dma(kvs_(OKR + t0, OKR + t0 + w), kst[:, LKR:LKR + w])
                            S.dma(kvs_(OV + tg0 * 256, OV + (tg0 + nt_) * 256), kst[:, LV:LV + nt_ * 256], q="act")
                            S.dma(kvs_(OVD + tg0 * 512, OVD + (tg0 + nt_) * 512), kst[:, LVD:LVD + nt_ * 512])
                    S.barrier()
                for i in range(7):
                    S.allgather(kv_dst[i], kv_src[i], groups)
                with ExitStack() as ph2:
                    wo = load_w(ph2, "wo", w_out_cd[j].re("(kc p) n -> p kc n", p=128), [128, 8, 1024])
                    NK = 66
                    Kf = S.sb("Kf", [128, NK * 128], BF16, es=ph2)
                    Krf = S.sb("Krf", [64, NK * 128], BF16, es=ph2)
                    Vf = S.sb("Vf", [128, NK, 128], BF16, es=ph2)
                    qh = S.sb("qh", [128, NT], BF16, es=ph2)
                    qrh = S.sb("qrh", [64, NT], BF16, es=ph2)
                    oh = S.sb("oh", [128, NT], BF16, es=ph2)
                    Pb = [S.sb("Pb%d" % i, [128, 512], BF16, es=ph2) for i in range(3)]
                    rd = S.sb("rd", [128, 512], F32, es=ph2)
                    S.cp(Krf[:, 0:TCX], krctx.ap, e="pool")
                    S.dma(Krf[:, TCX:].re("p (r t) -> p r t", r=4), kvd_(OKR, OKR + TL, rows=64))
                    pcount = [0]

                    def attend(qv, qrv, keys, scale, ocol, wq):
                        pcount[0] += 1
                        o_ps = PS[4 + pcount[0] % 2]
                        d_ps = PS[6 + pcount[0] % 2]

                        def qk(kc):
                            s_ps = PS[kc % 4]
                            if qrv is None:
                                S.mm(s_ps[:, :wq], Kf[:, kc * 128:(kc + 1) * 128], qv)
                            else:
                                S.mm(s_ps[:, :wq], Kf[:, kc * 128:(kc + 1) * 128], qv, start=True, stop=False)
                                S.mm(s_ps[:, :wq], Krf[:, kc * 128:(kc + 1) * 128], qrv, start=False, stop=True)
                        for kc in range(min(2, keys)):
                            qk(kc)
                        for kc in range(keys):
                            if kc + 2 < keys:
                                qk(kc + 2)
                            P = Pb[kc % 3]
                            S.act(P[:, :wq], PS[kc % 4][:, :wq], AF.Exp, scale=scale)
                            S.mm(o_ps[:, :wq], Vf[:, kc, :], P[:, :wq], start=(kc == 0), stop=(kc == keys - 1))
                            S.mm(d_ps[:, :wq], ones_bf.ap, P[:, :wq], start=(kc == 0), stop=(kc == keys - 1))
                        S.recip(rd[:, :wq], d_ps[:, :wq])
                        S.tt(oh[:, ocol:ocol + wq], o_ps[:, :wq], rd[:, :wq], ALU.mult)

                    for hidx in range(8):
                        mla = hidx >= 4
                        h = hidx % 4
                        if not mla:
                            if h % 2 == 0:
                                kvh = h // 2
                                S.cp(Kf[:, 0:TCX], kctx[:, kvh, :], e="pool")
                                S.dma(Kf[:, TCX:].re("p (r t) -> p r t", r=4),
                                      kvd_(OK_ + kvh * TL, OK_ + (kvh + 1) * TL))
                                S.cp(Vf[:, 0:2, :], vctx.ap.re("p i (h d) -> p i h d", h=2)[:, :, kvh, :], e="pool")
                                for r_ in range(4):
                                    S.dma(Vf[:, 2 + 16 * r_:2 + 16 * (r_ + 1), :],
                                          kvd_(OV, OV + 4096).re("p r (i h d) -> p r i h d", i=16, h=2)[:, r_, :, kvh, :],
                                          q=("sp" if r_ % 2 == 0 else "act"))
                            S.dma(qh.ap, q_scr[:, h, :])
                            scale = 128.0 ** -0.5
                        else:
                            S.cp(Kf[:, 0:TCX], kdctx[:, h, :], e="pool")
                            S.dma(Kf[:, TCX:].re("p (r t) -> p r t", r=4),
                                  kvd_(OKD + h * TL, OKD + (h + 1) * TL))
                            S.cp(Vf[:, 0:2, :], vdctx.ap.re("p i (h d) -> p i h d", h=4)[:, :, h, :], e="pool")
                            for hf in range(2):
                                for r_ in range(4):
                                    S.dma(Vf[:, 2 + 16 * r_ + 8 * hf:2 + 16 * r_ + 8 * hf + 8, :],
                                          kvd_(OVD + 4096 * hf, OVD + 4096 * (hf + 1)).re("p r (i h d) -> p r i h d", i=8, h=4)[:, r_, :, h, :],
                                          q=("sp" if r_ % 2 == 0 else "act"))
                            S.dma(qh.ap, q_scr[:, 4 + h, :])
                            S.dma(qrh.ap, q_scr[0:64, 8 + h, :])
                            scale = 192.0 ** -0.5
                        for qb in range(4):
                            attend(qh[:, qb * 512:(qb + 1) * 512], qrh[:, qb * 512:(qb + 1) * 512] if mla else None,
                                   NK, scale, qb * 512, 512)
                        attend(qh[:, TL:NT], qrh[:, TL:NT] if mla else None, 2, scale, TL, TCX)
                        for ci, (t0, w) in enumerate(CH):
                            st = 0 if t0 < TL else 1
                            for oc in range(8):
                                p = PS[oc % 4]
                                S.mm(p[:, :w], wo[:, hidx, oc * 128:(oc + 1) * 128], oh[:, t0:t0 + w])
                                S.stt(xv(oc, ci), p[:, :w], prm[:, 2, oc, st:st + 1], xv(oc, ci), ALU.mult, ALU.add)
                    S.barrier()

        import os
        skip = os.environ.get("DEV_SKIP", "").split(",")
        for l in range(l0, n_layers):
            if "mod" not in skip:
                mod_phase(l)
            if "mix" not in skip:
                if l % 2 == 0:
                    even_phase(l)
                else:
                    odd_phase(l)
            if "moe" not in skip:
                moe_phase(l)
        if final:
            with ExitStack() as ph:
                gf = S.sb("gf", [128, 8], F32, es=ph)
                S.dma(gf.ap, gfinT.ap)
                of = S.sb("of", [128, 8, 512], F32, es=ph)
                for ci in norm_mod(ph, gf, None, lambda ci, c: of[:, c, :CH[ci][1]], "nf"):
                    t0, w = CH[ci]
                    if t0 < TL:
                        S.dma(y[:, :, t0:t0 + w], of[:, :, :w])
                S.barrier()
        else:
            for c in range(8):
                S.dma(y[:, c, :], xT[:, c, 0:TL])
        S.finish([y])
        print("ninst", S.ninst, flush=True)
    return nc


def _fm(a):
    t = a.shape[0]
    return np.ascontiguousarray(a.T.reshape(8, 128, t).transpose(1, 0, 2))


def _vecT(v, nchunk):
    return np.ascontiguousarray(np.swapaxes(v.reshape(v.shape[:-1] + (nchunk, 128)), -1, -2))


def make_in_maps(inp, n_layers=4):
    f = lambda k: np.ascontiguousarray(np.asarray(inp[k], dtype=np.float32))
    shared = {
        "mod_bT": _vecT(f("mod_b"), 48), "gmixT": _vecT(f("norm_mix_g"), 8),
        "gffnT": _vecT(f("norm_ffn_g"), 8), "gfinT": _vecT(f("final_norm_g"), 8),
        "w_in_ab": f("w_in_ab"),
        "conv_wT": np.ascontiguousarray(f("conv_w").transpose(0, 2, 1).reshape(2, 4, 128, 31).transpose(0, 2, 1, 3)),
        "conv_bT": _vecT(f("conv_b"), 4), "conv_gT": _vecT(f("conv_norm_g"), 4),
        "w_out_ab": f("w_out_ab"), "w_in_cd": f("w_in_cd"),
        "q_gT": _vecT(f("q_norm_g"), 1), "k_gT": _vecT(f("k_norm_g"), 1),
        "cq_gT": _vecT(f("cq_norm_g"), 3), "ckv_gT": _vecT(f("ckv_norm_g"), 2),
        "w_uq": f("w_uq"), "w_ukv": f("w_ukv"), "w_out_cd": f("w_out_cd"),
        "rw": np.ascontiguousarray(np.concatenate([f("router_grp_w"), f("router_exp_w")], -1)),
        "rb": np.ascontiguousarray(np.concatenate([f("router_grp_b"), f("router_exp_b")], -1)[:, None, :]),
    }
    for l in range(n_layers):
        shared["ewg%d" % l] = f("exp_w_gate")[l]
        shared["ewu%d" % l] = f("exp_w_up")[l]
        shared["ewd%d" % l] = f("exp_w_down")[l]
        shared["mod_w%d" % l] = f("mod_w")[l]
    shared.update(_consts_common())
    x = f("x")
    ctx = f("ctx")
    c = f("c")
    cc = f("c_ctx")
    maps = []
    for core in range(8):
        b, q = core // 4, core % 4
        m = dict(shared)
        xt = np.concatenate([x[b, 2048 * q:2048 * (q + 1)], ctx[b]], 0)
        m["x0"] = _fm(xt)
        m["scT"] = np.ascontiguousarray(np.stack([c[b], cc], 0).reshape(2, 8, 128).transpose(2, 1, 0))
        m.update(_consts_core(q))
        maps.append(m)
    return maps


def assemble(res):
    out = np.zeros((2, 8192, 1024), np.float32)
    for core in range(8):
        b, q = core // 4, core % 4
        yv = np.asarray(res[core]["y"])
        out[b, 2048 * q:2048 * (q + 1), :] = yv.transpose(2, 1, 0).reshape(2048, 1024)
    return out


_NC = {}


def kernel(**inputs):
    key = (4, True)
    if key not in _NC:
        _NC[key] = build(4, True)
    maps = make_in_maps(inputs)
    res = run_bass_kernel_spmd(_NC[key], maps, core_ids=list(range(8)))
    return assemble(res.results)
```
